# Optimizing a Trainium2 kernel written in Bass

```python
import math
import jax, jax.numpy as jnp
from jax import lax
import numpy as np

D_MODEL = 4096
BATCH = 4
SEQ = 2048
DEPTH = 1

N_MEM = 256
Q_BLOCK = 128
ROPE_THETA = 10000.0
EPS = 1e-6
FOX_HEADS = 16
FOX_HEAD_DIM = 128
DSA_HEADS = 16
DSA_NOPE_DIM = 128
DSA_ROPE_DIM = 64
DSA_V_DIM = 128
Q_LORA = 1024
KV_LORA = 256
IDX_HEADS = 32
IDX_DIM = 128
DSA_TOPK_MAX = 256
MEM_HEADS = 4
MEM_HEAD_DIM = 128
N_GROUPS = 8
EXPERTS_PER_GROUP = 8
N_EXPERTS = N_GROUPS * EXPERTS_PER_GROUP
TOPK_IN_GROUP = 2
D_EXPERT = 512
EXPERT_BLOCK = 128

FOX_WIDTH = FOX_HEADS * FOX_HEAD_DIM
DSA_WIDTH = DSA_HEADS * DSA_V_DIM
IN_SPLITS = (FOX_WIDTH, FOX_WIDTH, FOX_WIDTH, FOX_HEADS, Q_LORA, KV_LORA, DSA_ROPE_DIM, IDX_DIM, IDX_HEADS, D_MODEL, D_MODEL)
IN_WIDTH = sum(IN_SPLITS)

kernel_name = "fox_dsa_gated_hier_moe_block"


def rmsnorm(x, g):
    x32 = x.astype(jnp.float32)
    y = x32 * lax.rsqrt(jnp.mean(x32 * x32, axis=-1, keepdims=True) + EPS)
    return (y * g.astype(jnp.float32)).astype(x.dtype)


def layernorm(x, g, b):
    x32 = x.astype(jnp.float32)
    mu = jnp.mean(x32, axis=-1, keepdims=True)
    xc = x32 - mu
    y = xc * lax.rsqrt(jnp.mean(xc * xc, axis=-1, keepdims=True) + EPS)
    return (y * g.astype(jnp.float32) + b.astype(jnp.float32)).astype(x.dtype)


def rope(x, pos):
    d = x.shape[-1]
    inv = jnp.power(ROPE_THETA, -jnp.arange(0, d, 2, dtype=jnp.float32) / d)
    ang = pos.astype(jnp.float32)[:, None] * inv[None, :]
    shape = (1, x.shape[1]) + (1,) * (x.ndim - 3) + (d // 2,)
    cos = jnp.cos(ang).reshape(shape)
    sin = jnp.sin(ang).reshape(shape)
    x32 = x.astype(jnp.float32)
    x1, x2 = x32[..., : d // 2], x32[..., d // 2:]
    return jnp.concatenate([x1 * cos - x2 * sin, x2 * cos + x1 * sin], axis=-1).astype(x.dtype)


def fox_attention(q, k, v, logf):
    B, S, H, dh = q.shape
    cum = jnp.cumsum(logf, axis=1).transpose(0, 2, 1)
    scale = dh ** -0.5
    outs = []
    for i in range(S // Q_BLOCK):
        q0, q1 = i * Q_BLOCK, (i + 1) * Q_BLOCK
        s = jnp.einsum('bqhd,bkhd->bhqk', q[:, q0:q1], k[:, :q1], preferred_element_type=jnp.float32) * scale
        decay = cum[:, :, q0:q1, None] - cum[:, :, None, :q1]
        causal = jnp.arange(q0, q1)[:, None] >= jnp.arange(q1)[None, :]
        s = jnp.where(causal, s + decay, -jnp.inf)
        p = jax.nn.softmax(s, axis=-1).astype(v.dtype)
        outs.append(jnp.einsum('bhqk,bkhd->bqhd', p, v[:, :q1]))
    return jnp.concatenate(outs, axis=1)


def dsa_attention(q_cat, kv_cat, q_idx, k_idx, w_idx, topk):
    B, S = q_cat.shape[:2]
    nb = S // Q_BLOCK

    def blocks(a):
        return a.reshape((B, nb, Q_BLOCK) + a.shape[2:]).swapaxes(0, 1)

    qpos = jnp.arange(S, dtype=jnp.int32).reshape(nb, Q_BLOCK)
    kpos = jnp.arange(S, dtype=jnp.int32)
    scale = (DSA_NOPE_DIM + DSA_ROPE_DIM) ** -0.5

    def one_block(args):
        qc, qi, wi, qp = args
        dots = jnp.einsum('bqhd,bkd->bqhk', qi, k_idx, preferred_element_type=jnp.float32)
        score = jnp.einsum('bqh,bqhk->bqk', wi, jax.nn.relu(dots))
        score = jnp.where(kpos[None, None, :] <= qp[None, :, None], score, -jnp.inf)
        _, sel = lax.top_k(score, topk)
        kv_sel = jax.vmap(lambda kv, idx: kv[idx])(kv_cat, sel)
        s = jnp.einsum('bqhc,bqkc->bqhk', qc, kv_sel, preferred_element_type=jnp.float32) * scale
        valid = sel <= qp[None, :, None]
        s = jnp.where(valid[:, :, None, :], s, -jnp.inf)
        p = jax.nn.softmax(s, axis=-1).astype(kv_cat.dtype)
        return jnp.einsum('bqhk,bqkr->bqhr', p, kv_sel[..., :KV_LORA])

    o = lax.map(one_block, (blocks(q_cat), blocks(q_idx), blocks(w_idx), qpos))
    return o.swapaxes(0, 1).reshape(B, S, o.shape[3], o.shape[4])


def mixer_block(h, pos, topk, w_in, b_f, g_q_lat, g_kv_lat, g_idx_k, b_idx_k, w_uq, w_idx_q, w_uk, w_uv, w_up_a, w_up_b, w_out):
    B, S, _ = h.shape
    cuts = np.cumsum(IN_SPLITS)[:-1].tolist()
    qa, ka, va, fa, cq, ckv, kr, ik, iw, ga, gb = jnp.split(h @ w_in, cuts, axis=-1)
    hs = (B, S, FOX_HEADS, FOX_HEAD_DIM)
    logf = jax.nn.log_sigmoid(fa.astype(jnp.float32) + b_f.astype(jnp.float32))
    y_a = fox_attention(qa.reshape(hs), ka.reshape(hs), va.reshape(hs), logf).reshape(B, S, FOX_WIDTH) @ w_up_a
    cq = rmsnorm(cq, g_q_lat)
    q = (cq @ w_uq).reshape(B, S, DSA_HEADS, DSA_NOPE_DIM + DSA_ROPE_DIM)
    q_nope, q_rope = q[..., :DSA_NOPE_DIM], rope(q[..., DSA_NOPE_DIM:], pos)
    q_lat = jnp.einsum('bshn,hnr->bshr', q_nope, w_uk)
    q_cat = jnp.concatenate([q_lat, q_rope], axis=-1)
    kv_cat = jnp.concatenate([rmsnorm(ckv, g_kv_lat), rope(kr, pos)], axis=-1)
    q_idx = rope((cq @ w_idx_q).reshape(B, S, IDX_HEADS, IDX_DIM), pos)
    k_idx = rope(layernorm(ik, g_idx_k, b_idx_k), pos)
    w_idx = iw.astype(jnp.float32) * (IDX_HEADS * IDX_DIM) ** -0.5
    o_lat = dsa_attention(q_cat, kv_cat, q_idx, k_idx, w_idx, topk)
    y_b = jnp.einsum('bshr,hrv->bshv', o_lat, w_uv).reshape(B, S, DSA_WIDTH) @ w_up_b
    merged = jax.nn.sigmoid(ga) * y_a + jax.nn.sigmoid(gb) * y_b
    return merged @ w_out


def memory_cross_attention(h, mem_n, w_qm, w_km, w_vm, w_om):
    B, S, _ = h.shape
    M = mem_n.shape[1]
    q = (h @ w_qm).reshape(B, S, MEM_HEADS, MEM_HEAD_DIM)
    k = (mem_n @ w_km).reshape(B, M, MEM_HEADS, MEM_HEAD_DIM)
    v = (mem_n @ w_vm).reshape(B, M, MEM_HEADS, MEM_HEAD_DIM)
    s = jnp.einsum('bqhd,bmhd->bhqm', q, k, preferred_element_type=jnp.float32) * MEM_HEAD_DIM ** -0.5
    p = jax.nn.softmax(s, axis=-1).astype(v.dtype)
    return jnp.einsum('bhqm,bmhd->bqhd', p, v).reshape(B, S, MEM_HEADS * MEM_HEAD_DIM) @ w_om


def hierarchical_moe(h, w_rg, b_rg, w_re, b_re, w_gate, w_up, w_down):
    B, S, D = h.shape
    N = B * S
    M = N * TOPK_IN_GROUP
    x = h.reshape(N, D)
    x32 = x.astype(jnp.float32)
    p_grp = jax.nn.softmax(x32 @ w_rg.astype(jnp.float32) + b_rg.astype(jnp.float32), axis=-1)
    gate_g, grp = lax.top_k(p_grp, 1)
    logit_e = (x32 @ w_re.astype(jnp.float32) + b_re.astype(jnp.float32)).reshape(N, N_GROUPS, EXPERTS_PER_GROUP)
    logit_e = logit_e[jnp.arange(N), grp[:, 0]]
    val_e, loc_e = lax.top_k(logit_e, TOPK_IN_GROUP)
    weight = gate_g * jax.nn.softmax(val_e, axis=-1)
    expert = grp * EXPERTS_PER_GROUP + loc_e
    e_flat = expert.reshape(M)
    w_flat = weight.reshape(M)
    tok_flat = jnp.arange(M, dtype=jnp.int32) // TOPK_IN_GROUP
    order = jnp.argsort(e_flat)
    e_sorted, tok_sorted, w_sorted = e_flat[order], tok_flat[order], w_flat[order]
    counts = jnp.bincount(e_flat, length=N_EXPERTS)
    starts = jnp.cumsum(counts) - counts
    padded = (counts + EXPERT_BLOCK - 1) // EXPERT_BLOCK * EXPERT_BLOCK
    pend = jnp.cumsum(padded)
    pstart = pend - padded
    dest = pstart[e_sorted] + jnp.arange(M, dtype=jnp.int32) - starts[e_sorted]
    cap = -(-(M + N_EXPERTS * (EXPERT_BLOCK - 1)) // EXPERT_BLOCK) * EXPERT_BLOCK
    nblk = cap // EXPERT_BLOCK
    x_buf = jnp.zeros((cap, D), x.dtype).at[dest].set(x[tok_sorted])
    blk_expert = jnp.minimum(jnp.searchsorted(pend, jnp.arange(nblk, dtype=jnp.int32) * EXPERT_BLOCK, side='right'), N_EXPERTS - 1)

    def expert_block(args):
        xb, e = args
        return (jax.nn.silu(xb @ w_gate[e]) * (xb @ w_up[e])) @ w_down[e]

    y_buf = lax.map(expert_block, (x_buf.reshape(nblk, EXPERT_BLOCK, D), blk_expert)).reshape(cap, D)
    y = y_buf[dest] * w_sorted[:, None].astype(x.dtype)
    return jax.ops.segment_sum(y, tok_sorted, num_segments=N).reshape(B, S, D)


def setup_inputs(seed: int = 0) -> dict:
    key = jax.random.key(seed)
    ks = iter(jax.random.split(key, 48))
    L = DEPTH

    def nrm(shape, scale):
        return jax.random.normal(next(ks), shape, jnp.float32) * scale

    def gain(shape):
        return 1.0 + 0.02 * jax.random.normal(next(ks), shape, jnp.float32)

    return {
        'x': nrm((BATCH, SEQ, D_MODEL), 1.0),
        'mem': nrm((BATCH, N_MEM, D_MODEL), 1.0),
        'g_norm_mix': gain((L, D_MODEL)),
        'w_in': nrm((L, D_MODEL, IN_WIDTH), D_MODEL ** -0.5),
        'b_f': jax.random.uniform(next(ks), (L, FOX_HEADS), jnp.float32, 1.0, 4.0),
        'g_q_lat': gain((L, Q_LORA)),
        'g_kv_lat': gain((L, KV_LORA)),
        'g_idx_k': gain((L, IDX_DIM)),
        'b_idx_k': nrm((L, IDX_DIM), 0.02),
        'w_uq': nrm((L, Q_LORA, DSA_HEADS * (DSA_NOPE_DIM + DSA_ROPE_DIM)), Q_LORA ** -0.5),
        'w_idx_q': nrm((L, Q_LORA, IDX_HEADS * IDX_DIM), Q_LORA ** -0.5),
        'w_uk': nrm((L, DSA_HEADS, DSA_NOPE_DIM, KV_LORA), DSA_NOPE_DIM ** -0.5),
        'w_uv': nrm((L, DSA_HEADS, KV_LORA, DSA_V_DIM), KV_LORA ** -0.5),
        'w_up_a': nrm((L, FOX_WIDTH, D_MODEL), FOX_WIDTH ** -0.5),
        'w_up_b': nrm((L, DSA_WIDTH, D_MODEL), DSA_WIDTH ** -0.5),
        'w_out': nrm((L, D_MODEL, D_MODEL), D_MODEL ** -0.5),
        'g_norm_mem_x': gain((L, D_MODEL)),
        'g_mem': gain((L, D_MODEL)),
        'w_qm': nrm((L, D_MODEL, MEM_HEADS * MEM_HEAD_DIM), D_MODEL ** -0.5),
        'w_km': nrm((L, D_MODEL, MEM_HEADS * MEM_HEAD_DIM), D_MODEL ** -0.5),
        'w_vm': nrm((L, D_MODEL, MEM_HEADS * MEM_HEAD_DIM), D_MODEL ** -0.5),
        'w_om': nrm((L, MEM_HEADS * MEM_HEAD_DIM, D_MODEL), (MEM_HEADS * MEM_HEAD_DIM) ** -0.5),
        'g_norm_ffn': gain((L, D_MODEL)),
        'w_rg': nrm((L, D_MODEL, N_GROUPS), D_MODEL ** -0.5),
        'b_rg': nrm((L, N_GROUPS), 0.01),
        'w_re': nrm((L, D_MODEL, N_EXPERTS), D_MODEL ** -0.5),
        'b_re': nrm((L, N_EXPERTS), 0.01),
        'w_gate': nrm((L, N_EXPERTS, D_MODEL, D_EXPERT), D_MODEL ** -0.5),
        'w_up': nrm((L, N_EXPERTS, D_MODEL, D_EXPERT), D_MODEL ** -0.5),
        'w_down': nrm((L, N_EXPERTS, D_EXPERT, D_MODEL), D_EXPERT ** -0.5),
        'g_final': gain((D_MODEL,)),
    }


def reference(x, mem, g_norm_mix, w_in, b_f, g_q_lat, g_kv_lat, g_idx_k, b_idx_k, w_uq, w_idx_q, w_uk, w_uv,
              w_up_a, w_up_b, w_out, g_norm_mem_x, g_mem, w_qm, w_km, w_vm, w_om, g_norm_ffn,
              w_rg, b_rg, w_re, b_re, w_gate, w_up, w_down, g_final):
    S = x.shape[1]
    pos = jnp.arange(S, dtype=jnp.int32)
    topk = min(DSA_TOPK_MAX, S // 4)
    for l in range(DEPTH):
        h = rmsnorm(x, g_norm_mix[l])
        x = x + mixer_block(h, pos, topk, w_in[l], b_f[l], g_q_lat[l], g_kv_lat[l], g_idx_k[l], b_idx_k[l],
                            w_uq[l], w_idx_q[l], w_uk[l], w_uv[l], w_up_a[l], w_up_b[l], w_out[l])
        x = x + memory_cross_attention(rmsnorm(x, g_norm_mem_x[l]), rmsnorm(mem, g_mem[l]),
                                       w_qm[l], w_km[l], w_vm[l], w_om[l])
        x = x + hierarchical_moe(rmsnorm(x, g_norm_ffn[l]), w_rg[l], b_rg[l], w_re[l], b_re[l],
                                 w_gate[l], w_up[l], w_down[l])
    return rmsnorm(x, g_final)
```

```python
import numpy as np
import ml_dtypes
from contextlib import ExitStack
import concourse.bass as bass
import concourse.mybir as mybir
from concourse.bass_utils import run_bass_kernel_spmd

F32 = mybir.dt.float32
BF16 = mybir.dt.bfloat16
AF = mybir.ActivationFunctionType
ALU = mybir.AluOpType
AX = mybir.AxisListType

D = 4096
S = 2048
NQ = 1024
EPS = 1e-6
NBLK = 16
OWN = ([0, 3, 4, 7, 8, 11, 12, 15], [1, 2, 5, 6, 9, 10, 13, 14])
IN_SPLITS = (2048, 2048, 2048, 16, 1024, 256, 64, 128, 32, 4096, 4096)
OFF = np.concatenate([[0], np.cumsum(IN_SPLITS)]).tolist()
O_QA, O_KA, O_VA, O_FA, O_CQ, O_CKV, O_KR, O_IK, O_IW, O_GA, O_GB = OFF[:11]
CAP = 256


class Dep:
    __slots__ = ("w", "r", "dsem", "dval", "multi")

    def __init__(self, multi=False):
        self.multi = multi
        self.w = {}
        self.r = {}
        self.dsem = None
        self.dval = 0


class EngS:
    def __init__(self, name, eng, sem):
        self.name, self.eng, self.sem = name, eng, sem
        self.cnt = 0
        self.waited = {}
        self.pend = []
        self.ninst = 0


class FW:
    def __init__(self, nc):
        self.nc = nc
        self.es = ExitStack()
        self.E = {}
        for name, eng in (("pe", nc.tensor), ("act", nc.scalar), ("dve", nc.vector),
                          ("pool", nc.gpsimd), ("sp", nc.sync)):
            sem = self.es.enter_context(nc.semaphore("s_" + name))
            self.E[name] = EngS(name, eng, sem)
        self.dma_deps = []
        self.free_sems = []

    def sb(self, name, shape, dtype, st=None):
        self.uid = getattr(self, "uid", 0) + 1
        return (st or self.es).enter_context(self.nc.sbuf_tensor("%s_u%d" % (name, self.uid), list(shape), dtype))

    def ps(self, name, shape, dtype=F32, st=None):
        self.uid = getattr(self, "uid", 0) + 1
        return (st or self.es).enter_context(self.nc.psum_tensor("%s_u%d" % (name, self.uid), list(shape), dtype))

    def _wait(self, e, sem, val):
        if val <= 0:
            return
        k = id(sem)
        if e.waited.get(k, (None, 0))[1] >= val:
            return
        e.eng.wait_ge(sem, val)
        e.waited[k] = (sem, val)
        e.ninst += 1

    def _pre(self, e, reads, writes):
        for d in reads:
            for k, (sem, val) in d.w.items():
                self._wait(e, sem, val)
        for d in writes:
            if not d.multi:
                for k, (sem, val) in d.w.items():
                    if sem is e.sem:
                        continue
                    self._wait(e, sem, val)
            for k, (sem, val) in d.r.items():
                if sem is e.sem:
                    continue
                self._wait(e, sem, val)

    def op(self, ename, fn, reads=(), writes=(), inc=True):
        e = self.E[ename]
        self._pre(e, reads, writes)
        ins = fn(e.eng)
        e.ninst += 1
        if inc:
            e.cnt += 1
            ins.then_inc(e.sem, 1)
            k = id(e.sem)
            for d in list(reads) + e.pend:
                if d.r.get(k, (None, 0))[1] < e.cnt:
                    d.r[k] = (e.sem, e.cnt)
            e.pend = []
            for d in writes:
                if d.multi:
                    d.w[k] = (e.sem, e.cnt)
                else:
                    d.w = {k: (e.sem, e.cnt)}
                    d.r = {}
        else:
            e.pend.extend(reads)
        return ins

    def dma(self, q, out, in_, sbd, reads=(), writes=(), **kw):
        e = self.E[q]
        if sbd.dsem is None:
            if self.free_sems:
                sbd.dsem, sbd.dval = self.free_sems.pop()
            else:
                self.nsem = getattr(self, "nsem", 0) + 1
                sbd.dsem = self.es.enter_context(self.nc.semaphore("d%d" % self.nsem))
                sbd.dval = 0
            self.dma_deps.append(sbd)
        sem = sbd.dsem
        k = id(sem)
        for d in reads:
            for kk, (s, v) in d.w.items():
                self._wait(e, s, v)
        for d in writes:
            for kk, (s, v) in ([] if d.multi else list(d.w.items())) + list(d.r.items()):
                if s is sem:
                    continue
                self._wait(e, s, v)
        ins = e.eng.dma_start(out=out, in_=in_, **kw)
        e.ninst += 1
        sbd.dval += 16
        ins.then_inc(sem, 16)
        val = sbd.dval
        for d in reads:
            if d.r.get(k, (None, 0))[1] < val:
                d.r[k] = (sem, val)
        for d in writes:
            if d.multi or set(d.w.keys()) <= {k}:
                d.w[k] = (sem, val)
            else:
                d.w = {k: (sem, val)}
            if not d.multi:
                d.r = {}
        return ins

    def barrier(self):
        names = ["pe", "act", "dve", "pool", "sp"]
        for n in names:
            e = self.E[n]
            for m in names:
                o = self.E[m]
                if o is not e:
                    self._wait(e, o.sem, o.cnt)
            for d in self.dma_deps:
                self._wait(e, d.dsem, d.dval)
        for d in self.dma_deps:
            self.free_sems.append((d.dsem, d.dval))
            d.dsem = None
        self.dma_deps = []


def _rope_tab(pos, d):
    inv = np.power(np.float32(10000.0), -np.arange(0, d, 2, dtype=np.float32) / np.float32(d)).astype(np.float32)
    ang = pos.astype(np.float32)[:, None] * inv[None, :]
    c, s = np.cos(ang).astype(np.float32), np.sin(ang).astype(np.float32)
    return np.concatenate([c, c], 1), np.concatenate([-s, s], 1)


def host_consts(par):
    own = OWN[par]
    oth = OWN[1 - par]
    perm = own + oth
    pos = (np.array(perm)[:, None] * 128 + np.arange(128)[None, :]).reshape(-1)
    c64, s64 = _rope_tab(pos, 64)
    c128, s128 = _rope_tab(pos, 128)
    prec = np.zeros((16, 16, 16), np.float32)
    for b1 in range(16):
        for b2 in range(16):
            prec[:, b1, b2] = 1.0 if perm[b2] < perm[b1] else 0.0
    visnb = np.zeros((128, 8, 4), np.float32)
    for i in range(8):
        v = 1.0 if oth[i] < own[i] else 0.0
        visnb[:, i, 0] = v
        visnb[:, i, 1] = -30000.0 * (1 - v)
        visnb[:, i, 2] = -1e30 * (1 - v)
    k = np.arange(128)
    sel63 = np.zeros((128, 128), np.float32)
    sel63[63, :] = 1.0
    return {
        "c_ident_bf": np.eye(128).astype(ml_dtypes.bfloat16),
        "c_ident_f": np.eye(128, dtype=np.float32),
        "c_tri_bf": (k[:, None] <= k[None, :]).astype(ml_dtypes.bfloat16),
        "c_tril_bf": (k[:, None] < k[None, :]).astype(ml_dtypes.bfloat16),
        "c_ones_bf": np.ones((128, 128), ml_dtypes.bfloat16),
        "c_ones_f": np.ones((128, 128), np.float32),
        "c_sel63": sel63,
        "c_cos64T": np.ascontiguousarray(c64.T), "c_sin64T": np.ascontiguousarray(s64.T),
        "c_cos128T": np.ascontiguousarray(c128[:NQ].T), "c_sin128T": np.ascontiguousarray(s128[:NQ].T),
        "c_cos128tm": np.ascontiguousarray(c128.reshape(16, 128, 128).transpose(1, 0, 2)),
        "c_sin128tm": np.ascontiguousarray(s128.reshape(16, 128, 128).transpose(1, 0, 2)),
        "c_prec": np.ascontiguousarray(prec.reshape(16, 256)),
        "c_visnb": visnb,
        "c_iota": np.tile(np.arange(CAP, dtype=np.float32)[None, :], (128, 1)),
        "c_causq": (k[:, None] >= k[None, :]).astype(np.float32),
    }


CONST_SPECS = {
    "c_ident_bf": ([128, 128], BF16), "c_ident_f": ([128, 128], F32), "c_tri_bf": ([128, 128], BF16),
    "c_tril_bf": ([128, 128], BF16), "c_ones_bf": ([128, 128], BF16), "c_ones_f": ([128, 128], F32),
    "c_sel63": ([128, 128], F32), "c_cos64T": ([64, S], F32), "c_sin64T": ([64, S], F32),
    "c_cos128T": ([128, NQ], F32), "c_sin128T": ([128, NQ], F32),
    "c_cos128tm": ([128, 16, 128], F32), "c_sin128tm": ([128, 16, 128], F32),
    "c_prec": ([16, 256], F32), "c_visnb": ([128, 8, 4], F32), "c_iota": ([128, CAP], F32),
    "c_causq": ([128, 128], F32),
}

W_SPECS = {
    "g_norm_mix": [D], "w_in": [D, 15856], "w_kr_sw": [D, 64], "b_f": [16, 1], "g_q_lat": [128, 8], "g_kv_lat": [128, 2],
    "g_idx_k": [128], "b_idx_k": [128], "w_uq_nope": [1024, 2048], "w_uq_rope": [1024, 1024],
    "w_uq_rope_sw": [1024, 1024], "w_idx_q": [1024, 4096], "w_idx_q_sw": [1024, 4096],
    "w_uk": [16, 128, 256], "w_uv": [16, 256, 128], "w_up_a": [2048, D], "w_up_b": [2048, D], "w_out": [D, D],
    "g_norm_mem_x": [D], "g_mem": [D], "w_qm": [D, 512], "w_km": [D, 512], "w_vm": [D, 512], "w_om": [512, D],
    "g_norm_ffn": [D], "w_r": [D, 72], "b_r": [72], "w_gate": [64, D, 512], "w_up": [64, D, 512],
    "w_down": [64, 512, D], "g_final": [D],
}


def _swap_halves(w, nheads, hd):
    k = w.shape[0]
    w3 = w.reshape(k, nheads, hd)
    return np.ascontiguousarray(np.concatenate([w3[:, :, hd // 2:], w3[:, :, :hd // 2]], axis=2).reshape(k, nheads * hd))


def host_weights(inp):
    g = lambda n: np.ascontiguousarray(np.asarray(inp[n], np.float32)[0])
    w_uq = g("w_uq").reshape(1024, 16, 192)
    w_uq_rope = np.ascontiguousarray(w_uq[:, :, 128:].reshape(1024, 1024))
    w_in = g("w_in")
    out = {
        "g_norm_mix": g("g_norm_mix"), "w_in": w_in,
        "w_kr_sw": _swap_halves(np.ascontiguousarray(w_in[:, O_KR:O_KR + 64]), 1, 64),
        "b_f": np.ascontiguousarray(g("b_f").reshape(16, 1)), "g_q_lat": np.ascontiguousarray(g("g_q_lat").reshape(8, 128).T), "g_kv_lat": np.ascontiguousarray(g("g_kv_lat").reshape(2, 128).T), "g_idx_k": g("g_idx_k"),
        "b_idx_k": g("b_idx_k"),
        "w_uq_nope": np.ascontiguousarray(w_uq[:, :, :128].reshape(1024, 2048)),
        "w_uq_rope": w_uq_rope, "w_uq_rope_sw": _swap_halves(w_uq_rope, 16, 64),
        "w_idx_q": g("w_idx_q"), "w_idx_q_sw": _swap_halves(g("w_idx_q"), 32, 128),
        "w_uk": g("w_uk"), "w_uv": g("w_uv"), "w_up_a": g("w_up_a"), "w_up_b": g("w_up_b"), "w_out": g("w_out"),
        "g_norm_mem_x": g("g_norm_mem_x"), "g_mem": g("g_mem"), "w_qm": g("w_qm"), "w_km": g("w_km"),
        "w_vm": g("w_vm"), "w_om": g("w_om"), "g_norm_ffn": g("g_norm_ffn"),
        "w_r": np.ascontiguousarray(np.concatenate([g("w_rg"), g("w_re")], axis=1)),
        "b_r": np.ascontiguousarray(np.concatenate([g("b_rg"), g("b_re")], axis=0)),
        "w_gate": g("w_gate"), "w_up": g("w_up"), "w_down": g("w_down"),
        "g_final": np.ascontiguousarray(np.asarray(inp["g_final"], np.float32)),
    }
    return out


def build_program(stop_after=99, dbg=False):
    nc = bass.Bass("TRN2", target_bir_lowering=False)
    fw = FW(nc)
    op, dma = fw.op, fw.dma

    def din(name, shape, dt=F32):
        return nc.dram_tensor(name, list(shape), dt, kind="ExternalInput").ap()

    dbg_outs = {}

    def dscr(name, shape, dt):
        kind = "ExternalOutput" if dbg else "Internal"
        t = nc.dram_tensor(name, list(shape), dt, kind=kind).ap()
        if dbg:
            dbg_outs[name] = t
        return t

    x_seq = din("x_seq", [S, D])
    mem_in = din("mem_b", [256, D])
    W = {n: din(n, s) for n, s in W_SPECS.items()}
    C = {n: din(n, s, dt) for n, (s, dt) in CONST_SPECS.items()}
    out_d = nc.dram_tensor("out", [NQ, D], F32, kind="ExternalOutput").ap()
    d_out = Dep(multi=True)

    kT_d = dscr("kT_d", [16, 128, S], BF16); d_kT = Dep(multi=True)
    v_d = dscr("v_d", [S, 2048], BF16); d_v = Dep(multi=True)
    qT_d = dscr("qT_d", [16, 128, NQ], BF16); d_qT = Dep(multi=True)
    gT_d = dscr("gT_d", [64, 128, NQ], BF16); d_gT = Dep(multi=True)
    qcat_d = dscr("qcat_d", [16, 320, NQ], BF16); d_qcat = Dep(multi=True)
    qidx_d = dscr("qidx_d", [32, 128, NQ], BF16); d_qidx = Dep(multi=True)
    foT_d = dscr("foT_d", [16, 128, NQ], BF16); d_foT = Dep(multi=True)
    obT_d = dscr("obT_d", [16, 128, NQ], BF16); d_obT = Dep(multi=True)
    x1_d = dscr("x1_d", [NQ, D], F32); d_x1 = Dep(multi=True)
    x2_d = dscr("x2_d", [NQ, D], F32); d_x2 = Dep(multi=True)
    yg_d = dscr("yg_d", [8, CAP, D], F32); d_yg = Dep(multi=True)

    G = fw.es

    def cload(name, shape, dt, src, q="sp"):
        t = fw.sb(name, shape, dt, G)
        d = Dep()
        dma(q, t[:], src, d, writes=[d])
        return t, d

    ident_bf, d_idb = cload("ident_bf", [128, 128], BF16, C["c_ident_bf"][:, :])
    ident_f, d_idf = cload("ident_f", [128, 128], F32, C["c_ident_f"][:, :])
    tri_bf, d_tri = cload("tri_bf", [128, 128], BF16, C["c_tri_bf"][:, :])
    ones_bf, d_onb = cload("ones_bf", [128, 128], BF16, C["c_ones_bf"][:, :])
    ones_f, d_onf = cload("ones_f", [128, 128], F32, C["c_ones_f"][:, :])
    visnb, d_vis = cload("visnb", [128, 8, 4], F32, C["c_visnb"][:, :, :])
    epsc = fw.sb("epsc", [128, 1], F32, G); d_eps = Dep()
    op("dve", lambda e: e.memset(epsc[:], EPS), writes=[d_eps])

    MS = ExitStack()
    G = MS
    kvcT = fw.sb("kvcT", [128, 3, S], BF16, G); d_kvcT = Dep(multi=True)
    kvlat = fw.sb("kvlat", [128, 16, 256], BF16, G); d_kvlat = Dep(multi=True)
    kidxT = fw.sb("kidxT", [128, S], BF16, G); d_kidxT = Dep(multi=True)
    negck = fw.sb("negck", [128, 16, 16], F32, G); d_negck = Dep()
    Rref = fw.sb("Rref", [128, 16, 16], F32, G); d_Rref = Dep()
    cqT = fw.sb("cqT", [128, 8, NQ], BF16, G); d_cqT = Dep(multi=True)
    Lsb = fw.sb("Lsb", [16, S], F32, G); dL = Dep(multi=True)
    wi_sb = fw.sb("wi_sb", [128, 8, 32], F32, G); d_wi = Dep(multi=True)

    def norm_T(tag, st, src, ntok, g_ap, dst_fn, f32_fn=None, src_deps=()):
        gbc = fw.sb(tag + "gbc", [128, D], BF16, st); dg = Dep()
        dma("pool", gbc[:], g_ap.partition_broadcast(128), dg, writes=[dg])
        xin = [fw.sb(tag + "xin%d" % i, [128, D], F32, st) for i in range(2)]
        dx = [Dep(), Dep()]
        xs = [fw.sb(tag + "xs%d" % i, [128, D], BF16, st) for i in range(2)]
        dxs = [Dep(), Dep()]
        ss = [fw.sb(tag + "ss%d" % i, [128, 2], F32, st) for i in range(2)]
        dss = [Dep(), Dep()]
        pst = [fw.ps(tag + "pt%d" % i, [128, 1024], BF16, st) for i in range(2)]
        dpt = [Dep(), Dep()]
        nev = 0
        for t in range(ntok // 128):
            b = t % 2
            dma("sp", xin[b][:], src[t * 128:(t + 1) * 128, :], dx[b], reads=list(src_deps), writes=[dx[b]])
            op("act", lambda e: e.activation(out=xs[b][:], in_=xin[b][:], func=AF.Square, accum_out=ss[b][:, 0:1]),
               reads=[dx[b]], writes=[dxs[b], dss[b]])
            op("dve", lambda e: e.tensor_scalar(out=ss[b][:, 1:2], in0=ss[b][:, 0:1], scalar1=1.0 / D, scalar2=EPS,
                                                op0=ALU.mult, op1=ALU.add), reads=[dss[b]], writes=[dss[b]])
            op("act", lambda e: e.activation(out=ss[b][:, 1:2], in_=ss[b][:, 1:2], func=AF.Sqrt),
               reads=[dss[b]], writes=[dss[b]])
            op("dve", lambda e: e.reciprocal(out=ss[b][:, 1:2], in_=ss[b][:, 1:2]), reads=[dss[b]], writes=[dss[b]])
            if f32_fn is not None:
                f32_fn(t, xin[b], dx[b], ss[b], dss[b], gbc, dg)
            op("dve", lambda e: e.scalar_tensor_tensor(out=xs[b][:], in0=xin[b][:], scalar=ss[b][:, 1:2], in1=gbc[:],
                                                       op0=ALU.mult, op1=ALU.mult),
               reads=[dx[b], dss[b], dg], writes=[dxs[b]])
            dst, ddst = dst_fn(t)
            for q4 in range(4):
                pb = nev % 2
                nev += 1
                for c8 in range(8):
                    c = q4 * 8 + c8
                    op("pe", lambda e: e.transpose(pst[pb][:, c8 * 128:(c8 + 1) * 128], xs[b][:, c * 128:(c + 1) * 128],
                                                   ident_bf[:]),
                       reads=[dxs[b], d_idb], writes=[dpt[pb]], inc=(c8 == 7))
                src_ps = pst[pb][:].rearrange("p (c t) -> p c t", c=8)
                if q4 % 2 == 0:
                    op("act", lambda e: e.activation(out=dst[:, q4 * 8:(q4 + 1) * 8, :], in_=src_ps, func=AF.Copy),
                       reads=[dpt[pb]], writes=[ddst])
                else:
                    op("dve", lambda e: e.tensor_copy(out=dst[:, q4 * 8:(q4 + 1) * 8, :], in_=src_ps),
                       reads=[dpt[pb]], writes=[ddst])

    class PsPool:
        def __init__(self, tag, n, st, shape=(128, 512), dt=F32):
            self.t = [fw.ps("%s%d" % (tag, i), list(shape), dt, st) for i in range(n)]
            self.d = [Dep() for _ in range(n)]
            self.i = 0

        def next(self):
            j = self.i % len(self.t)
            self.i += 1
            return self.t[j], self.d[j]

    class WPool:
        def __init__(self, tag, st, nbuf, nelem=8192):
            self.t = [fw.sb("%s%d" % (tag, i), [128, nelem], BF16, st) for i in range(nbuf)]
            self.d = [Dep() for _ in range(nbuf)]
            self.i = 0
            self.nelem = nelem

        def load(self, Wap, K, lo, n, ct):
            kc = K // 128
            assert kc * ct <= self.nelem
            j = self.i % len(self.t)
            self.i += 1
            v = self.t[j][:, 0:kc * ct].rearrange("p (c n) -> p c n", c=kc)
            dma("pool", v[:, :, 0:n], Wap.rearrange("(c p) n -> p c n", p=128)[:, :, lo:lo + n], self.d[j],
                writes=[self.d[j]])
            return v, self.d[j]

    def lin_fm(wp, Ws, K, col_lo, ncols, xchunks, pspool, consume, CT=256):
        kc = K // 128
        nw = len(Ws)
        ntile = (ncols + CT - 1) // CT
        for ti in range(ntile):
            lo = col_lo + ti * CT
            n = min(CT, col_lo + ncols - lo)
            wv = [wp.load(Ws[j], K, lo, n, CT) for j in range(nw)]
            for g0 in range(0, n, 128):
                gw = min(128, n - g0)
                for xi, (xfn, xdeps, xn) in enumerate(xchunks):
                    pss, dps = [], []
                    for j in range(nw):
                        ps, dp = pspool.next()
                        for c in range(kc):
                            op("pe", lambda e: e.matmul(ps[0:gw, 0:xn], lhsT=wv[j][0][:, c, g0:g0 + gw], rhs=xfn(c),
                                                        start=(c == 0), stop=(c == kc - 1)),
                               reads=([wv[j][1]] + list(xdeps)) if c == 0 else (), writes=[dp], inc=(c == kc - 1))
                        pss.append(ps[0:gw, 0:xn])
                        dps.append(dp)
                    consume(lo + g0, gw, xi, pss, dps)

    def lin_tm(wp, Wap, K, col_lo, ncols, xT_fn, xdeps_fn, ntiles, pspool, consume, CT=256):
        kc = K // 128
        ntile = (ncols + CT - 1) // CT
        for ti in range(ntile):
            lo = col_lo + ti * CT
            n = min(CT, col_lo + ncols - lo)
            wv, dw = wp.load(Wap, K, lo, n, CT)
            for t in range(ntiles):
                ps, dp = pspool.next()
                for c in range(kc):
                    op("pe", lambda e: e.matmul(ps[:, 0:n], lhsT=xT_fn(c, t), rhs=wv[:, c, 0:n],
                                                start=(c == 0), stop=(c == kc - 1)),
                       reads=([dw] + list(xdeps_fn(t))) if c == 0 else (), writes=[dp], inc=(c == kc - 1))
                consume(lo, n, t, ps[:, 0:n], dp)

    evac_rr = [0]

    def evac(out, in_, reads, writes, eng=None):
        if eng is None:
            eng = ("act", "dve")[evac_rr[0] % 2]
            evac_rr[0] += 1
        if eng == "act":
            op("act", lambda e: e.activation(out=out, in_=in_, func=AF.Copy), reads=reads, writes=writes)
        else:
            op("dve", lambda e: e.tensor_copy(out=out, in_=in_), reads=reads, writes=writes)

    hst = ExitStack()
    hT = fw.sb("hT", [128, 32, NQ], BF16, hst)
    dh = [Dep() for _ in range(8)]

    def load_hT(half):
        with ExitStack() as st:
            norm_T("nA%d" % half, st, x_seq[half * NQ:(half + 1) * NQ, :], NQ, W["g_norm_mix"],
                   lambda t: (hT[:, :, t * 128:(t + 1) * 128], dh[t]))
            fw.barrier()

    xch = [(lambda c: hT[:, c, 0:512], dh[0:4], 512), (lambda c: hT[:, c, 512:1024], dh[4:8], 512)]

    def hT_tile(c, t):
        return hT[:, c, t * 128:(t + 1) * 128]

    def kside(half):
        T0 = half * NQ
        with ExitStack() as st:
            pp = PsPool("pk%d" % half, 6, st)
            wp = WPool("wk", st, 3)
            kstage = [fw.sb("kstg%d" % i, [128, NQ], BF16, st) for i in range(2)]
            dks = [Dep(), Dep()]

            def cons_k(col, gw, xi, pss, dps):
                h = (col - O_KA) // 128
                b = h % 2
                evac(kstage[b][:, xi * 512:(xi + 1) * 512], pss[0], [dps[0]], [dks[b]])
                if xi == 1:
                    dma("sp", kT_d[h, :, T0:T0 + NQ], kstage[b][:], dks[b], reads=[dks[b]], writes=[d_kT])
            lin_fm(wp, [W["w_in"]], D, O_KA, 2048, xch, pp, cons_k)

            nbf = fw.sb("nbf", [16, 1], F32, st); dnbf = Dep()
            dma("sp", nbf[:], W["b_f"][:, :], dnbf, writes=[dnbf])
            op("act", lambda e: e.mul(nbf[:], nbf[:], -1.0), reads=[dnbf], writes=[dnbf])

            def cons_f(col, gw, xi, pss, dps):
                sl = Lsb[:, T0 + xi * 512:T0 + (xi + 1) * 512]
                op("act", lambda e: e.activation(out=sl, in_=pss[0], func=AF.Exp, bias=nbf[:, 0:1], scale=-1.0),
                   reads=[dps[0], dnbf], writes=[dL])
                op("act", lambda e: e.activation(out=sl, in_=sl, func=AF.Ln, bias=1.0, scale=1.0), reads=[dL], writes=[dL])
            lin_fm(wp, [W["w_in"]], D, O_FA, 16, xch, pp, cons_f, CT=16)

            xc = fw.sb("xckv", [128, 2, NQ], BF16, st); dxc = Dep(multi=True)
            sq = fw.sb("sqkv", [128, 2, NQ], BF16, st); dsq = Dep(multi=True)

            def cons_ckv(col, gw, xi, pss, dps):
                g = (col - O_CKV) // 128
                op("act", lambda e: e.activation(out=xc[:, g, xi * 512:(xi + 1) * 512], in_=pss[0], func=AF.Copy),
                   reads=[dps[0]], writes=[dxc])
                op("dve", lambda e: e.tensor_tensor(out=sq[:, g, xi * 512:(xi + 1) * 512], in0=pss[0],
                                                    in1=xc[:, g, xi * 512:(xi + 1) * 512], op=ALU.mult),
                   reads=[dps[0], dxc], writes=[dsq])
            lin_fm(wp, [W["w_in"]], D, O_CKV, 256, xch, pp, cons_ckv)
            gkv = fw.sb("gkv", [128, 2], F32, st); dgkv = Dep()
            dma("sp", gkv[:], W["g_kv_lat"][:, :], dgkv, writes=[dgkv])
            rs = fw.sb("rskv", [128, 512], F32, st); drs = Dep()
            for xi in range(2):
                ps, dp = pp.next()
                for g in range(2):
                    op("pe", lambda e: e.matmul(ps[:, :], lhsT=ones_bf[:], rhs=sq[:, g, xi * 512:(xi + 1) * 512],
                                                start=(g == 0), stop=(g == 1)), reads=[d_onb, dsq], writes=[dp], inc=(g == 1))
                op("act", lambda e: e.activation(out=rs[:], in_=ps[:, :], func=AF.Sqrt, scale=1.0 / 256, bias=epsc[:, 0:1]),
                   reads=[dp, d_eps], writes=[drs])
                op("dve", lambda e: e.reciprocal(out=rs[:], in_=rs[:]), reads=[drs], writes=[drs])
                for g in range(2):
                    op("dve", lambda e: e.scalar_tensor_tensor(out=kvcT[:, g, T0 + xi * 512:T0 + (xi + 1) * 512],
                                                               in0=xc[:, g, xi * 512:(xi + 1) * 512], scalar=gkv[:, g:g + 1],
                                                               in1=rs[:], op0=ALU.mult, op1=ALU.mult),
                       reads=[dxc, dgkv, drs], writes=[d_kvcT])
            ptk = fw.ps("ptk", [128, 1024], BF16, st); dptk = Dep()
            for bl4 in range(2):
                for j in range(4):
                    bl = half * 8 + bl4 * 4 + j
                    for g in range(2):
                        last = (j == 3 and g == 1)
                        op("pe", lambda e: e.transpose(ptk[:, (j * 2 + g) * 128:(j * 2 + g + 1) * 128],
                                                       kvcT[:, g, bl * 128:(bl + 1) * 128], ident_bf[:]),
                           reads=[d_kvcT, d_idb], writes=[dptk], inc=last)
                b0 = half * 8 + bl4 * 4
                evac(kvlat[:, b0:b0 + 4, :], ptk[:].rearrange("p (a b) -> p a b", a=4), [dptk], [d_kvlat])

            cos64 = fw.sb("cos64", [64, NQ], F32, st); dc64 = Dep()
            sin64 = fw.sb("sin64", [64, NQ], F32, st); ds64 = Dep()
            dma("sp", cos64[:], C["c_cos64T"][:, T0:T0 + NQ], dc64, writes=[dc64])
            dma("sp", sin64[:], C["c_sin64T"][:, T0:T0 + NQ], ds64, writes=[ds64])
            tA = fw.sb("tA", [64, 512], F32, st); dtA = Dep()
            tB = fw.sb("tB", [64, 512], F32, st); dtB = Dep()

            def cons_kr(col, gw, xi, pss, dps):
                op("dve", lambda e: e.tensor_tensor(out=tA[:, :], in0=pss[0], in1=cos64[:, xi * 512:(xi + 1) * 512],
                                                    op=ALU.mult), reads=[dps[0], dc64], writes=[dtA])
                op("dve", lambda e: e.tensor_tensor(out=tB[:, :], in0=pss[1], in1=sin64[:, xi * 512:(xi + 1) * 512],
                                                    op=ALU.mult), reads=[dps[1], ds64], writes=[dtB])
                op("pool", lambda e: e.tensor_tensor(out=kvcT[0:64, 2, T0 + xi * 512:T0 + (xi + 1) * 512], in0=tA[:, :],
                                                     in1=tB[:, :], op=ALU.add), reads=[dtA, dtB], writes=[d_kvcT])
            lin_fm(wp, [W["w_in"][:, O_KR:O_KR + 64], W["w_kr_sw"]], D, 0, 64, xch, pp, cons_kr, CT=64)

            vstage = [fw.sb("vstg%d" % i, [128, 256], BF16, st) for i in range(2)]
            dvs = [Dep(), Dep()]
            vcnt = [0]

            def cons_v(lo, n, t, ps, dp):
                b = vcnt[0] % 2
                vcnt[0] += 1
                evac(vstage[b][:, 0:n], ps, [dp], [dvs[b]])
                dma("sp", v_d[T0 + t * 128:T0 + (t + 1) * 128, lo - O_VA:lo - O_VA + n], vstage[b][:, 0:n], dvs[b],
                    reads=[dvs[b]], writes=[d_v])
            lin_tm(wp, W["w_in"], D, O_VA, 2048, hT_tile, lambda t: [dh[t]], 8, pp, cons_v, CT=256)

            gi = fw.sb("gi", [128, 128], F32, st); dgi = Dep()
            bi = fw.sb("bi", [128, 128], F32, st); dbi = Dep()
            dma("sp", gi[:], W["g_idx_k"].partition_broadcast(128), dgi, writes=[dgi])
            dma("sp", bi[:], W["b_idx_k"].partition_broadcast(128), dbi, writes=[dbi])
            c128 = fw.sb("c128tm", [128, 8, 128], F32, st); dc128 = Dep()
            s128 = fw.sb("s128tm", [128, 8, 128], F32, st); ds128 = Dep()
            dma("sp", c128[:], C["c_cos128tm"][:, half * 8:(half + 1) * 8, :], dc128, writes=[dc128])
            dma("sp", s128[:], C["c_sin128tm"][:, half * 8:(half + 1) * 8, :], ds128, writes=[ds128])
            bst = fw.sb("bst", [128, 8], F32, st); dbst = Dep()
            xk = fw.sb("xk", [128, 128], F32, st); dxk = Dep()
            xr = fw.sb("xr", [128, 128], F32, st); dxr = Dep()
            xo = fw.sb("xo", [128, 128], BF16, st); dxo = Dep()
            ptx = fw.ps("ptx", [128, 128], BF16, st); dptx = Dep()

            def cons_ik(lo, n, t, ps, dp):
                op("dve", lambda e: e.bn_stats(out=bst[:, 0:6], in_=ps), reads=[dp], writes=[dbst])
                op("dve", lambda e: e.bn_aggr(out=bst[:, 6:8], in_=bst[:, 0:6]), reads=[dbst], writes=[dbst])
                op("act", lambda e: e.activation(out=bst[:, 7:8], in_=bst[:, 7:8], func=AF.Sqrt, bias=epsc[:, 0:1], scale=1.0),
                   reads=[dbst, d_eps], writes=[dbst])
                op("dve", lambda e: e.reciprocal(out=bst[:, 7:8], in_=bst[:, 7:8]), reads=[dbst], writes=[dbst])
                op("dve", lambda e: e.tensor_scalar(out=xk[:], in0=ps, scalar1=bst[:, 6:7], scalar2=bst[:, 7:8],
                                                    op0=ALU.subtract, op1=ALU.mult), reads=[dp, dbst], writes=[dxk])
                op("dve", lambda e: e.tensor_tensor(out=xk[:], in0=xk[:], in1=gi[:], op=ALU.mult), reads=[dxk, dgi], writes=[dxk])
                op("dve", lambda e: e.tensor_tensor(out=xk[:], in0=xk[:], in1=bi[:], op=ALU.add), reads=[dxk, dbi], writes=[dxk])
                op("pool", lambda e: e.tensor_tensor(out=xr[:, 0:64], in0=xk[:, 64:128], in1=s128[:, t, 0:64], op=ALU.mult),
                   reads=[dxk, ds128], writes=[dxr])
                op("pool", lambda e: e.tensor_tensor(out=xr[:, 64:128], in0=xk[:, 0:64], in1=s128[:, t, 64:128], op=ALU.mult),
                   reads=[dxk, ds128], writes=[dxr])
                op("dve", lambda e: e.tensor_tensor(out=xk[:], in0=xk[:], in1=c128[:, t, :], op=ALU.mult),
                   reads=[dxk, dc128, dxr], writes=[dxk])
                op("dve", lambda e: e.tensor_tensor(out=xo[:], in0=xk[:], in1=xr[:], op=ALU.add), reads=[dxk, dxr], writes=[dxo])
                op("pe", lambda e: e.transpose(ptx[:], xo[:], ident_bf[:]), reads=[dxo, d_idb], writes=[dptx])
                evac(kidxT[:, T0 + t * 128:T0 + (t + 1) * 128], ptx[:], [dptx], [d_kidxT])
            lin_tm(wp, W["w_in"], D, O_IK, 128, hT_tile, lambda t: [dh[t]], 8, pp, cons_ik, CT=128)
            fw.barrier()

    def qside():
        with ExitStack() as st:
            pp = PsPool("pq", 6, st)
            wp = WPool("wq", st, 3)
            qstage = [fw.sb("qstg%d" % i, [128, NQ], BF16, st) for i in range(2)]
            dqs = [Dep(), Dep()]

            def cons_q(col, gw, xi, pss, dps):
                h = (col - O_QA) // 128
                b = h % 2
                evac(qstage[b][:, xi * 512:(xi + 1) * 512], pss[0], [dps[0]], [dqs[b]])
                if xi == 1:
                    dma("sp", qT_d[h, :, :], qstage[b][:], dqs[b], reads=[dqs[b]], writes=[d_qT])
            lin_fm(wp, [W["w_in"]], D, O_QA, 2048, xch, pp, cons_q)

            def cons_g(col, gw, xi, pss, dps):
                gidx = (col - O_GA) // 128
                b = gidx % 2
                op("act", lambda e: e.activation(out=qstage[b][:, xi * 512:(xi + 1) * 512], in_=pss[0], func=AF.Sigmoid),
                   reads=[dps[0]], writes=[dqs[b]])
                if xi == 1:
                    dma("sp", gT_d[gidx, :, :], qstage[b][:], dqs[b], reads=[dqs[b]], writes=[d_gT])
            lin_fm(wp, [W["w_in"]], D, O_GA, 8192, xch, pp, cons_g)

            def cons_iw(lo, n, t, ps, dp):
                op("act", lambda e: e.activation(out=wi_sb[:, t, :], in_=ps, func=AF.Copy, scale=float(4096 ** -0.5)),
                   reads=[dp], writes=[d_wi])
            lin_tm(wp, W["w_in"], D, O_IW, 32, hT_tile, lambda t: [dh[t]], 8, pp, cons_iw, CT=32)

            gq = fw.sb("gq", [128, 8], F32, st); dgq = Dep()
            dma("sp", gq[:], W["g_q_lat"][:, :], dgq, writes=[dgq])
            xc = fw.sb("xcq", [128, 8, 512], BF16, st); dxc = Dep(multi=True)
            sq = fw.sb("sqq", [128, 8, 512], BF16, st); dsq = Dep(multi=True)
            rs = fw.sb("rsq", [128, 512], F32, st); drs = Dep()
            for xi in range(2):
                def cons_cq(col, gw, xi_, pss, dps):
                    g = (col - O_CQ) // 128
                    op("act", lambda e: e.activation(out=xc[:, g, :], in_=pss[0], func=AF.Copy), reads=[dps[0]], writes=[dxc])
                    op("dve", lambda e: e.tensor_tensor(out=sq[:, g, :], in0=pss[0], in1=xc[:, g, :], op=ALU.mult),
                       reads=[dps[0], dxc], writes=[dsq])
                lin_fm(wp, [W["w_in"]], D, O_CQ, 1024, [xch[xi]], pp, cons_cq)
                ps, dp = pp.next()
                for g in range(8):
                    op("pe", lambda e: e.matmul(ps[:, :], lhsT=ones_bf[:], rhs=sq[:, g, :], start=(g == 0), stop=(g == 7)),
                       reads=[d_onb, dsq], writes=[dp], inc=(g == 7))
                op("act", lambda e: e.activation(out=rs[:], in_=ps[:, :], func=AF.Sqrt, scale=1.0 / 1024, bias=epsc[:, 0:1]),
                   reads=[dp, d_eps], writes=[drs])
                op("dve", lambda e: e.reciprocal(out=rs[:], in_=rs[:]), reads=[drs], writes=[drs])
                for g in range(8):
                    op("dve", lambda e: e.scalar_tensor_tensor(out=cqT[:, g, xi * 512:(xi + 1) * 512], in0=xc[:, g, :],
                                                               scalar=gq[:, g:g + 1], in1=rs[:], op0=ALU.mult, op1=ALU.mult),
                       reads=[dxc, dgq, drs], writes=[d_cqT, dxc, dsq])
            fw.barrier()

    def fox_cum():
        with ExitStack() as st:
            Wc = fw.sb("Wc", [16, 16, 128], F32, st); dWc = Dep()
            onesr = fw.sb("onesr", [16, 128], F32, st); dor = Dep()
            op("dve", lambda e: e.memset(onesr[:], 1.0), writes=[dor])
            for bl in range(16):
                op("dve", lambda e: e.tensor_tensor_scan(out=Wc[:, bl, :], data0=onesr[:], data1=Lsb[:, bl * 128:(bl + 1) * 128],
                                                         initial=0.0, op0=ALU.mult, op1=ALU.add),
                   reads=[dL, dor], writes=[dWc])
            prec_sb = fw.sb("prec_sb", [16, 16, 16], F32, st); dpr = Dep()
            dma("sp", prec_sb[:], C["c_prec"].rearrange("p (a b) -> p a b", a=16), dpr, writes=[dpr])
            tmpP = fw.sb("tmpP", [16, 16, 16], F32, st); dtp = Dep()
            Pfx = fw.sb("Pfx", [16, 16], F32, st); dpf = Dep()
            for b1 in range(16):
                op("dve", lambda e: e.tensor_tensor(out=tmpP[:, b1, :], in0=prec_sb[:, b1, :], in1=Wc[:, :, 127],
                                                    op=ALU.mult), reads=[dpr, dWc], writes=[dtp])
            op("dve", lambda e: e.tensor_reduce(out=Pfx[:], in_=tmpP[:], axis=AX.X, op=ALU.add), reads=[dtp], writes=[dpf])
            for bl in range(16):
                op("dve", lambda e: e.tensor_scalar(out=Wc[:, bl, :], in0=Wc[:, bl, :], scalar1=Pfx[:, bl:bl + 1], scalar2=None,
                                                    op0=ALU.add), reads=[dpf, dWc], writes=[dWc])
            pT = fw.ps("pTck", [128, 256], F32, st); dpT = Dep()
            for bl in range(16):
                op("pe", lambda e: e.transpose(pT[:, bl * 16:(bl + 1) * 16], Wc[:, bl, :], ident_f[0:16, 0:16]),
                   reads=[dWc, d_idf], writes=[dpT], inc=(bl == 15))
            evac(negck[:].rearrange("p a b -> p (a b)"), pT[:], [dpT], [d_negck], eng="dve")
            sel63, dsel = fw.sb("sel63", [128, 128], F32, st), Dep()
            dma("sp", sel63[:], C["c_sel63"][:, :], dsel, writes=[dsel])
            op("pe", lambda e: e.matmul(pT[:], lhsT=sel63[:], rhs=negck[:].rearrange("p a b -> p (a b)"), start=True, stop=True),
               reads=[dsel, d_negck], writes=[dpT])
            evac(Rref[:].rearrange("p a b -> p (a b)"), pT[:], [dpT], [d_Rref], eng="dve")
            fw.barrier()

    load_hT(0)
    kside(0)
    qside()
    load_hT(1)
    kside(1)
    hst.close()
    fox_cum()

    def dump_sb(name, t, shape, dt, dep):
        o = nc.dram_tensor("dbg_" + name, list(shape), dt, kind="ExternalOutput").ap()
        dd = Dep()
        dma("sp", o, t, dep, reads=[dep], writes=[dd])
        dbg_outs["dbg_" + name] = o

    if stop_after <= 1:
        dump_sb("negck", negck[:], [128, 16, 16], F32, d_negck)
        dump_sb("kvcT", kvcT[:], [128, 3, S], BF16, d_kvcT)
        dump_sb("kidxT", kidxT[:], [128, S], BF16, d_kidxT)
        dump_sb("cqT", cqT[:], [128, 8, NQ], BF16, d_cqT)
        fw.barrier()
        return nc, fw, dbg_outs, {}

    xchq = [(lambda c: cqT[:, c, 0:512], [d_cqT], 512), (lambda c: cqT[:, c, 512:1024], [d_cqT], 512)]
    with ExitStack() as st:
        pp = PsPool("pc", 6, st)
        wp = WPool("wc", st, 4, nelem=2048)
        wuk = fw.sb("wuk", [128, 16, 256], BF16, st); dwuk = Dep()
        dma("pool", wuk[:], W["w_uk"].rearrange("h n r -> n h r"), dwuk, writes=[dwuk])
        qn = [fw.sb("qn%d" % i, [128, NQ], BF16, st) for i in range(2)]; dqn = [Dep(), Dep()]
        qcs = [fw.sb("qcs%d" % i, [128, NQ], BF16, st) for i in range(2)]; dqcs = [Dep(), Dep()]
        cnt = [0]

        def cons_qn(col, gw, xi, pss, dps):
            h = col // 128
            b = h % 2
            evac(qn[b][:, xi * 512:(xi + 1) * 512], pss[0], [dps[0]], [dqn[b]])
            if xi == 1:
                for rc in range(2):
                    b2 = cnt[0] % 2
                    cnt[0] += 1
                    for tc in range(2):
                        ps, dp = pp.next()
                        op("pe", lambda e: e.matmul(ps[:, :], lhsT=wuk[:, h, rc * 128:(rc + 1) * 128],
                                                    rhs=qn[b][:, tc * 512:(tc + 1) * 512], start=True, stop=True),
                           reads=[dwuk, dqn[b]], writes=[dp])
                        evac(qcs[b2][:, tc * 512:(tc + 1) * 512], ps[:, :], [dp], [dqcs[b2]])
                    dma("sp", qcat_d[h, rc * 128:(rc + 1) * 128, :], qcs[b2][:], dqcs[b2], reads=[dqcs[b2]], writes=[d_qcat])
        lin_fm(wp, [W["w_uq_nope"]], 1024, 0, 2048, xchq, pp, cons_qn)

        tA = fw.sb("tAc", [128, 512], F32, st); dtA = Dep()
        tB = fw.sb("tBc", [128, 512], F32, st); dtB = Dep()
        cs = fw.sb("cs64x2", [128, NQ], F32, st); dcs = Dep()
        sn = fw.sb("sn64x2", [128, NQ], F32, st); dsn = Dep()
        for hh in range(2):
            dma("sp", cs[hh * 64:(hh + 1) * 64, :], C["c_cos64T"][:, 0:NQ], dcs, writes=[dcs])
            dma("sp", sn[hh * 64:(hh + 1) * 64, :], C["c_sin64T"][:, 0:NQ], dsn, writes=[dsn])

        def rope_cons(cosT, dcos, sinT, dsin, store):
            def cons(col, gw, xi, pss, dps):
                g = col // 128
                b = g % 2
                sl = slice(xi * 512, (xi + 1) * 512)
                op("dve", lambda e: e.tensor_tensor(out=tA[:, :], in0=pss[0], in1=cosT[:, sl], op=ALU.mult),
                   reads=[dps[0], dcos], writes=[dtA])
                op("dve", lambda e: e.tensor_tensor(out=tB[:, :], in0=pss[1], in1=sinT[:, sl], op=ALU.mult),
                   reads=[dps[1], dsin], writes=[dtB])
                op("pool", lambda e: e.tensor_tensor(out=qcs[b][:, sl], in0=tA[:, :], in1=tB[:, :], op=ALU.add),
                   reads=[dtA, dtB], writes=[dqcs[b]])
                if xi == 1:
                    store(g, qcs[b], dqcs[b])
            return cons

        def store_qr(g, t, d):
            for hh in range(2):
                dma("sp", qcat_d[2 * g + hh, 256:320, :], t[hh * 64:(hh + 1) * 64, :], d, reads=[d], writes=[d_qcat])
        lin_fm(wp, [W["w_uq_rope"], W["w_uq_rope_sw"]], 1024, 0, 1024, xchq, pp, rope_cons(cs, dcs, sn, dsn, store_qr))
        dma("sp", cs[:], C["c_cos128T"][:, :], dcs, writes=[dcs])
        dma("sp", sn[:], C["c_sin128T"][:, :], dsn, writes=[dsn])

        def store_qi(g, t, d):
            dma("sp", qidx_d[g, :, :], t[:], d, reads=[d], writes=[d_qidx])
        lin_fm(wp, [W["w_idx_q"], W["w_idx_q_sw"]], 1024, 0, 4096, xchq, pp, rope_cons(cs, dcs, sn, dsn, store_qi))
        fw.barrier()
    if stop_after <= 2:
        fw.barrier()
        return nc, fw, dbg_outs, {}

    def run_interleaved(gens):
        gens = list(gens)
        while gens:
            for g_ in list(gens):
                try:
                    next(g_)
                except StopIteration:
                    gens.remove(g_)

    SC_DSA = float(192 ** -0.5)
    with ExitStack() as st:
        pp = PsPool("pd", 4, st)
        acc = [fw.ps("accd%d" % i, [128, 512], F32, st) for i in range(3)]; dacc = [Dep() for _ in range(3)]
        ptm = fw.ps("ptm", [128, 1024], BF16, st); dptm = Dep()
        Ssc2 = [fw.sb("Ssc%d" % i, [128, S], F32, st) for i in range(2)]
        dS2 = [[Dep() for _ in range(4)] for _ in range(2)]
        Ssb2 = [fw.sb("Ssb%d" % i, [128, S], F32, st) for i in range(2)]
        dSb2 = [[Dep() for _ in range(4)] for _ in range(2)]
        rlp = [fw.sb("rlp%d" % i, [128, 512], F32, st) for i in range(2)]; drlp = [Dep(), Dep()]
        wk = fw.sb("wk", [128, S], F32, st); dwk = Dep()
        qi2 = [fw.sb("qi%d" % i, [128, 32, 128], BF16, st) for i in range(2)]; dqi2 = [Dep(), Dep()]
        qc = fw.sb("qc", [128, 3, 16, 128], BF16, st); dqc = Dep()
        rl = [fw.sb("rl%d" % i, [128, 512], F32, st) for i in range(3)]; drl = [Dep(), Dep(), Dep()]
        m8 = fw.sb("m8", [128, 8], F32, st); dm8 = Dep()
        Mq = fw.sb("Mq", [128, S], BF16, st); dMq = Dep()
        MT = fw.sb("MT", [128, 16, 128], BF16, st); dMT = Dep()
        exs = [fw.sb("exs%d" % i, [128, 512], BF16, st) for i in range(3)]; dexs = [Dep(), Dep(), Dep()]
        PT = [fw.sb("PT%d" % i, [128, 512], BF16, st) for i in range(3)]; dPT = [Dep(), Dep(), Dep()]
        rden = fw.sb("rden", [128, 512], F32, st); drd = Dep()
        ol = fw.sb("ol", [128, 2, 512], BF16, st); dol = Dep()
        obst = fw.sb("obst", [128, 512], BF16, st); dob = Dep()
        wuv = fw.sb("wuv", [128, 16, 2, 128], BF16, st); dwuv = Dep()
        dma("pool", wuv[:], W["w_uv"].rearrange("h (rc r) v -> r h rc v", rc=2), dwuv, writes=[dwuv])
        causq = fw.sb("causq", [128, 128], F32, st); dcq = Dep()
        negm = fw.sb("negm", [128, 128], F32, st); dng = Dep()
        dma("sp", causq[:], C["c_causq"][:, :], dcq, writes=[dcq])
        op("dve", lambda e: e.tensor_scalar(out=negm[:], in0=causq[:], scalar1=-1.0, scalar2=1e30, op0=ALU.add, op1=ALU.mult),
           reads=[dcq], writes=[dng])
        cnts = {"rl": 0, "ex": 0}

        def chunks_of(i):
            nb_ = i + 1
            out_ = []
            for base, off in ((0, 0), (NQ, 128 * nb_)):
                for c0 in range(0, 128 * nb_, 512):
                    n = min(512, 128 * nb_ - c0)
                    out_.append((base + c0, n, off + c0))
            return out_

        def stage1(i):
            sb_ = i % 2
            Ssc, dSc, qi, dqi = Ssc2[sb_], dS2[sb_], qi2[sb_], dqi2[sb_]
            Ssb, dSbc = Ssb2[sb_], dSb2[sb_]
            dma("sp", qi[:], qidx_d[:, :, i * 128:(i + 1) * 128].rearrange("h d q -> d h q"), dqi, reads=[d_qidx], writes=[dqi])
            chunks = chunks_of(i)
            for h in range(32):
                for ci, (t0, n, off) in enumerate(chunks):
                    ps, dp = pp.next()
                    op("pe", lambda e: e.matmul(ps[:, 0:n], lhsT=qi[:, h, :], rhs=kidxT[:, t0:t0 + n], start=True, stop=True),
                       reads=[dqi, d_kidxT], writes=[dp])
                    b = cnts["rl"] % 3
                    cnts["rl"] += 1
                    op("act", lambda e: e.activation(out=rl[b][:, 0:n], in_=ps[:, 0:n], func=AF.Relu), reads=[dp], writes=[drl[b]])
                    if h == 0:
                        op("dve", lambda e: e.tensor_scalar(out=Ssc[:, off:off + n], in0=rl[b][:, 0:n], scalar1=wi_sb[:, i, 0:1],
                                                            scalar2=None, op0=ALU.mult), reads=[drl[b], d_wi], writes=[dSc[ci]])
                    elif h == 1:
                        op("pool", lambda e: e.tensor_scalar(out=Ssb[:, off:off + n], in0=rl[b][:, 0:n], scalar1=wi_sb[:, i, 1:2],
                                                             scalar2=0.0, op0=ALU.mult, op1=ALU.add), reads=[drl[b], d_wi], writes=[dSbc[ci]])
                    elif h % 2 == 1:
                        bp = cnts["rl"] % 2
                        op("pool", lambda e: e.tensor_scalar(out=rlp[bp][:, 0:n], in0=rl[b][:, 0:n], scalar1=wi_sb[:, i, h:h + 1],
                                                             scalar2=0.0, op0=ALU.mult, op1=ALU.add), reads=[drl[b], d_wi], writes=[drlp[bp]])
                        op("pool", lambda e: e.tensor_tensor(out=Ssb[:, off:off + n], in0=Ssb[:, off:off + n], in1=rlp[bp][:, 0:n], op=ALU.add),
                           reads=[drlp[bp], dSbc[ci]], writes=[dSbc[ci]])
                    else:
                        op("dve", lambda e: e.scalar_tensor_tensor(out=Ssc[:, off:off + n], in0=rl[b][:, 0:n],
                                                                   scalar=wi_sb[:, i, h:h + 1], in1=Ssc[:, off:off + n],
                                                                   op0=ALU.mult, op1=ALU.add), reads=[drl[b], d_wi, dSc[ci]], writes=[dSc[ci]])
                    yield

        def stage23(i):
            sb_ = i % 2
            Ssc, dSc = Ssc2[sb_], dS2[sb_]
            nb_ = i + 1
            L = 256 * nb_
            chunks = chunks_of(i)
            dSall = dSc[0:len(chunks)]
            for ch in range(2):
                dma("sp", qc[:, ch, :, :], qcat_d[:, ch * 128:(ch + 1) * 128, i * 128:(i + 1) * 128].rearrange("h c q -> c h q"),
                    dqc, reads=[d_qcat], writes=[dqc])
            dma("sp", qc[0:64, 2, :, :], qcat_d[:, 256:320, i * 128:(i + 1) * 128].rearrange("h c q -> c h q"),
                dqc, reads=[d_qcat], writes=[dqc])
            op("dve", lambda e: e.tensor_tensor(out=Ssc[:, 0:L], in0=Ssc[:, 0:L], in1=Ssb2[sb_][:, 0:L], op=ALU.add),
               reads=dSall + dSb2[sb_][0:len(chunks)], writes=dSall)
            dsl = slice(128 * i, 128 * (i + 1))
            op("dve", lambda e: e.tensor_tensor(out=Ssc[:, dsl], in0=Ssc[:, dsl], in1=causq[:], op=ALU.mult), reads=dSall + [dcq], writes=dSall)
            op("dve", lambda e: e.tensor_tensor(out=Ssc[:, dsl], in0=Ssc[:, dsl], in1=negm[:], op=ALU.add), reads=dSall + [dng], writes=dSall)
            osl = slice(128 * nb_ + 128 * i, 128 * nb_ + 128 * (i + 1))
            op("dve", lambda e: e.tensor_scalar(out=Ssc[:, osl], in0=Ssc[:, osl], scalar1=visnb[:, i, 0:1], scalar2=visnb[:, i, 2:3],
                                                op0=ALU.mult, op1=ALU.add), reads=dSall + [d_vis], writes=dSall)
            yield
            for r in range(32):
                src = Ssc if r == 0 else wk
                dsrc = dSall if r == 0 else [dwk]
                op("dve", lambda e: e.max(out=m8[:], in_=src[:, 0:L]), reads=dsrc, writes=[dm8])
                yield
                if r < 31:
                    op("dve", lambda e: e.match_replace(out=wk[:, 0:L], in_to_replace=m8[:], in_values=src[:, 0:L], imm_value=-3e38),
                       reads=dsrc + [dm8], writes=[dwk])
                    yield
            op("dve", lambda e: e.tensor_scalar(out=m8[:, 7:8], in0=m8[:, 7:8], scalar1=-1e29, scalar2=None, op0=ALU.max),
               reads=[dm8], writes=[dm8])
            op("dve", lambda e: e.tensor_scalar(out=Mq[:, 0:L], in0=Ssc[:, 0:L], scalar1=m8[:, 7:8], scalar2=None, op0=ALU.is_ge),
               reads=dSall + [dm8], writes=[dMq])
            yield
            nkb = 2 * nb_
            for k0 in range(0, nkb, 8):
                kn = min(8, nkb - k0)
                for j in range(kn):
                    op("pe", lambda e: e.transpose(ptm[:, j * 128:(j + 1) * 128], Mq[:, (k0 + j) * 128:(k0 + j + 1) * 128], ident_bf[:]),
                       reads=[dMq, d_idb], writes=[dptm], inc=(j == kn - 1))
                evac(MT[:, k0:k0 + kn, :], ptm[:, 0:kn * 128].rearrange("p (a b) -> p a b", a=kn), [dptm], [dMT])
                yield
            blks = list(range(nb_)) + [8 + m for m in range(nb_)]
            steps = [(hg, kb, bl) for hg in range(4) for kb, bl in enumerate(blks)]

            def front(stp):
                hg, kb, bl = stp
                t0 = bl * 128
                ps, dp = pp.next()
                for ch in range(3):
                    kp = 64 if ch == 2 else 128
                    op("pe", lambda e: e.matmul(ps[:, :], lhsT=kvcT[0:kp, ch, t0:t0 + 128],
                                                rhs=qc[0:kp, ch, hg * 4:(hg + 1) * 4, :].rearrange("p a b -> p (a b)"),
                                                start=(ch == 0), stop=(ch == 2)),
                       reads=[d_kvcT, dqc] if ch == 0 else (), writes=[dp], inc=(ch == 2))
                b = cnts["ex"] % 3
                cnts["ex"] += 1
                op("act", lambda e: e.activation(out=exs[b][:], in_=ps[:, :], func=AF.Exp, scale=SC_DSA), reads=[dp], writes=[dexs[b]])
                for hh in range(4):
                    eng = "pool"
                    op(eng, lambda e: e.tensor_tensor(out=PT[b][:, hh * 128:(hh + 1) * 128], in0=exs[b][:, hh * 128:(hh + 1) * 128],
                                                      in1=MT[:, kb, :], op=ALU.mult), reads=[dexs[b], dMT], writes=[dPT[b]])
                return b

            deferred = []

            def tail_pe(hg):
                ps, dp = pp.next()
                for hh in range(4):
                    h = hg * 4 + hh
                    for rc in range(2):
                        op("pe", lambda e: e.matmul(ps[:, hh * 128:(hh + 1) * 128], lhsT=wuv[:, h, rc, :], rhs=ol[:, rc, hh * 128:(hh + 1) * 128],
                                                    start=(rc == 0), stop=(rc == 1)),
                           reads=[dwuv, dol], writes=[dp], inc=(hh == 3 and rc == 1))
                evac(obst[:], ps[:, :], [dp], [dob])
                dma("sp", obT_d[hg * 4:(hg + 1) * 4, :, i * 128:(i + 1) * 128].rearrange("h v q -> v h q"),
                    obst[:].rearrange("p (a b) -> p a b", a=4), dob, reads=[dob], writes=[d_obT])

            def back(stp, b):
                hg, kb, bl = stp
                for a in range(3):
                    lh = ones_bf[:] if a == 2 else kvlat[:, bl, a * 128:(a + 1) * 128]
                    op("pe", lambda e: e.matmul(acc[a][:, :], lhsT=lh, rhs=PT[b][:], start=(kb == 0), stop=(kb == len(blks) - 1)),
                       reads=[dPT[b], d_kvlat, d_onb], writes=[dacc[a]], inc=(kb == len(blks) - 1))
                if kb == len(blks) - 1:
                    op("act", lambda e: e.activation(out=rden[:], in_=acc[2][:, :], func=AF.Ln), reads=[dacc[2]], writes=[drd])
                    op("act", lambda e: e.activation(out=rden[:], in_=rden[:], func=AF.Exp, scale=-1.0), reads=[drd], writes=[drd])
                    for a in range(2):
                        op("dve", lambda e: e.tensor_tensor(out=ol[:, a, :], in0=acc[a][:, :], in1=rden[:], op=ALU.mult),
                           reads=[dacc[a], drd], writes=[dol])
                    deferred.append(hg)

            pend = None
            for stp in steps:
                b = front(stp)
                if pend is not None:
                    back(*pend)
                    if deferred and pend[0][0] != deferred[0]:
                        pass
                pend = (stp, b)
                while deferred and (deferred[0] != stp[0]):
                    tail_pe(deferred.pop(0))
                yield
            back(*pend)
            while deferred:
                tail_pe(deferred.pop(0))
            yield

        run_interleaved([stage1(0)])
        for i in range(8):
            gens = [stage23(i)]
            if i < 7:
                gens.append(stage1(i + 1))
            run_interleaved(gens)
        fw.barrier()
    if stop_after <= 3:
        fw.barrier()
        return nc, fw, dbg_outs, {}

    SC_FOX = float(128 ** -0.5)
    with ExitStack() as st:
        pp = PsPool("pe", 4, st, shape=(128, 128))
        ao = [fw.ps("ao%d" % i, [128, 128], F32, st) for i in range(2)]; dao = [Dep(), Dep()]
        ad = [fw.ps("ad%d" % i, [128, 128], F32, st) for i in range(2)]; dad = [Dep(), Dep()]
        Bias = fw.sb("Bias", [128, 8, 16, 16], F32, st); dB = Dep()
        for i in range(8):
            for bl in range(16):
                op("dve" if bl % 2 else "pool", lambda e: e.tensor_tensor(out=Bias[:, i, bl, :], in0=negck[:, bl, :], in1=Rref[:, i, :],
                                                                          op=ALU.subtract), reads=[d_negck, d_Rref], writes=[dB])
            op("dve", lambda e: e.tensor_scalar(out=Bias[:, i, 8 + i, :], in0=Bias[:, i, 8 + i, :], scalar1=visnb[:, i, 1:2], scalar2=None,
                                                op0=ALU.add), reads=[dB, d_vis], writes=[dB])
        kh = [fw.sb("kh%d" % i, [128, S], BF16, st) for i in range(2)]; dkh = [Dep(), Dep()]
        qh = [fw.sb("qh%d" % i, [128, NQ], BF16, st) for i in range(2)]; dqh = [Dep(), Dep()]
        vh = [fw.sb("vh%d" % i, [128, 16, 128], BF16, st) for i in range(2)]; dvh = [Dep(), Dep()]
        fost = [fw.sb("fost%d" % i, [128, NQ], BF16, st) for i in range(2)]; dfo = [Dep(), Dep()]
        ex = [[fw.sb("ex%d_%d" % (p_, i), [128, 128], BF16, st) for i in range(3)] for p_ in range(2)]
        dex = [[Dep(), Dep(), Dep()] for p_ in range(2)]
        rdn = [fw.sb("rdn%d" % i, [128, 128], F32, st) for i in range(2)]; drdn = [Dep(), Dep()]

        cnt_e = [0, 0]

        def fox_thread(hb):
            for h in range(hb, 16, 2):
                dma("sp", kh[hb][:], kT_d[h, :, :], dkh[hb], reads=[d_kT], writes=[dkh[hb]])
                dma("sp", qh[hb][:], qT_d[h, :, :], dqh[hb], reads=[d_qT], writes=[dqh[hb]])
                dma("sp", vh[hb][:], v_d[:, h * 128:(h + 1) * 128].rearrange("(b k) d -> k b d", k=128), dvh[hb], reads=[d_v], writes=[dvh[hb]])
                steps = [(i, kb, bl, len(range(i + 1)) * 2) for i in range(8) for kb, bl in enumerate(list(range(i + 1)) + [8 + m for m in range(i + 1)])]

                def front(stp):
                    i, kb, bl, nk = stp
                    ps, dp = pp.next()
                    op("pe", lambda e: e.matmul(ps[:, :], lhsT=kh[hb][:, bl * 128:(bl + 1) * 128], rhs=qh[hb][:, i * 128:(i + 1) * 128],
                                                start=True, stop=True), reads=[dkh[hb], dqh[hb]], writes=[dp])
                    b = cnt_e[hb] % 3
                    cnt_e[hb] += 1
                    op("act", lambda e: e.activation(out=ex[hb][b][:], in_=ps[:, :], func=AF.Exp, scale=SC_FOX, bias=Bias[:, i, bl, h:h + 1]),
                       reads=[dp, dB], writes=[dex[hb][b]])
                    if bl == i:
                        op("dve", lambda e: e.tensor_tensor(out=ex[hb][b][:], in0=ex[hb][b][:], in1=tri_bf[:], op=ALU.mult),
                           reads=[dex[hb][b], d_tri], writes=[dex[hb][b]])
                    return b

                def back(stp, b):
                    i, kb, bl, nk = stp
                    last = (kb == nk - 1)
                    op("pe", lambda e: e.matmul(ao[hb][:, :], lhsT=vh[hb][:, bl, :], rhs=ex[hb][b][:], start=(kb == 0), stop=last),
                       reads=[dvh[hb], dex[hb][b]], writes=[dao[hb]], inc=last)
                    op("pe", lambda e: e.matmul(ad[hb][:, :], lhsT=ones_bf[:], rhs=ex[hb][b][:], start=(kb == 0), stop=last),
                       reads=[d_onb, dex[hb][b]], writes=[dad[hb]], inc=last)
                    if last:
                        op("dve", lambda e: e.reciprocal(out=rdn[hb][:], in_=ad[hb][:, :]), reads=[dad[hb]], writes=[drdn[hb]])
                        op("dve", lambda e: e.tensor_tensor(out=fost[hb][:, i * 128:(i + 1) * 128], in0=ao[hb][:, :], in1=rdn[hb][:], op=ALU.mult),
                           reads=[dao[hb], drdn[hb]], writes=[dfo[hb]])

                pend = None
                for stp in steps:
                    b = front(stp)
                    if pend is not None:
                        back(*pend)
                    pend = (stp, b)
                    yield
                back(*pend)
                yield
                dma("sp", foT_d[h, :, :], fost[hb][:], dfo[hb], reads=[dfo[hb]], writes=[d_foT])

        run_interleaved([fox_thread(0), fox_thread(1)])
        fw.barrier()
    MS.close()
    if stop_after <= 4:
        fw.barrier()
        return nc, fw, dbg_outs, {}

    with ExitStack() as st:
        pp = PsPool("pf", 6, st)
        wp = WPool("wf", st, 3)
        foT = fw.sb("foT", [128, 16, NQ], BF16, st); dfoT = Dep()
        obT = fw.sb("obT", [128, 16, NQ], BF16, st); dobT = Dep()
        mg = fw.sb("mg", [128, 32, NQ], BF16, st); dmg = [Dep() for _ in range(32)]
        dma("sp", foT[:], foT_d.rearrange("h d q -> d h q"), dfoT, reads=[d_foT], writes=[dfoT])
        dma("sp", obT[:], obT_d.rearrange("h d q -> d h q"), dobT, reads=[d_obT], writes=[dobT])
        gt = [fw.sb("gt%d" % i, [128, NQ], BF16, st) for i in range(4)]; dgt = [Dep() for _ in range(4)]

        def gload(gi):
            dma("sp", gt[gi % 4][:], gT_d[gi, :, :], dgt[gi % 4], reads=[d_gT], writes=[dgt[gi % 4]])
        gload(0)
        gload(1)
        tmpb = fw.sb("tmpb", [128, 512], BF16, st); dtb = Dep()
        xfo = [(lambda c: foT[:, c, 0:512], [dfoT], 512), (lambda c: foT[:, c, 512:1024], [dfoT], 512)]
        xob = [(lambda c: obT[:, c, 0:512], [dobT], 512), (lambda c: obT[:, c, 512:1024], [dobT], 512)]

        def cons_ya(col, gw, xi, pss, dps):
            g = col // 128
            b = g % 4
            if xi == 0:
                gload(g + 2)
            op("dve", lambda e: e.tensor_tensor(out=mg[:, g, xi * 512:(xi + 1) * 512], in0=pss[0], in1=gt[b][:, xi * 512:(xi + 1) * 512],
                                                op=ALU.mult), reads=[dps[0], dgt[b]], writes=[dmg[g]])
        lin_fm(wp, [W["w_up_a"]], 2048, 0, D, xfo, pp, cons_ya)

        def cons_yb(col, gw, xi, pss, dps):
            g = col // 128
            b = (32 + g) % 4
            if xi == 0 and 32 + g + 2 < 64:
                gload(32 + g + 2)
            op("dve", lambda e: e.tensor_tensor(out=tmpb[:], in0=pss[0], in1=gt[b][:, xi * 512:(xi + 1) * 512], op=ALU.mult),
               reads=[dps[0], dgt[b]], writes=[dtb])
            op("pool", lambda e: e.tensor_tensor(out=mg[:, g, xi * 512:(xi + 1) * 512], in0=mg[:, g, xi * 512:(xi + 1) * 512], in1=tmpb[:],
                                                 op=ALU.add), reads=[dtb, dmg[g]], writes=[dmg[g]])
        lin_fm(wp, [W["w_up_b"]], 2048, 0, D, xob, pp, cons_yb)
        xt = [fw.sb("xt%d" % i, [128, 256], F32, st) for i in range(2)]; dxt = [Dep(), Dep()]
        cn = [0]

        def cons_o(lo, n, t, ps, dp):
            b = cn[0] % 2
            cn[0] += 1
            dma("sp", xt[b][:, 0:n], x_seq[t * 128:(t + 1) * 128, lo:lo + n], dxt[b], writes=[dxt[b]])
            op("dve", lambda e: e.tensor_tensor(out=xt[b][:, 0:n], in0=ps, in1=xt[b][:, 0:n], op=ALU.add), reads=[dp, dxt[b]], writes=[dxt[b]])
            dma("sp", x1_d[t * 128:(t + 1) * 128, lo:lo + n], xt[b][:, 0:n], dxt[b], reads=[dxt[b]], writes=[d_x1])
        lin_tm(wp, W["w_out"], D, 0, D, lambda c, t: mg[:, c, t * 128:(t + 1) * 128], lambda t: dmg, 8, pp, cons_o)
        fw.barrier()
    if stop_after <= 5:
        fw.barrier()
        return nc, fw, dbg_outs, {}

    SC_MEM = float(128 ** -0.5)
    with ExitStack() as st:
        memT = fw.sb("memT", [128, 32, 256], BF16, st); dmemT = [Dep(), Dep()]
        hxT = fw.sb("hxT", [128, 32, NQ], BF16, st); dhx = [Dep() for _ in range(8)]
        with ExitStack() as st2:
            norm_T("nM", st2, mem_in, 256, W["g_mem"], lambda t: (memT[:, :, t * 128:(t + 1) * 128], dmemT[t]))
            fw.barrier()
        with ExitStack() as st2:
            norm_T("nX", st2, x1_d, NQ, W["g_norm_mem_x"], lambda t: (hxT[:, :, t * 128:(t + 1) * 128], dhx[t]), src_deps=[d_x1])
            fw.barrier()
        pp = PsPool("pg", 4, st)
        wp = WPool("wg", st, 3)
        kmT = fw.sb("kmT", [128, 4, 256], BF16, st); dkm = Dep(multi=True)
        vm = fw.sb("vm", [128, 2, 512], BF16, st); dvm = Dep(multi=True)
        qmT = fw.sb("qmT", [128, 4, NQ], BF16, st); dqm = Dep(multi=True)
        omT = fw.sb("omT", [128, 4, NQ], BF16, st); dom = Dep(multi=True)
        lin_fm(wp, [W["w_km"]], D, 0, 512, [(lambda c: memT[:, c, :], dmemT, 256)], pp,
               lambda col, gw, xi, pss, dps: evac(kmT[:, col // 128, :], pss[0], [dps[0]], [dkm]))
        lin_tm(wp, W["w_vm"], D, 0, 512, lambda c, t: memT[:, c, t * 128:(t + 1) * 128], lambda t: [dmemT[t]], 2, pp,
               lambda lo, n, t, ps, dp: evac(vm[:, t, lo:lo + n], ps, [dp], [dvm]))
        xhx = [(lambda c: hxT[:, c, 0:512], dhx[0:4], 512), (lambda c: hxT[:, c, 512:1024], dhx[4:8], 512)]
        lin_fm(wp, [W["w_qm"]], D, 0, 512, xhx, pp,
               lambda col, gw, xi, pss, dps: evac(qmT[:, col // 128, xi * 512:(xi + 1) * 512], pss[0], [dps[0]], [dqm]))
        accm = [fw.ps("accm%d" % i, [128, 512], F32, st) for i in range(2)]; daccm = [Dep(), Dep()]
        exm = [fw.sb("exm%d" % i, [128, 512], BF16, st) for i in range(2)]; dexm = [Dep(), Dep()]
        rdm = fw.sb("rdm", [128, 512], F32, st); drdm = Dep()
        nex = 0
        for h in range(4):
            for tc in range(2):
                for mb in range(2):
                    ps, dp = pp.next()
                    op("pe", lambda e: e.matmul(ps[:, :], lhsT=kmT[:, h, mb * 128:(mb + 1) * 128], rhs=qmT[:, h, tc * 512:(tc + 1) * 512],
                                                start=True, stop=True), reads=[dkm, dqm], writes=[dp])
                    b = nex % 2
                    nex += 1
                    op("act", lambda e: e.activation(out=exm[b][:], in_=ps[:, :], func=AF.Exp, scale=SC_MEM), reads=[dp], writes=[dexm[b]])
                    op("pe", lambda e: e.matmul(accm[0][:, :], lhsT=vm[:, mb, h * 128:(h + 1) * 128], rhs=exm[b][:], start=(mb == 0), stop=(mb == 1)),
                       reads=[dvm, dexm[b]], writes=[daccm[0]], inc=(mb == 1))
                    op("pe", lambda e: e.matmul(accm[1][:, :], lhsT=ones_bf[:], rhs=exm[b][:], start=(mb == 0), stop=(mb == 1)),
                       reads=[d_onb, dexm[b]], writes=[daccm[1]], inc=(mb == 1))
                op("dve", lambda e: e.reciprocal(out=rdm[:], in_=accm[1][:, :]), reads=[daccm[1]], writes=[drdm])
                op("dve", lambda e: e.tensor_tensor(out=omT[:, h, tc * 512:(tc + 1) * 512], in0=accm[0][:, :], in1=rdm[:], op=ALU.mult),
                   reads=[daccm[0], drdm], writes=[dom])
        xt = [fw.sb("xtg%d" % i, [128, 256], F32, st) for i in range(2)]; dxt = [Dep(), Dep()]
        cn = [0]

        def cons_om(lo, n, t, ps, dp):
            b = cn[0] % 2
            cn[0] += 1
            dma("sp", xt[b][:, 0:n], x1_d[t * 128:(t + 1) * 128, lo:lo + n], dxt[b], reads=[d_x1], writes=[dxt[b]])
            op("dve", lambda e: e.tensor_tensor(out=xt[b][:, 0:n], in0=ps, in1=xt[b][:, 0:n], op=ALU.add), reads=[dp, dxt[b]], writes=[dxt[b]])
            dma("sp", x2_d[t * 128:(t + 1) * 128, lo:lo + n], xt[b][:, 0:n], dxt[b], reads=[dxt[b]], writes=[d_x2])
        lin_tm(wp, W["w_om"], 512, 0, D, lambda c, t: omT[:, c, t * 128:(t + 1) * 128], lambda t: [dom], 8, pp, cons_om)
        fw.barrier()
    if stop_after <= 6:
        fw.barrier()
        return nc, fw, dbg_outs, {}

    with ExitStack() as st:
        hf = fw.sb("hf", [128, 8, D], BF16, st); dhf = [Dep() for _ in range(8)]
        Sel = fw.sb("Sel", [128, 8, 8, CAP], BF16, st); dSel = Dep(multi=True)
        RWg = fw.sb("RWg", [128, 8, 2, 8], F32, st); dRWg = Dep(multi=True)
        oh = fw.sb("oh", [128, 8, 8], F32, st); doh = Dep(multi=True)
        RW = fw.sb("RW", [128, 8, 64], F32, st); dRW = Dep(multi=True)
        pp = PsPool("ph", 4, st)
        ptb = fw.ps("ptb", [128, 1024], BF16, st); dptb = Dep()
        with ExitStack() as s2:
            gbc = fw.sb("hgbc", [128, D], F32, s2); dg = Dep()
            dma("sp", gbc[:], W["g_norm_ffn"].partition_broadcast(128), dg, writes=[dg])
            xin = fw.sb("hxin", [128, D], F32, s2); dx = Dep()
            hT32 = fw.sb("hT32", [128, 32, 128], F32, s2); dh32 = Dep()
            wr = fw.sb("wr", [128, 32, 72], F32, s2); dwr = Dep()
            dma("sp", wr[:], W["w_r"].rearrange("(c p) n -> p c n", p=128), dwr, writes=[dwr])
            brb = fw.sb("brb", [128, 72], F32, s2); dbr = Dep()
            dma("sp", brb[:], W["b_r"].partition_broadcast(128), dbr, writes=[dbr])
            lg = fw.sb("lg", [128, 72], F32, s2); dlg = Dep()
            sm = fw.sb("sm", [128, 16], F32, s2); dsm = Dep()
            e8 = fw.sb("e8", [128, 8], F32, s2); de8 = Dep()
            les = fw.sb("les", [128, 8], F32, s2); dles = Dep()
            m8 = fw.sb("hm8", [128, 8], F32, s2); dm8 = Dep()
            o12 = fw.sb("o12", [128, 2, 8], F32, s2); do12 = Dep()
            rwl = fw.sb("rwl", [128, 8], F32, s2); drwl = Dep()
            plg = fw.ps("plg", [128, 72], F32, s2); dplg = Dep()
            ptr = [fw.ps("ptr%d" % i, [128, 512], F32, s2) for i in range(2)]; dptr = [Dep(), Dep()]
            ntr = 0
            for m in range(8):
                dma("sp", xin[:], x2_d[m * 128:(m + 1) * 128, :], dx, reads=[d_x2], writes=[dx])
                op("act", lambda e: e.activation(out=hf[:, m, :], in_=xin[:], func=AF.Square, accum_out=sm[:, 0:1]),
                   reads=[dx], writes=[dhf[m], dsm])
                op("dve", lambda e: e.tensor_scalar(out=sm[:, 1:2], in0=sm[:, 0:1], scalar1=1.0 / D, scalar2=EPS, op0=ALU.mult, op1=ALU.add),
                   reads=[dsm], writes=[dsm])
                op("act", lambda e: e.activation(out=sm[:, 1:2], in_=sm[:, 1:2], func=AF.Sqrt), reads=[dsm], writes=[dsm])
                op("dve", lambda e: e.reciprocal(out=sm[:, 1:2], in_=sm[:, 1:2]), reads=[dsm], writes=[dsm])
                op("dve", lambda e: e.scalar_tensor_tensor(out=xin[:], in0=xin[:], scalar=sm[:, 1:2], in1=gbc[:], op0=ALU.mult, op1=ALU.mult),
                   reads=[dx, dsm, dg], writes=[dx])
                op("act", lambda e: e.activation(out=hf[:, m, :], in_=xin[:], func=AF.Copy), reads=[dx], writes=[dhf[m]])
                for q in range(8):
                    b = ntr % 2
                    ntr += 1
                    for j in range(4):
                        c = q * 4 + j
                        op("pe", lambda e: e.transpose(ptr[b][:, j * 128:(j + 1) * 128], xin[:, c * 128:(c + 1) * 128], ident_f[:]),
                           reads=[dx, d_idf], writes=[dptr[b]], inc=(j == 3))
                    evac(hT32[:, q * 4:(q + 1) * 4, :], ptr[b][:, :].rearrange("p (a b) -> p a b", a=4), [dptr[b]], [dh32])
                for c in range(32):
                    op("pe", lambda e: e.matmul(plg[:, :], lhsT=hT32[:, c, :], rhs=wr[:, c, :], start=(c == 0), stop=(c == 31)),
                       reads=[dh32, dwr], writes=[dplg], inc=(c == 31))
                op("dve", lambda e: e.tensor_tensor(out=lg[:], in0=plg[:, :], in1=brb[:], op=ALU.add), reads=[dplg, dbr], writes=[dlg])
                op("dve", lambda e: e.tensor_reduce(out=sm[:, 2:3], in_=lg[:, 0:8], axis=AX.X, op=ALU.max), reads=[dlg], writes=[dsm])
                op("dve", lambda e: e.tensor_scalar(out=sm[:, 3:4], in0=sm[:, 2:3], scalar1=-1.0, scalar2=None, op0=ALU.mult), reads=[dsm], writes=[dsm])
                op("act", lambda e: e.activation(out=e8[:], in_=lg[:, 0:8], func=AF.Exp, bias=sm[:, 3:4], scale=1.0, accum_out=sm[:, 4:5]),
                   reads=[dlg, dsm], writes=[de8, dsm])
                op("dve", lambda e: e.reciprocal(out=sm[:, 5:6], in_=sm[:, 4:5]), reads=[dsm], writes=[dsm])
                op("dve", lambda e: e.tensor_scalar(out=oh[:, m, :], in0=lg[:, 0:8], scalar1=sm[:, 2:3], scalar2=None, op0=ALU.is_equal),
                   reads=[dlg, dsm], writes=[doh])
                op("dve", lambda e: e.tensor_scalar(out=les[:], in0=lg[:, 8:16], scalar1=oh[:, m, 0:1], scalar2=None, op0=ALU.mult),
                   reads=[dlg, doh], writes=[dles])
                for g in range(1, 8):
                    op("dve", lambda e: e.scalar_tensor_tensor(out=les[:], in0=lg[:, 8 + g * 8:16 + g * 8], scalar=oh[:, m, g:g + 1], in1=les[:],
                                                               op0=ALU.mult, op1=ALU.add), reads=[dlg, doh, dles], writes=[dles])
                op("dve", lambda e: e.max(out=m8[:], in_=les[:]), reads=[dles], writes=[dm8])
                op("dve", lambda e: e.tensor_tensor(out=sm[:, 6:7], in0=m8[:, 1:2], in1=m8[:, 0:1], op=ALU.subtract), reads=[dm8], writes=[dsm])
                op("act", lambda e: e.activation(out=sm[:, 7:8], in_=sm[:, 6:7], func=AF.Exp), reads=[dsm], writes=[dsm])
                op("dve", lambda e: e.tensor_scalar(out=sm[:, 7:8], in0=sm[:, 7:8], scalar1=1.0, scalar2=None, op0=ALU.add), reads=[dsm], writes=[dsm])
                op("dve", lambda e: e.reciprocal(out=sm[:, 8:9], in_=sm[:, 7:8]), reads=[dsm], writes=[dsm])
                op("dve", lambda e: e.tensor_tensor(out=sm[:, 9:10], in0=sm[:, 8:9], in1=sm[:, 5:6], op=ALU.mult), reads=[dsm], writes=[dsm])
                op("dve", lambda e: e.tensor_tensor(out=sm[:, 10:11], in0=sm[:, 5:6], in1=sm[:, 9:10], op=ALU.subtract), reads=[dsm], writes=[dsm])
                op("dve", lambda e: e.tensor_scalar(out=o12[:, 0, :], in0=les[:], scalar1=m8[:, 0:1], scalar2=sm[:, 9:10], op0=ALU.is_equal, op1=ALU.mult),
                   reads=[dles, dm8, dsm], writes=[do12])
                op("dve", lambda e: e.tensor_scalar(out=o12[:, 1, :], in0=les[:], scalar1=m8[:, 1:2], scalar2=sm[:, 10:11], op0=ALU.is_equal, op1=ALU.mult),
                   reads=[dles, dm8, dsm], writes=[do12])
                op("dve", lambda e: e.tensor_tensor(out=rwl[:], in0=o12[:, 0, :], in1=o12[:, 1, :], op=ALU.add), reads=[do12], writes=[drwl])
                for g in range(8):
                    op("dve", lambda e: e.tensor_scalar(out=RW[:, m, g * 8:(g + 1) * 8], in0=rwl[:], scalar1=oh[:, m, g:g + 1], scalar2=None, op0=ALU.mult),
                       reads=[drwl, doh], writes=[dRW])
            tril, dtril = fw.sb("tril", [128, 128], BF16, s2), Dep()
            dma("sp", tril[:], C["c_tril_bf"][:, :], dtril, writes=[dtril])
            iota, diota = fw.sb("iota", [128, CAP], F32, s2), Dep()
            dma("sp", iota[:], C["c_iota"][:, :], diota, writes=[diota])
            ohb = fw.sb("ohb", [128, 8, 8], BF16, s2); dohb = Dep()
            op("dve", lambda e: e.tensor_copy(out=ohb[:], in_=oh[:]), reads=[doh], writes=[dohb])
            RWh = fw.sb("RWh", [128, 8, 64], BF16, s2); dRWh = Dep()
            RWl = fw.sb("RWl", [128, 8, 64], BF16, s2); dRWl = Dep()
            op("dve", lambda e: e.tensor_copy(out=RWh[:], in_=RW[:]), reads=[dRW], writes=[dRWh])
            op("dve", lambda e: e.tensor_tensor(out=RWl[:], in0=RW[:], in1=RWh[:], op=ALU.subtract), reads=[dRW, dRWh], writes=[dRWl])
            t8 = fw.sb("t8", [128, 8], F32, s2); dt8 = Dep()
            posv = fw.sb("posv", [128, 1], F32, s2); dpos = Dep()
            pcn = plg; dpcn = dplg
            for m in range(8):
                for m2 in range(m + 1):
                    op("pe", lambda e: e.matmul(pcn[:, 0:8], lhsT=(ones_bf[:] if m2 < m else tril[:]), rhs=ohb[:, m2, :], start=(m2 == 0), stop=(m2 == m)),
                       reads=[d_onb, dtril, dohb], writes=[dpcn], inc=(m2 == m))
                op("dve", lambda e: e.tensor_tensor(out=t8[:], in0=pcn[:, 0:8], in1=oh[:, m, :], op=ALU.mult), reads=[dpcn, doh], writes=[dt8])
                op("dve", lambda e: e.tensor_reduce(out=posv[:], in_=t8[:], axis=AX.X, op=ALU.add), reads=[dt8], writes=[dpos])
                for g in range(8):
                    op("dve" if g % 2 else "pool", lambda e: e.tensor_scalar(out=Sel[:, m, g, :], in0=iota[:], scalar1=posv[:, 0:1], scalar2=oh[:, m, g:g + 1],
                                                                             op0=ALU.is_equal, op1=ALU.mult), reads=[diota, dpos, doh], writes=[dSel])
            for g in range(8):
                for sc in range(2):
                    ps, dp = pp.next()
                    for m in range(8):
                        for hl, (Rt, dRt) in enumerate(((RWh, dRWh), (RWl, dRWl))):
                            op("pe", lambda e: e.matmul(ps[:, 0:8], lhsT=Sel[:, m, g, sc * 128:(sc + 1) * 128], rhs=Rt[:, m, g * 8:(g + 1) * 8],
                                                        start=(m == 0 and hl == 0), stop=(m == 7 and hl == 1)),
                               reads=[dSel, dRt], writes=[dp], inc=(m == 7 and hl == 1))
                    evac(RWg[:, g, sc, :], ps[:, 0:8], [dp], [dRWg], eng="dve")
            fw.barrier()
        with ExitStack() as s2:
            wp = WPool("wh", s2, 3)
            xgT = fw.sb("xgT", [128, 32, CAP], BF16, s2); dxg = Dep()
            actT = fw.sb("actT", [128, 8, 4, CAP], BF16, s2); daT = Dep(multi=True)
            sg = fw.sb("sg", [128, 256], F32, s2); dsg = Dep()
            asb = [fw.sb("asb%d" % i, [128, 512], BF16, s2) for i in range(2)]; dasb = [Dep(), Dep()]
            ygst = [fw.sb("ygst%d" % i, [128, 256], F32, s2) for i in range(2)]; dyg = [Dep(), Dep()]
            ny = 0
            for g in range(8):
                for c in range(32):
                    ps, dp = pp.next()
                    for m in range(8):
                        op("pe", lambda e: e.matmul(ps[:, 0:CAP], lhsT=hf[:, m, c * 128:(c + 1) * 128], rhs=Sel[:, m, g, :], start=(m == 0), stop=(m == 7)),
                           reads=[dhf[m], dSel] if c == 0 else (), writes=[dp], inc=(m == 7))
                    evac(xgT[:, c, :], ps[:, 0:CAP], [dp], [dxg])
                for e8i in range(8):
                    ex_ = g * 8 + e8i
                    for fh in range(2):
                        wg, dwg = wp.load(W["w_gate"][ex_], D, fh * 256, 256, 256)
                        wu, dwu = wp.load(W["w_up"][ex_], D, fh * 256, 256, 256)
                        for s_ in range(2):
                            psg, dpg = pp.next()
                            for c in range(32):
                                op("pe", lambda e: e.matmul(psg[:, 0:256], lhsT=xgT[:, c, s_ * 128:(s_ + 1) * 128], rhs=wg[:, c, :], start=(c == 0), stop=(c == 31)),
                                   reads=[dxg, dwg] if c == 0 else (), writes=[dpg], inc=(c == 31))
                            psu, dpu = pp.next()
                            for c in range(32):
                                op("pe", lambda e: e.matmul(psu[:, 0:256], lhsT=xgT[:, c, s_ * 128:(s_ + 1) * 128], rhs=wu[:, c, :], start=(c == 0), stop=(c == 31)),
                                   reads=[dxg, dwu] if c == 0 else (), writes=[dpu], inc=(c == 31))
                            op("act", lambda e: e.activation(out=sg[:], in_=psg[:, 0:256], func=AF.Silu), reads=[dpg], writes=[dsg])
                            op("dve", lambda e: e.scalar_tensor_tensor(out=asb[s_][:, fh * 256:(fh + 1) * 256], in0=psu[:, 0:256],
                                                                       scalar=RWg[:, g, s_, e8i:e8i + 1], in1=sg[:], op0=ALU.mult, op1=ALU.mult),
                               reads=[dpu, dRWg, dsg], writes=[dasb[s_]])
                    for s_ in range(2):
                        for fc in range(4):
                            op("pe", lambda e: e.transpose(ptb[:, fc * 128:(fc + 1) * 128], asb[s_][:, fc * 128:(fc + 1) * 128], ident_bf[:]),
                               reads=[dasb[s_], d_idb], writes=[dptb], inc=(fc == 3))
                        evac(actT[:, e8i, :, s_ * 128:(s_ + 1) * 128], ptb[:, 0:512].rearrange("p (a b) -> p a b", a=4), [dptb], [daT])
                wdsrc = W["w_down"][g * 8:(g + 1) * 8].rearrange("e f n -> (e f) n")
                for ct in range(16):
                    wd, dwd = wp.load(wdsrc, D, ct * 256, 256, 256)
                    for s_ in range(2):
                        ps, dp = pp.next()
                        for kc in range(32):
                            op("pe", lambda e: e.matmul(ps[:, 0:256], lhsT=actT[:, kc // 4, kc % 4, s_ * 128:(s_ + 1) * 128], rhs=wd[:, kc, :],
                                                        start=(kc == 0), stop=(kc == 31)),
                               reads=[daT, dwd] if kc == 0 else (), writes=[dp], inc=(kc == 31))
                        b = ny % 2
                        ny += 1
                        evac(ygst[b][:], ps[:, 0:256], [dp], [dyg[b]])
                        dma("sp", yg_d[g, s_ * 128:(s_ + 1) * 128, ct * 256:(ct + 1) * 256], ygst[b][:], dyg[b], reads=[dyg[b]], writes=[d_yg])
            fw.barrier()
        with ExitStack() as s2:
            SelT = fw.sb("SelT", [128, 8, 2, NQ], BF16, s2); dST = Dep(multi=True)
            for g in range(8):
                for sc in range(2):
                    for m in range(8):
                        op("pe", lambda e: e.transpose(ptb[:, m * 128:(m + 1) * 128], Sel[:, m, g, sc * 128:(sc + 1) * 128], ident_bf[:]),
                           reads=[dSel, d_idb], writes=[dptb], inc=(m == 7))
                    evac(SelT[:, g, sc, :], ptb[:, :], [dptb], [dST])
            ygf = fw.sb("ygf", [128, 16, 512], F32, s2); dygf = Dep()
            yh = fw.sb("yh", [128, 16, 512], BF16, s2); dyh = Dep()
            yl = fw.sb("yl", [128, 16, 512], BF16, s2); dyl = Dep()
            xz = [fw.sb("xz%d" % i, [128, 512], F32, s2) for i in range(2)]; dxz = [Dep(), Dep()]
            nz = 0
            for ct in range(8):
                dma("sp", ygf[:], yg_d[:, :, ct * 512:(ct + 1) * 512].rearrange("g (sc s) n -> s (g sc) n", sc=2), dygf, reads=[d_yg], writes=[dygf])
                op("act", lambda e: e.activation(out=yh[:], in_=ygf[:], func=AF.Copy), reads=[dygf], writes=[dyh])
                op("dve", lambda e: e.tensor_tensor(out=yl[:], in0=ygf[:], in1=yh[:], op=ALU.subtract), reads=[dygf, dyh], writes=[dyl])
                for m in range(8):
                    ps, dp = pp.next()
                    k = 0
                    for gs in range(16):
                        for (Yt, dY) in ((yh, dyh), (yl, dyl)):
                            op("pe", lambda e: e.matmul(ps[:, :], lhsT=SelT[:, gs // 2, gs % 2, m * 128:(m + 1) * 128], rhs=Yt[:, gs, :],
                                                        start=(k == 0), stop=(k == 31)), reads=[dST, dY], writes=[dp], inc=(k == 31))
                            k += 1
                    b = nz % 2
                    nz += 1
                    dma("sp", xz[b][:], x2_d[m * 128:(m + 1) * 128, ct * 512:(ct + 1) * 512], dxz[b], reads=[d_x2], writes=[dxz[b]])
                    op("dve", lambda e: e.tensor_tensor(out=xz[b][:], in0=ps[:, :], in1=xz[b][:], op=ALU.add), reads=[dp, dxz[b]], writes=[dxz[b]])
                    dma("sp", x1_d[m * 128:(m + 1) * 128, ct * 512:(ct + 1) * 512], xz[b][:], dxz[b], reads=[dxz[b]], writes=[d_x1])
            fw.barrier()
    with ExitStack() as st:
        gbc = fw.sb("fgbc", [128, D], F32, st); dg = Dep()
        dma("sp", gbc[:], W["g_final"].partition_broadcast(128), dg, writes=[dg])
        zin = [fw.sb("zin%d" % i, [128, D], F32, st) for i in range(2)]; dz = [Dep(), Dep()]
        zo = [fw.sb("zo%d" % i, [128, D], F32, st) for i in range(2)]; dzo = [Dep(), Dep()]
        sm = [fw.sb("fsm%d" % i, [128, 2], F32, st) for i in range(2)]; dsm = [Dep(), Dep()]
        for m in range(8):
            b = m % 2
            dma("sp", zin[b][:], x1_d[m * 128:(m + 1) * 128, :], dz[b], reads=[d_x1], writes=[dz[b]])
            op("act", lambda e: e.activation(out=zo[b][:], in_=zin[b][:], func=AF.Square, accum_out=sm[b][:, 0:1]), reads=[dz[b]], writes=[dzo[b], dsm[b]])
            op("dve", lambda e: e.tensor_scalar(out=sm[b][:, 1:2], in0=sm[b][:, 0:1], scalar1=1.0 / D, scalar2=EPS, op0=ALU.mult, op1=ALU.add),
               reads=[dsm[b]], writes=[dsm[b]])
            op("act", lambda e: e.activation(out=sm[b][:, 1:2], in_=sm[b][:, 1:2], func=AF.Sqrt), reads=[dsm[b]], writes=[dsm[b]])
            op("dve", lambda e: e.reciprocal(out=sm[b][:, 1:2], in_=sm[b][:, 1:2]), reads=[dsm[b]], writes=[dsm[b]])
            op("dve", lambda e: e.scalar_tensor_tensor(out=zo[b][:], in0=zin[b][:], scalar=sm[b][:, 1:2], in1=gbc[:], op0=ALU.mult, op1=ALU.mult),
               reads=[dz[b], dsm[b], dg], writes=[dzo[b]])
            dma("sp", out_d[m * 128:(m + 1) * 128, :], zo[b][:], dzo[b], reads=[dzo[b]], writes=[d_out])
        fw.barrier()
    return nc, fw, dbg_outs, {}


_PROG = {}


def kernel(**inputs):
    hw = host_weights(inputs)
    x = np.asarray(inputs["x"], np.float32)
    mem = np.asarray(inputs["mem"], np.float32)
    if "nc" not in _PROG:
        _PROG["nc"] = build_program()[0]
    nc = _PROG["nc"]
    csts = [host_consts(0), host_consts(1)]
    in_maps = []
    for core in range(8):
        b, par = core // 2, core % 2
        perm = OWN[par] + OWN[1 - par]
        xb = np.ascontiguousarray(x[b].reshape(16, 128, D)[perm].reshape(S, D))
        m = {"x_seq": xb, "mem_b": np.ascontiguousarray(mem[b])}
        m.update(hw)
        m.update(csts[par])
        in_maps.append(m)
    res = run_bass_kernel_spmd(nc, in_maps, core_ids=list(range(8)))
    out = np.empty((4, S, D), np.float32)
    for core in range(8):
        b, par = core // 2, core % 2
        o = np.asarray(res.results[core]["out"], np.float32).reshape(8, 128, D)
        for i, blk in enumerate(OWN[par]):
            out[b, blk * 128:(blk + 1) * 128, :] = o[i]
    return out
```

```python
import numpy as np
import ml_dtypes
from contextlib import ExitStack
import concourse.bass as bass
import concourse.mybir as mybir
from concourse.bass_utils import run_bass_kernel_spmd

F32 = mybir.dt.float32
BF16 = mybir.dt.bfloat16
AF = mybir.ActivationFunctionType
ALU = mybir.AluOpType
AX = mybir.AxisListType

D = 4096
S = 2048
NQ = 1024
EPS = 1e-6
NBLK = 16
OWN = ([0, 3, 4, 7, 8, 11, 12, 15], [1, 2, 5, 6, 9, 10, 13, 14])
IN_SPLITS = (2048, 2048, 2048, 16, 1024, 256, 64, 128, 32, 4096, 4096)
OFF = np.concatenate([[0], np.cumsum(IN_SPLITS)]).tolist()
O_QA, O_KA, O_VA, O_FA, O_CQ, O_CKV, O_KR, O_IK, O_IW, O_GA, O_GB = OFF[:11]
CAP = 256


class Dep:
    __slots__ = ("w", "r", "dsem", "dval", "multi")

    def __init__(self, multi=False):
        self.multi = multi
        self.w = {}
        self.r = {}
        self.dsem = None
        self.dval = 0


class EngS:
    def __init__(self, name, eng, sem):
        self.name, self.eng, self.sem = name, eng, sem
        self.cnt = 0
        self.waited = {}
        self.pend = []
        self.ninst = 0


class FW:
    def __init__(self, nc):
        self.nc = nc
        self.es = ExitStack()
        self.E = {}
        for name, eng in (("pe", nc.tensor), ("act", nc.scalar), ("dve", nc.vector),
                          ("pool", nc.gpsimd), ("sp", nc.sync)):
            sem = self.es.enter_context(nc.semaphore("s_" + name))
            self.E[name] = EngS(name, eng, sem)
        self.dma_deps = []
        self.free_sems = []

    def sb(self, name, shape, dtype, st=None):
        self.uid = getattr(self, "uid", 0) + 1
        return (st or self.es).enter_context(self.nc.sbuf_tensor("%s_u%d" % (name, self.uid), list(shape), dtype))

    def ps(self, name, shape, dtype=F32, st=None):
        self.uid = getattr(self, "uid", 0) + 1
        return (st or self.es).enter_context(self.nc.psum_tensor("%s_u%d" % (name, self.uid), list(shape), dtype))

    def _wait(self, e, sem, val):
        if val <= 0:
            return
        k = id(sem)
        if e.waited.get(k, (None, 0))[1] >= val:
            return
        e.eng.wait_ge(sem, val)
        e.waited[k] = (sem, val)
        e.ninst += 1

    def _pre(self, e, reads, writes):
        for d in reads:
            for k, (sem, val) in d.w.items():
                self._wait(e, sem, val)
        for d in writes:
            if not d.multi:
                for k, (sem, val) in d.w.items():
                    if sem is e.sem:
                        continue
                    self._wait(e, sem, val)
            for k, (sem, val) in d.r.items():
                if sem is e.sem:
                    continue
                self._wait(e, sem, val)

    def op(self, ename, fn, reads=(), writes=(), inc=True):
        e = self.E[ename]
        self._pre(e, reads, writes)
        ins = fn(e.eng)
        e.ninst += 1
        if inc:
            e.cnt += 1
            ins.then_inc(e.sem, 1)
            k = id(e.sem)
            for d in list(reads) + e.pend:
                if d.r.get(k, (None, 0))[1] < e.cnt:
                    d.r[k] = (e.sem, e.cnt)
            e.pend = []
            for d in writes:
                if d.multi:
                    d.w[k] = (e.sem, e.cnt)
                else:
                    d.w = {k: (e.sem, e.cnt)}
                    d.r = {}
        else:
            e.pend.extend(reads)
        return ins

    def dma(self, q, out, in_, sbd, reads=(), writes=(), **kw):
        e = self.E[q]
        if sbd.dsem is None:
            if self.free_sems:
                sbd.dsem, sbd.dval = self.free_sems.pop()
            else:
                self.nsem = getattr(self, "nsem", 0) + 1
                sbd.dsem = self.es.enter_context(self.nc.semaphore("d%d" % self.nsem))
                sbd.dval = 0
            self.dma_deps.append(sbd)
        sem = sbd.dsem
        k = id(sem)
        for d in reads:
            for kk, (s, v) in d.w.items():
                self._wait(e, s, v)
        for d in writes:
            if not d.multi:
                for kk, (s, v) in list(d.w.items()):
                    if s is sem:
                        continue
                    self._wait(e, s, v)
            for kk, (s, v) in list(d.r.items()):
                self._wait(e, s, v)
        ins = e.eng.dma_start(out=out, in_=in_, **kw)
        e.ninst += 1
        sbd.dval += 16
        ins.then_inc(sem, 16)
        val = sbd.dval
        for d in reads:
            if d.r.get(k, (None, 0))[1] < val:
                d.r[k] = (sem, val)
        for d in writes:
            if d.multi or set(d.w.keys()) <= {k}:
                d.w[k] = (sem, val)
            else:
                d.w = {k: (sem, val)}
            if not d.multi:
                d.r = {}
        return ins

    def barrier(self):
        names = ["pe", "act", "dve", "pool", "sp"]
        for n in names:
            e = self.E[n]
            for m in names:
                o = self.E[m]
                if o is not e:
                    self._wait(e, o.sem, o.cnt)
            for d in self.dma_deps:
                self._wait(e, d.dsem, d.dval)
        for d in self.dma_deps:
            self.free_sems.append((d.dsem, d.dval))
            d.dsem = None
        self.dma_deps = []


def _rope_tab(pos, d):
    inv = np.power(np.float32(10000.0), -np.arange(0, d, 2, dtype=np.float32) / np.float32(d)).astype(np.float32)
    ang = pos.astype(np.float32)[:, None] * inv[None, :]
    c, s = np.cos(ang).astype(np.float32), np.sin(ang).astype(np.float32)
    return np.concatenate([c, c], 1), np.concatenate([-s, s], 1)


def host_consts(par):
    own = OWN[par]
    oth = OWN[1 - par]
    perm = own + oth
    pos = (np.array(perm)[:, None] * 128 + np.arange(128)[None, :]).reshape(-1)
    c64, s64 = _rope_tab(pos, 64)
    c128, s128 = _rope_tab(pos, 128)
    prec = np.zeros((16, 16, 16), np.float32)
    for b1 in range(16):
        for b2 in range(16):
            prec[:, b1, b2] = 1.0 if perm[b2] < perm[b1] else 0.0
    visnb = np.zeros((128, 8, 4), np.float32)
    for i in range(8):
        v = 1.0 if oth[i] < own[i] else 0.0
        visnb[:, i, 0] = v
        visnb[:, i, 1] = -30000.0 * (1 - v)
        visnb[:, i, 2] = -1e30 * (1 - v)
    k = np.arange(128)
    sel63 = np.zeros((128, 128), np.float32)
    sel63[63, :] = 1.0
    return {
        "c_ident_bf": np.eye(128).astype(ml_dtypes.bfloat16),
        "c_ident_f": np.eye(128, dtype=np.float32),
        "c_tri_bf": (k[:, None] <= k[None, :]).astype(ml_dtypes.bfloat16),
        "c_tril_bf": (k[:, None] < k[None, :]).astype(ml_dtypes.bfloat16),
        "c_ones_bf": np.ones((128, 128), ml_dtypes.bfloat16),
        "c_ones_f": np.ones((128, 128), np.float32),
        "c_sel63": sel63,
        "c_cos64T": np.ascontiguousarray(c64.T), "c_sin64T": np.ascontiguousarray(s64.T),
        "c_cos128T": np.ascontiguousarray(c128[:NQ].T), "c_sin128T": np.ascontiguousarray(s128[:NQ].T),
        "c_cos128tm": np.ascontiguousarray(c128.reshape(16, 128, 128).transpose(1, 0, 2)),
        "c_sin128tm": np.ascontiguousarray(s128.reshape(16, 128, 128).transpose(1, 0, 2)),
        "c_prec": np.ascontiguousarray(prec.reshape(16, 256)),
        "c_visnb": visnb,
        "c_iota": np.tile(np.arange(CAP, dtype=np.float32)[None, :], (128, 1)),
        "c_causq": (k[:, None] >= k[None, :]).astype(np.float32),
    }


CONST_SPECS = {
    "c_ident_bf": ([128, 128], BF16), "c_ident_f": ([128, 128], F32), "c_tri_bf": ([128, 128], BF16),
    "c_tril_bf": ([128, 128], BF16), "c_ones_bf": ([128, 128], BF16), "c_ones_f": ([128, 128], F32),
    "c_sel63": ([128, 128], F32), "c_cos64T": ([64, S], F32), "c_sin64T": ([64, S], F32),
    "c_cos128T": ([128, NQ], F32), "c_sin128T": ([128, NQ], F32),
    "c_cos128tm": ([128, 16, 128], F32), "c_sin128tm": ([128, 16, 128], F32),
    "c_prec": ([16, 256], F32), "c_visnb": ([128, 8, 4], F32), "c_iota": ([128, CAP], F32),
    "c_causq": ([128, 128], F32),
}

W_SPECS = {
    "g_norm_mix": [D], "w_in": [D, 15856], "w_kr_sw": [D, 64], "b_f": [16, 1], "g_q_lat": [128, 8], "g_kv_lat": [128, 2],
    "g_idx_k": [128], "b_idx_k": [128], "w_uq_nope": [1024, 2048], "w_uq_rope": [1024, 1024],
    "w_uq_rope_sw": [1024, 1024], "w_idx_q": [1024, 4096], "w_idx_q_sw": [1024, 4096],
    "w_uk": [16, 128, 256], "w_uv": [16, 256, 128], "w_up_a": [2048, D], "w_up_b": [2048, D], "w_out": [D, D],
    "g_norm_mem_x": [D], "g_mem": [D], "w_qm": [D, 512], "w_km": [D, 512], "w_vm": [D, 512], "w_om": [512, D],
    "g_norm_ffn": [D], "w_r": [D, 72], "b_r": [72], "w_gate": [64, D, 512], "w_up": [64, D, 512],
    "w_down": [64, 512, D], "g_final": [D],
}


def _swap_halves(w, nheads, hd):
    k = w.shape[0]
    w3 = w.reshape(k, nheads, hd)
    return np.ascontiguousarray(np.concatenate([w3[:, :, hd // 2:], w3[:, :, :hd // 2]], axis=2).reshape(k, nheads * hd))


def host_weights(inp):
    g = lambda n: np.ascontiguousarray(np.asarray(inp[n], np.float32)[0])
    w_uq = g("w_uq").reshape(1024, 16, 192)
    w_uq_rope = np.ascontiguousarray(w_uq[:, :, 128:].reshape(1024, 1024))
    w_in = g("w_in")
    out = {
        "g_norm_mix": g("g_norm_mix"), "w_in": w_in,
        "w_kr_sw": _swap_halves(np.ascontiguousarray(w_in[:, O_KR:O_KR + 64]), 1, 64),
        "b_f": np.ascontiguousarray(g("b_f").reshape(16, 1)), "g_q_lat": np.ascontiguousarray(g("g_q_lat").reshape(8, 128).T), "g_kv_lat": np.ascontiguousarray(g("g_kv_lat").reshape(2, 128).T), "g_idx_k": g("g_idx_k"),
        "b_idx_k": g("b_idx_k"),
        "w_uq_nope": np.ascontiguousarray(w_uq[:, :, :128].reshape(1024, 2048)),
        "w_uq_rope": w_uq_rope, "w_uq_rope_sw": _swap_halves(w_uq_rope, 16, 64),
        "w_idx_q": g("w_idx_q"), "w_idx_q_sw": _swap_halves(g("w_idx_q"), 32, 128),
        "w_uk": g("w_uk"), "w_uv": g("w_uv"), "w_up_a": g("w_up_a"), "w_up_b": g("w_up_b"), "w_out": g("w_out"),
        "g_norm_mem_x": g("g_norm_mem_x"), "g_mem": g("g_mem"), "w_qm": g("w_qm"), "w_km": g("w_km"),
        "w_vm": g("w_vm"), "w_om": g("w_om"), "g_norm_ffn": g("g_norm_ffn"),
        "w_r": np.ascontiguousarray(np.concatenate([g("w_rg"), g("w_re")], axis=1)),
        "b_r": np.ascontiguousarray(np.concatenate([g("b_rg"), g("b_re")], axis=0)),
        "w_gate": g("w_gate"), "w_up": g("w_up"), "w_down": g("w_down"),
        "g_final": np.ascontiguousarray(np.asarray(inp["g_final"], np.float32)),
    }
    return out


def build_program(stop_after=99, dbg=False):
    nc = bass.Bass("TRN2", target_bir_lowering=False)
    fw = FW(nc)
    op, dma = fw.op, fw.dma

    def din(name, shape, dt=F32):
        return nc.dram_tensor(name, list(shape), dt, kind="ExternalInput").ap()

    dbg_outs = {}

    def dscr(name, shape, dt):
        kind = "ExternalOutput" if dbg else "Internal"
        t = nc.dram_tensor(name, list(shape), dt, kind=kind).ap()
        if dbg:
            dbg_outs[name] = t
        return t

    x_seq = din("x_seq", [S, D])
    mem_in = din("mem_b", [256, D])
    W = {n: din(n, s) for n, s in W_SPECS.items()}
    C = {n: din(n, s, dt) for n, (s, dt) in CONST_SPECS.items()}
    out_d = nc.dram_tensor("out", [NQ, D], F32, kind="ExternalOutput").ap()
    d_out = Dep(multi=True)

    kT_d = dscr("kT_d", [16, 128, S], BF16); d_kT = Dep(multi=True)
    v_d = dscr("v_d", [S, 2048], BF16); d_v = Dep(multi=True)
    qT_d = dscr("qT_d", [16, 128, NQ], BF16); d_qT = Dep(multi=True)
    gT_d = dscr("gT_d", [64, 128, NQ], BF16); d_gT = Dep(multi=True)
    qcat_d = dscr("qcat_d", [16, 320, NQ], BF16); d_qcat = Dep(multi=True)
    qidx_d = dscr("qidx_d", [32, 128, NQ], BF16); d_qidx = Dep(multi=True)
    foT_d = dscr("foT_d", [16, 128, NQ], BF16); d_foT = Dep(multi=True)
    obT_d = dscr("obT_d", [16, 128, NQ], BF16); d_obT = Dep(multi=True)
    x1_d = dscr("x1_d", [NQ, D], F32); d_x1 = Dep(multi=True)
    x2_d = dscr("x2_d", [NQ, D], F32); d_x2 = Dep(multi=True)
    yg_d = dscr("yg_d", [8, CAP, D], F32); d_yg = Dep(multi=True)

    G = fw.es

    def cload(name, shape, dt, src, q="sp"):
        t = fw.sb(name, shape, dt, G)
        d = Dep()
        dma(q, t[:], src, d, writes=[d])
        return t, d

    ident_bf, d_idb = cload("ident_bf", [128, 128], BF16, C["c_ident_bf"][:, :])
    ident_f, d_idf = cload("ident_f", [128, 128], F32, C["c_ident_f"][:, :])
    tri_bf, d_tri = cload("tri_bf", [128, 128], BF16, C["c_tri_bf"][:, :])
    ones_bf, d_onb = cload("ones_bf", [128, 128], BF16, C["c_ones_bf"][:, :])
    ones_f, d_onf = cload("ones_f", [128, 128], F32, C["c_ones_f"][:, :])
    visnb, d_vis = cload("visnb", [128, 8, 4], F32, C["c_visnb"][:, :, :])
    epsc = fw.sb("epsc", [128, 1], F32, G); d_eps = Dep()
    op("dve", lambda e: e.memset(epsc[:], EPS), writes=[d_eps])

    MS = ExitStack()
    G = MS
    kvcT = fw.sb("kvcT", [128, 3, S], BF16, G); d_kvcT = Dep(multi=True)
    kvlat = fw.sb("kvlat", [128, 16, 256], BF16, G); d_kvlat = Dep(multi=True)
    kidxT = fw.sb("kidxT", [128, S], BF16, G); d_kidxT = Dep(multi=True)
    negck = fw.sb("negck", [128, 16, 16], F32, G); d_negck = Dep()
    Rref = fw.sb("Rref", [128, 16, 16], F32, G); d_Rref = Dep()
    cqT = fw.sb("cqT", [128, 8, NQ], BF16, G); d_cqT = Dep(multi=True)
    Lsb = fw.sb("Lsb", [16, S], F32, G); dL = Dep(multi=True)
    wi_sb = fw.sb("wi_sb", [128, 8, 32], F32, G); d_wi = Dep(multi=True)

    def norm_T(tag, st, src, ntok, g_ap, dst_fn, f32_fn=None, src_deps=()):
        gbc = fw.sb(tag + "gbc", [128, D], BF16, st); dg = Dep()
        dma("pool", gbc[:], g_ap.partition_broadcast(128), dg, writes=[dg])
        xin = [fw.sb(tag + "xin%d" % i, [128, D], F32, st) for i in range(2)]
        dx = [Dep(), Dep()]
        xs = [fw.sb(tag + "xs%d" % i, [128, D], BF16, st) for i in range(2)]
        dxs = [Dep(), Dep()]
        ss = [fw.sb(tag + "ss%d" % i, [128, 2], F32, st) for i in range(2)]
        dss = [Dep(), Dep()]
        pst = [fw.ps(tag + "pt%d" % i, [128, 1024], BF16, st) for i in range(2)]
        dpt = [Dep(), Dep()]
        nev = 0
        for t in range(ntok // 128):
            b = t % 2
            dma("sp", xin[b][:], src[t * 128:(t + 1) * 128, :], dx[b], reads=list(src_deps), writes=[dx[b]])
            op("act", lambda e: e.activation(out=xs[b][:], in_=xin[b][:], func=AF.Square, accum_out=ss[b][:, 0:1]),
               reads=[dx[b]], writes=[dxs[b], dss[b]])
            op("dve", lambda e: e.tensor_scalar(out=ss[b][:, 1:2], in0=ss[b][:, 0:1], scalar1=1.0 / D, scalar2=EPS,
                                                op0=ALU.mult, op1=ALU.add), reads=[dss[b]], writes=[dss[b]])
            op("act", lambda e: e.activation(out=ss[b][:, 1:2], in_=ss[b][:, 1:2], func=AF.Sqrt),
               reads=[dss[b]], writes=[dss[b]])
            op("dve", lambda e: e.reciprocal(out=ss[b][:, 1:2], in_=ss[b][:, 1:2]), reads=[dss[b]], writes=[dss[b]])
            if f32_fn is not None:
                f32_fn(t, xin[b], dx[b], ss[b], dss[b], gbc, dg)
            op("dve", lambda e: e.scalar_tensor_tensor(out=xs[b][:], in0=xin[b][:], scalar=ss[b][:, 1:2], in1=gbc[:],
                                                       op0=ALU.mult, op1=ALU.mult),
               reads=[dx[b], dss[b], dg], writes=[dxs[b]])
            dst, ddst = dst_fn(t)
            for q4 in range(4):
                pb = nev % 2
                nev += 1
                for c8 in range(8):
                    c = q4 * 8 + c8
                    op("pe", lambda e: e.transpose(pst[pb][:, c8 * 128:(c8 + 1) * 128], xs[b][:, c * 128:(c + 1) * 128],
                                                   ident_bf[:]),
                       reads=[dxs[b], d_idb], writes=[dpt[pb]], inc=(c8 == 7))
                src_ps = pst[pb][:].rearrange("p (c t) -> p c t", c=8)
                if q4 % 2 == 0:
                    op("act", lambda e: e.activation(out=dst[:, q4 * 8:(q4 + 1) * 8, :], in_=src_ps, func=AF.Copy),
                       reads=[dpt[pb]], writes=[ddst])
                else:
                    op("dve", lambda e: e.tensor_copy(out=dst[:, q4 * 8:(q4 + 1) * 8, :], in_=src_ps),
                       reads=[dpt[pb]], writes=[ddst])

    class PsPool:
        def __init__(self, tag, n, st, shape=(128, 512), dt=F32):
            self.t = [fw.ps("%s%d" % (tag, i), list(shape), dt, st) for i in range(n)]
            self.d = [Dep() for _ in range(n)]
            self.i = 0

        def next(self):
            j = self.i % len(self.t)
            self.i += 1
            return self.t[j], self.d[j]

    class WPool:
        def __init__(self, tag, st, nbuf, nelem=8192):
            self.t = [fw.sb("%s%d" % (tag, i), [128, nelem], BF16, st) for i in range(nbuf)]
            self.d = [Dep() for _ in range(nbuf)]
            self.i = 0
            self.nelem = nelem

        def load(self, Wap, K, lo, n, ct):
            kc = K // 128
            assert kc * ct <= self.nelem
            j = self.i % len(self.t)
            self.i += 1
            v = self.t[j][:, 0:kc * ct].rearrange("p (c n) -> p c n", c=kc)
            dma("pool", v[:, :, 0:n], Wap.rearrange("(c p) n -> p c n", p=128)[:, :, lo:lo + n], self.d[j],
                writes=[self.d[j]])
            return v, self.d[j]

    def lin_fm(wp, Ws, K, col_lo, ncols, xchunks, pspool, consume, CT=256):
        kc = K // 128
        nw = len(Ws)
        ntile = (ncols + CT - 1) // CT
        for ti in range(ntile):
            lo = col_lo + ti * CT
            n = min(CT, col_lo + ncols - lo)
            wv = [wp.load(Ws[j], K, lo, n, CT) for j in range(nw)]
            for g0 in range(0, n, 128):
                gw = min(128, n - g0)
                for xi, (xfn, xdeps, xn) in enumerate(xchunks):
                    pss, dps = [], []
                    for j in range(nw):
                        ps, dp = pspool.next()
                        for c in range(kc):
                            op("pe", lambda e: e.matmul(ps[0:gw, 0:xn], lhsT=wv[j][0][:, c, g0:g0 + gw], rhs=xfn(c),
                                                        start=(c == 0), stop=(c == kc - 1)),
                               reads=([wv[j][1]] + list(xdeps)) if c == 0 else (), writes=[dp], inc=(c == kc - 1))
                        pss.append(ps[0:gw, 0:xn])
                        dps.append(dp)
                    consume(lo + g0, gw, xi, pss, dps)

    def lin_tm(wp, Wap, K, col_lo, ncols, xT_fn, xdeps_fn, ntiles, pspool, consume, CT=256):
        kc = K // 128
        ntile = (ncols + CT - 1) // CT
        for ti in range(ntile):
            lo = col_lo + ti * CT
            n = min(CT, col_lo + ncols - lo)
            wv, dw = wp.load(Wap, K, lo, n, CT)
            for t in range(ntiles):
                ps, dp = pspool.next()
                for c in range(kc):
                    op("pe", lambda e: e.matmul(ps[:, 0:n], lhsT=xT_fn(c, t), rhs=wv[:, c, 0:n],
                                                start=(c == 0), stop=(c == kc - 1)),
                       reads=([dw] + list(xdeps_fn(t))) if c == 0 else (), writes=[dp], inc=(c == kc - 1))
                consume(lo, n, t, ps[:, 0:n], dp)

    evac_rr = [0]

    def evac(out, in_, reads, writes, eng=None):
        if eng is None:
            eng = ("act", "dve")[evac_rr[0] % 2]
            evac_rr[0] += 1
        if eng == "act":
            op("act", lambda e: e.activation(out=out, in_=in_, func=AF.Copy), reads=reads, writes=writes)
        else:
            op("dve", lambda e: e.tensor_copy(out=out, in_=in_), reads=reads, writes=writes)

    hst = ExitStack()
    hT = fw.sb("hT", [128, 32, NQ], BF16, hst)
    dh = [Dep() for _ in range(8)]

    def load_hT(half):
        with ExitStack() as st:
            norm_T("nA%d" % half, st, x_seq[half * NQ:(half + 1) * NQ, :], NQ, W["g_norm_mix"],
                   lambda t: (hT[:, :, t * 128:(t + 1) * 128], dh[t]))
            fw.barrier()

    xch = [(lambda c: hT[:, c, 0:512], dh[0:4], 512), (lambda c: hT[:, c, 512:1024], dh[4:8], 512)]

    def hT_tile(c, t):
        return hT[:, c, t * 128:(t + 1) * 128]

    def kside(half):
        T0 = half * NQ
        with ExitStack() as st:
            pp = PsPool("pk%d" % half, 6, st)
            wp = WPool("wk", st, 3)
            kstage = [fw.sb("kstg%d" % i, [128, NQ], BF16, st) for i in range(2)]
            dks = [Dep(), Dep()]

            def cons_k(col, gw, xi, pss, dps):
                h = (col - O_KA) // 128
                b = h % 2
                evac(kstage[b][:, xi * 512:(xi + 1) * 512], pss[0], [dps[0]], [dks[b]])
                if xi == 1:
                    dma("sp", kT_d[h, :, T0:T0 + NQ], kstage[b][:], dks[b], reads=[dks[b]], writes=[d_kT])
            lin_fm(wp, [W["w_in"]], D, O_KA, 2048, xch, pp, cons_k)

            nbf = fw.sb("nbf", [16, 1], F32, st); dnbf = Dep()
            dma("sp", nbf[:], W["b_f"][:, :], dnbf, writes=[dnbf])
            op("act", lambda e: e.mul(nbf[:], nbf[:], -1.0), reads=[dnbf], writes=[dnbf])

            def cons_f(col, gw, xi, pss, dps):
                sl = Lsb[:, T0 + xi * 512:T0 + (xi + 1) * 512]
                op("act", lambda e: e.activation(out=sl, in_=pss[0], func=AF.Exp, bias=nbf[:, 0:1], scale=-1.0),
                   reads=[dps[0], dnbf], writes=[dL])
                op("act", lambda e: e.activation(out=sl, in_=sl, func=AF.Ln, bias=1.0, scale=1.0), reads=[dL], writes=[dL])
            lin_fm(wp, [W["w_in"]], D, O_FA, 16, xch, pp, cons_f, CT=16)

            xc = fw.sb("xckv", [128, 2, NQ], BF16, st); dxc = Dep(multi=True)
            sq = fw.sb("sqkv", [128, 2, NQ], BF16, st); dsq = Dep(multi=True)

            def cons_ckv(col, gw, xi, pss, dps):
                g = (col - O_CKV) // 128
                op("act", lambda e: e.activation(out=xc[:, g, xi * 512:(xi + 1) * 512], in_=pss[0], func=AF.Copy),
                   reads=[dps[0]], writes=[dxc])
                op("dve", lambda e: e.tensor_tensor(out=sq[:, g, xi * 512:(xi + 1) * 512], in0=pss[0],
                                                    in1=xc[:, g, xi * 512:(xi + 1) * 512], op=ALU.mult),
                   reads=[dps[0], dxc], writes=[dsq])
            lin_fm(wp, [W["w_in"]], D, O_CKV, 256, xch, pp, cons_ckv)
            gkv = fw.sb("gkv", [128, 2], F32, st); dgkv = Dep()
            dma("sp", gkv[:], W["g_kv_lat"][:, :], dgkv, writes=[dgkv])
            rs = fw.sb("rskv", [128, 512], F32, st); drs = Dep()
            for xi in range(2):
                ps, dp = pp.next()
                for g in range(2):
                    op("pe", lambda e: e.matmul(ps[:, :], lhsT=ones_bf[:], rhs=sq[:, g, xi * 512:(xi + 1) * 512],
                                                start=(g == 0), stop=(g == 1)), reads=[d_onb, dsq], writes=[dp], inc=(g == 1))
                op("act", lambda e: e.activation(out=rs[:], in_=ps[:, :], func=AF.Sqrt, scale=1.0 / 256, bias=epsc[:, 0:1]),
                   reads=[dp, d_eps], writes=[drs])
                op("dve", lambda e: e.reciprocal(out=rs[:], in_=rs[:]), reads=[drs], writes=[drs])
                for g in range(2):
                    op("dve", lambda e: e.scalar_tensor_tensor(out=kvcT[:, g, T0 + xi * 512:T0 + (xi + 1) * 512],
                                                               in0=xc[:, g, xi * 512:(xi + 1) * 512], scalar=gkv[:, g:g + 1],
                                                               in1=rs[:], op0=ALU.mult, op1=ALU.mult),
                       reads=[dxc, dgkv, drs], writes=[d_kvcT])
            ptk = fw.ps("ptk", [128, 1024], BF16, st); dptk = Dep()
            for bl4 in range(2):
                for j in range(4):
                    bl = half * 8 + bl4 * 4 + j
                    for g in range(2):
                        last = (j == 3 and g == 1)
                        op("pe", lambda e: e.transpose(ptk[:, (j * 2 + g) * 128:(j * 2 + g + 1) * 128],
                                                       kvcT[:, g, bl * 128:(bl + 1) * 128], ident_bf[:]),
                           reads=[d_kvcT, d_idb], writes=[dptk], inc=last)
                b0 = half * 8 + bl4 * 4
                evac(kvlat[:, b0:b0 + 4, :], ptk[:].rearrange("p (a b) -> p a b", a=4), [dptk], [d_kvlat])

            cos64 = fw.sb("cos64", [64, NQ], F32, st); dc64 = Dep()
            sin64 = fw.sb("sin64", [64, NQ], F32, st); ds64 = Dep()
            dma("sp", cos64[:], C["c_cos64T"][:, T0:T0 + NQ], dc64, writes=[dc64])
            dma("sp", sin64[:], C["c_sin64T"][:, T0:T0 + NQ], ds64, writes=[ds64])
            tA = fw.sb("tA", [64, 512], F32, st); dtA = Dep()
            tB = fw.sb("tB", [64, 512], F32, st); dtB = Dep()

            def cons_kr(col, gw, xi, pss, dps):
                op("dve", lambda e: e.tensor_tensor(out=tA[:, :], in0=pss[0], in1=cos64[:, xi * 512:(xi + 1) * 512],
                                                    op=ALU.mult), reads=[dps[0], dc64], writes=[dtA])
                op("dve", lambda e: e.tensor_tensor(out=tB[:, :], in0=pss[1], in1=sin64[:, xi * 512:(xi + 1) * 512],
                                                    op=ALU.mult), reads=[dps[1], ds64], writes=[dtB])
                op("pool", lambda e: e.tensor_tensor(out=kvcT[0:64, 2, T0 + xi * 512:T0 + (xi + 1) * 512], in0=tA[:, :],
                                                     in1=tB[:, :], op=ALU.add), reads=[dtA, dtB], writes=[d_kvcT])
            lin_fm(wp, [W["w_in"][:, O_KR:O_KR + 64], W["w_kr_sw"]], D, 0, 64, xch, pp, cons_kr, CT=64)

            vstage = [fw.sb("vstg%d" % i, [128, 256], BF16, st) for i in range(2)]
            dvs = [Dep(), Dep()]
            vcnt = [0]

            def cons_v(lo, n, t, ps, dp):
                b = vcnt[0] % 2
                vcnt[0] += 1
                evac(vstage[b][:, 0:n], ps, [dp], [dvs[b]])
                dma("sp", v_d[T0 + t * 128:T0 + (t + 1) * 128, lo - O_VA:lo - O_VA + n], vstage[b][:, 0:n], dvs[b],
                    reads=[dvs[b]], writes=[d_v])
            lin_tm(wp, W["w_in"], D, O_VA, 2048, hT_tile, lambda t: [dh[t]], 8, pp, cons_v, CT=256)

            gi = fw.sb("gi", [128, 128], F32, st); dgi = Dep()
            bi = fw.sb("bi", [128, 128], F32, st); dbi = Dep()
            dma("sp", gi[:], W["g_idx_k"].partition_broadcast(128), dgi, writes=[dgi])
            dma("sp", bi[:], W["b_idx_k"].partition_broadcast(128), dbi, writes=[dbi])
            c128 = fw.sb("c128tm", [128, 8, 128], F32, st); dc128 = Dep()
            s128 = fw.sb("s128tm", [128, 8, 128], F32, st); ds128 = Dep()
            dma("sp", c128[:], C["c_cos128tm"][:, half * 8:(half + 1) * 8, :], dc128, writes=[dc128])
            dma("sp", s128[:], C["c_sin128tm"][:, half * 8:(half + 1) * 8, :], ds128, writes=[ds128])
            bst = fw.sb("bst", [128, 8], F32, st); dbst = Dep()
            xk = fw.sb("xk", [128, 128], F32, st); dxk = Dep()
            xr = fw.sb("xr", [128, 128], F32, st); dxr = Dep()
            xo = fw.sb("xo", [128, 128], BF16, st); dxo = Dep()
            ptx = fw.ps("ptx", [128, 128], BF16, st); dptx = Dep()

            def cons_ik(lo, n, t, ps, dp):
                op("dve", lambda e: e.bn_stats(out=bst[:, 0:6], in_=ps), reads=[dp], writes=[dbst])
                op("dve", lambda e: e.bn_aggr(out=bst[:, 6:8], in_=bst[:, 0:6]), reads=[dbst], writes=[dbst])
                op("act", lambda e: e.activation(out=bst[:, 7:8], in_=bst[:, 7:8], func=AF.Sqrt, bias=epsc[:, 0:1], scale=1.0),
                   reads=[dbst, d_eps], writes=[dbst])
                op("dve", lambda e: e.reciprocal(out=bst[:, 7:8], in_=bst[:, 7:8]), reads=[dbst], writes=[dbst])
                op("dve", lambda e: e.tensor_scalar(out=xk[:], in0=ps, scalar1=bst[:, 6:7], scalar2=bst[:, 7:8],
                                                    op0=ALU.subtract, op1=ALU.mult), reads=[dp, dbst], writes=[dxk])
                op("dve", lambda e: e.tensor_tensor(out=xk[:], in0=xk[:], in1=gi[:], op=ALU.mult), reads=[dxk, dgi], writes=[dxk])
                op("dve", lambda e: e.tensor_tensor(out=xk[:], in0=xk[:], in1=bi[:], op=ALU.add), reads=[dxk, dbi], writes=[dxk])
                op("pool", lambda e: e.tensor_tensor(out=xr[:, 0:64], in0=xk[:, 64:128], in1=s128[:, t, 0:64], op=ALU.mult),
                   reads=[dxk, ds128], writes=[dxr])
                op("pool", lambda e: e.tensor_tensor(out=xr[:, 64:128], in0=xk[:, 0:64], in1=s128[:, t, 64:128], op=ALU.mult),
                   reads=[dxk, ds128], writes=[dxr])
                op("dve", lambda e: e.tensor_tensor(out=xk[:], in0=xk[:], in1=c128[:, t, :], op=ALU.mult),
                   reads=[dxk, dc128, dxr], writes=[dxk])
                op("dve", lambda e: e.tensor_tensor(out=xo[:], in0=xk[:], in1=xr[:], op=ALU.add), reads=[dxk, dxr], writes=[dxo])
                op("pe", lambda e: e.transpose(ptx[:], xo[:], ident_bf[:]), reads=[dxo, d_idb], writes=[dptx])
                evac(kidxT[:, T0 + t * 128:T0 + (t + 1) * 128], ptx[:], [dptx], [d_kidxT])
            lin_tm(wp, W["w_in"], D, O_IK, 128, hT_tile, lambda t: [dh[t]], 8, pp, cons_ik, CT=128)
            fw.barrier()

    def qside():
        with ExitStack() as st:
            pp = PsPool("pq", 6, st)
            wp = WPool("wq", st, 3)
            qstage = [fw.sb("qstg%d" % i, [128, NQ], BF16, st) for i in range(2)]
            dqs = [Dep(), Dep()]

            def cons_q(col, gw, xi, pss, dps):
                h = (col - O_QA) // 128
                b = h % 2
                evac(qstage[b][:, xi * 512:(xi + 1) * 512], pss[0], [dps[0]], [dqs[b]])
                if xi == 1:
                    dma("sp", qT_d[h, :, :], qstage[b][:], dqs[b], reads=[dqs[b]], writes=[d_qT])
            lin_fm(wp, [W["w_in"]], D, O_QA, 2048, xch, pp, cons_q)

            def cons_g(col, gw, xi, pss, dps):
                gidx = (col - O_GA) // 128
                b = gidx % 2
                op("act", lambda e: e.activation(out=qstage[b][:, xi * 512:(xi + 1) * 512], in_=pss[0], func=AF.Sigmoid),
                   reads=[dps[0]], writes=[dqs[b]])
                if xi == 1:
                    dma("sp", gT_d[gidx, :, :], qstage[b][:], dqs[b], reads=[dqs[b]], writes=[d_gT])
            lin_fm(wp, [W["w_in"]], D, O_GA, 8192, xch, pp, cons_g)

            def cons_iw(lo, n, t, ps, dp):
                op("act", lambda e: e.activation(out=wi_sb[:, t, :], in_=ps, func=AF.Copy, scale=float(4096 ** -0.5)),
                   reads=[dp], writes=[d_wi])
            lin_tm(wp, W["w_in"], D, O_IW, 32, hT_tile, lambda t: [dh[t]], 8, pp, cons_iw, CT=32)

            gq = fw.sb("gq", [128, 8], F32, st); dgq = Dep()
            dma("sp", gq[:], W["g_q_lat"][:, :], dgq, writes=[dgq])
            xc = fw.sb("xcq", [128, 8, 512], BF16, st); dxc = Dep(multi=True)
            sq = fw.sb("sqq", [128, 8, 512], BF16, st); dsq = Dep(multi=True)
            rs = fw.sb("rsq", [128, 512], F32, st); drs = Dep()
            for xi in range(2):
                def cons_cq(col, gw, xi_, pss, dps):
                    g = (col - O_CQ) // 128
                    op("act", lambda e: e.activation(out=xc[:, g, :], in_=pss[0], func=AF.Copy), reads=[dps[0]], writes=[dxc])
                    op("dve", lambda e: e.tensor_tensor(out=sq[:, g, :], in0=pss[0], in1=xc[:, g, :], op=ALU.mult),
                       reads=[dps[0], dxc], writes=[dsq])
                lin_fm(wp, [W["w_in"]], D, O_CQ, 1024, [xch[xi]], pp, cons_cq)
                ps, dp = pp.next()
                for g in range(8):
                    op("pe", lambda e: e.matmul(ps[:, :], lhsT=ones_bf[:], rhs=sq[:, g, :], start=(g == 0), stop=(g == 7)),
                       reads=[d_onb, dsq], writes=[dp], inc=(g == 7))
                op("act", lambda e: e.activation(out=rs[:], in_=ps[:, :], func=AF.Sqrt, scale=1.0 / 1024, bias=epsc[:, 0:1]),
                   reads=[dp, d_eps], writes=[drs])
                op("dve", lambda e: e.reciprocal(out=rs[:], in_=rs[:]), reads=[drs], writes=[drs])
                for g in range(8):
                    op("dve", lambda e: e.scalar_tensor_tensor(out=cqT[:, g, xi * 512:(xi + 1) * 512], in0=xc[:, g, :],
                                                               scalar=gq[:, g:g + 1], in1=rs[:], op0=ALU.mult, op1=ALU.mult),
                       reads=[dxc, dgq, drs], writes=[d_cqT, dxc, dsq])
            fw.barrier()

    def fox_cum():
        with ExitStack() as st:
            Wc = fw.sb("Wc", [16, 16, 128], F32, st); dWc = Dep()
            onesr = fw.sb("onesr", [16, 128], F32, st); dor = Dep()
            op("dve", lambda e: e.memset(onesr[:], 1.0), writes=[dor])
            for bl in range(16):
                op("dve", lambda e: e.tensor_tensor_scan(out=Wc[:, bl, :], data0=onesr[:], data1=Lsb[:, bl * 128:(bl + 1) * 128],
                                                         initial=0.0, op0=ALU.mult, op1=ALU.add),
                   reads=[dL, dor], writes=[dWc])
            prec_sb = fw.sb("prec_sb", [16, 16, 16], F32, st); dpr = Dep()
            dma("sp", prec_sb[:], C["c_prec"].rearrange("p (a b) -> p a b", a=16), dpr, writes=[dpr])
            tmpP = fw.sb("tmpP", [16, 16, 16], F32, st); dtp = Dep()
            Pfx = fw.sb("Pfx", [16, 16], F32, st); dpf = Dep()
            for b1 in range(16):
                op("dve", lambda e: e.tensor_tensor(out=tmpP[:, b1, :], in0=prec_sb[:, b1, :], in1=Wc[:, :, 127],
                                                    op=ALU.mult), reads=[dpr, dWc], writes=[dtp])
            op("dve", lambda e: e.tensor_reduce(out=Pfx[:], in_=tmpP[:], axis=AX.X, op=ALU.add), reads=[dtp], writes=[dpf])
            for bl in range(16):
                op("dve", lambda e: e.tensor_scalar(out=Wc[:, bl, :], in0=Wc[:, bl, :], scalar1=Pfx[:, bl:bl + 1], scalar2=None,
                                                    op0=ALU.add), reads=[dpf, dWc], writes=[dWc])
            pT = fw.ps("pTck", [128, 256], F32, st); dpT = Dep()
            for bl in range(16):
                op("pe", lambda e: e.transpose(pT[:, bl * 16:(bl + 1) * 16], Wc[:, bl, :], ident_f[0:16, 0:16]),
                   reads=[dWc, d_idf], writes=[dpT], inc=(bl == 15))
            evac(negck[:].rearrange("p a b -> p (a b)"), pT[:], [dpT], [d_negck], eng="dve")
            sel63, dsel = fw.sb("sel63", [128, 128], F32, st), Dep()
            dma("sp", sel63[:], C["c_sel63"][:, :], dsel, writes=[dsel])
            op("pe", lambda e: e.matmul(pT[:], lhsT=sel63[:], rhs=negck[:].rearrange("p a b -> p (a b)"), start=True, stop=True),
               reads=[dsel, d_negck], writes=[dpT])
            evac(Rref[:].rearrange("p a b -> p (a b)"), pT[:], [dpT], [d_Rref], eng="dve")
            fw.barrier()

    load_hT(0)
    kside(0)
    qside()
    load_hT(1)
    kside(1)
    hst.close()
    fox_cum()

    def dump_sb(name, t, shape, dt, dep):
        o = nc.dram_tensor("dbg_" + name, list(shape), dt, kind="ExternalOutput").ap()
        dd = Dep()
        dma("sp", o, t, dep, reads=[dep], writes=[dd])
        dbg_outs["dbg_" + name] = o

    if stop_after <= 1:
        dump_sb("negck", negck[:], [128, 16, 16], F32, d_negck)
        dump_sb("kvcT", kvcT[:], [128, 3, S], BF16, d_kvcT)
        dump_sb("kidxT", kidxT[:], [128, S], BF16, d_kidxT)
        dump_sb("cqT", cqT[:], [128, 8, NQ], BF16, d_cqT)
        fw.barrier()
        return nc, fw, dbg_outs, {}

    xchq = [(lambda c: cqT[:, c, 0:512], [d_cqT], 512), (lambda c: cqT[:, c, 512:1024], [d_cqT], 512)]
    with ExitStack() as st:
        pp = PsPool("pc", 6, st)
        wp = WPool("wc", st, 4, nelem=2048)
        wuk = fw.sb("wuk", [128, 16, 256], BF16, st); dwuk = Dep()
        dma("pool", wuk[:], W["w_uk"].rearrange("h n r -> n h r"), dwuk, writes=[dwuk])
        qn = [fw.sb("qn%d" % i, [128, NQ], BF16, st) for i in range(2)]; dqn = [Dep(), Dep()]
        qcs = [fw.sb("qcs%d" % i, [128, NQ], BF16, st) for i in range(2)]; dqcs = [Dep(), Dep()]
        cnt = [0]

        def cons_qn(col, gw, xi, pss, dps):
            h = col // 128
            b = h % 2
            evac(qn[b][:, xi * 512:(xi + 1) * 512], pss[0], [dps[0]], [dqn[b]])
            if xi == 1:
                for rc in range(2):
                    b2 = cnt[0] % 2
                    cnt[0] += 1
                    for tc in range(2):
                        ps, dp = pp.next()
                        op("pe", lambda e: e.matmul(ps[:, :], lhsT=wuk[:, h, rc * 128:(rc + 1) * 128],
                                                    rhs=qn[b][:, tc * 512:(tc + 1) * 512], start=True, stop=True),
                           reads=[dwuk, dqn[b]], writes=[dp])
                        evac(qcs[b2][:, tc * 512:(tc + 1) * 512], ps[:, :], [dp], [dqcs[b2]])
                    dma("sp", qcat_d[h, rc * 128:(rc + 1) * 128, :], qcs[b2][:], dqcs[b2], reads=[dqcs[b2]], writes=[d_qcat])
        lin_fm(wp, [W["w_uq_nope"]], 1024, 0, 2048, xchq, pp, cons_qn)

        tA = fw.sb("tAc", [128, 512], F32, st); dtA = Dep()
        tB = fw.sb("tBc", [128, 512], F32, st); dtB = Dep()
        cs = fw.sb("cs64x2", [128, NQ], F32, st); dcs = Dep()
        sn = fw.sb("sn64x2", [128, NQ], F32, st); dsn = Dep()
        for hh in range(2):
            dma("sp", cs[hh * 64:(hh + 1) * 64, :], C["c_cos64T"][:, 0:NQ], dcs, writes=[dcs])
            dma("sp", sn[hh * 64:(hh + 1) * 64, :], C["c_sin64T"][:, 0:NQ], dsn, writes=[dsn])

        def rope_cons(cosT, dcos, sinT, dsin, store):
            def cons(col, gw, xi, pss, dps):
                g = col // 128
                b = g % 2
                sl = slice(xi * 512, (xi + 1) * 512)
                op("dve", lambda e: e.tensor_tensor(out=tA[:, :], in0=pss[0], in1=cosT[:, sl], op=ALU.mult),
                   reads=[dps[0], dcos], writes=[dtA])
                op("dve", lambda e: e.tensor_tensor(out=tB[:, :], in0=pss[1], in1=sinT[:, sl], op=ALU.mult),
                   reads=[dps[1], dsin], writes=[dtB])
                op("pool", lambda e: e.tensor_tensor(out=qcs[b][:, sl], in0=tA[:, :], in1=tB[:, :], op=ALU.add),
                   reads=[dtA, dtB], writes=[dqcs[b]])
                if xi == 1:
                    store(g, qcs[b], dqcs[b])
            return cons

        def store_qr(g, t, d):
            for hh in range(2):
                dma("sp", qcat_d[2 * g + hh, 256:320, :], t[hh * 64:(hh + 1) * 64, :], d, reads=[d], writes=[d_qcat])
        lin_fm(wp, [W["w_uq_rope"], W["w_uq_rope_sw"]], 1024, 0, 1024, xchq, pp, rope_cons(cs, dcs, sn, dsn, store_qr))
        dma("sp", cs[:], C["c_cos128T"][:, :], dcs, writes=[dcs])
        dma("sp", sn[:], C["c_sin128T"][:, :], dsn, writes=[dsn])

        def store_qi(g, t, d):
            dma("sp", qidx_d[g, :, :], t[:], d, reads=[d], writes=[d_qidx])
        lin_fm(wp, [W["w_idx_q"], W["w_idx_q_sw"]], 1024, 0, 4096, xchq, pp, rope_cons(cs, dcs, sn, dsn, store_qi))
        fw.barrier()
    if stop_after <= 2:
        fw.barrier()
        return nc, fw, dbg_outs, {}

    def run_interleaved(gens):
        gens = list(gens)
        while gens:
            for g_ in list(gens):
                try:
                    next(g_)
                except StopIteration:
                    gens.remove(g_)

    SC_DSA = float(192 ** -0.5)
    with ExitStack() as st:
        pp = PsPool("pd", 4, st)
        acc = [fw.ps("accd%d" % i, [128, 512], F32, st) for i in range(3)]; dacc = [Dep() for _ in range(3)]
        ptm = fw.ps("ptm", [128, 1024], BF16, st); dptm = Dep()
        Ssc2 = [fw.sb("Ssc%d" % i, [128, S], F32, st) for i in range(2)]
        dS2 = [[Dep() for _ in range(4)] for _ in range(2)]
        wk = fw.sb("wk", [128, S], F32, st); dwk = Dep()
        qi2 = [fw.sb("qi%d" % i, [128, 32, 128], BF16, st) for i in range(2)]; dqi2 = [Dep(), Dep()]
        qc = fw.sb("qc", [128, 3, 16, 128], BF16, st); dqc = Dep()
        rl = [fw.sb("rl%d" % i, [128, 512], F32, st) for i in range(3)]; drl = [Dep(), Dep(), Dep()]
        m8 = fw.sb("m8", [128, 8], F32, st); dm8 = Dep()
        Mq = fw.sb("Mq", [128, S], BF16, st); dMq = Dep()
        MT = fw.sb("MT", [128, 16, 128], BF16, st); dMT = Dep()
        exs = [fw.sb("exs%d" % i, [128, 512], BF16, st) for i in range(3)]; dexs = [Dep(), Dep(), Dep()]
        PT = [fw.sb("PT%d" % i, [128, 512], BF16, st) for i in range(3)]; dPT = [Dep(), Dep(), Dep()]
        rden = fw.sb("rden", [128, 512], F32, st); drd = Dep()
        ol = fw.sb("ol", [128, 2, 512], BF16, st); dol = Dep()
        obst = fw.sb("obst", [128, 512], BF16, st); dob = Dep()
        wuv = fw.sb("wuv", [128, 16, 2, 128], BF16, st); dwuv = Dep()
        dma("pool", wuv[:], W["w_uv"].rearrange("h (rc r) v -> r h rc v", rc=2), dwuv, writes=[dwuv])
        causq = fw.sb("causq", [128, 128], F32, st); dcq = Dep()
        negm = fw.sb("negm", [128, 128], F32, st); dng = Dep()
        dma("sp", causq[:], C["c_causq"][:, :], dcq, writes=[dcq])
        op("dve", lambda e: e.tensor_scalar(out=negm[:], in0=causq[:], scalar1=-1.0, scalar2=1e30, op0=ALU.add, op1=ALU.mult),
           reads=[dcq], writes=[dng])
        cnts = {"rl": 0, "ex": 0}

        def chunks_of(i):
            nb_ = i + 1
            out_ = []
            for base, off in ((0, 0), (NQ, 128 * nb_)):
                for c0 in range(0, 128 * nb_, 512):
                    n = min(512, 128 * nb_ - c0)
                    out_.append((base + c0, n, off + c0))
            return out_

        def stage1(i):
            sb_ = i % 2
            Ssc, dSc, qi, dqi = Ssc2[sb_], dS2[sb_], qi2[sb_], dqi2[sb_]
            dma("sp", qi[:], qidx_d[:, :, i * 128:(i + 1) * 128].rearrange("h d q -> d h q"), dqi, reads=[d_qidx], writes=[dqi])
            chunks = chunks_of(i)
            for h in range(32):
                for ci, (t0, n, off) in enumerate(chunks):
                    ps, dp = pp.next()
                    op("pe", lambda e: e.matmul(ps[:, 0:n], lhsT=qi[:, h, :], rhs=kidxT[:, t0:t0 + n], start=True, stop=True),
                       reads=[dqi, d_kidxT], writes=[dp])
                    b = cnts["rl"] % 3
                    cnts["rl"] += 1
                    op("act", lambda e: e.activation(out=rl[b][:, 0:n], in_=ps[:, 0:n], func=AF.Relu), reads=[dp], writes=[drl[b]])
                    if h == 0:
                        op("dve", lambda e: e.tensor_scalar(out=Ssc[:, off:off + n], in0=rl[b][:, 0:n], scalar1=wi_sb[:, i, 0:1],
                                                            scalar2=None, op0=ALU.mult), reads=[drl[b], d_wi], writes=[dSc[ci]])
                    else:
                        op("dve", lambda e: e.scalar_tensor_tensor(out=Ssc[:, off:off + n], in0=rl[b][:, 0:n],
                                                                   scalar=wi_sb[:, i, h:h + 1], in1=Ssc[:, off:off + n],
                                                                   op0=ALU.mult, op1=ALU.add), reads=[drl[b], d_wi, dSc[ci]], writes=[dSc[ci]])
                    yield

        def stage23(i):
            sb_ = i % 2
            Ssc, dSc = Ssc2[sb_], dS2[sb_]
            nb_ = i + 1
            L = 256 * nb_
            chunks = chunks_of(i)
            dSall = dSc[0:len(chunks)]
            for ch in range(2):
                dma("sp", qc[:, ch, :, :], qcat_d[:, ch * 128:(ch + 1) * 128, i * 128:(i + 1) * 128].rearrange("h c q -> c h q"),
                    dqc, reads=[d_qcat], writes=[dqc])
            dma("sp", qc[0:64, 2, :, :], qcat_d[:, 256:320, i * 128:(i + 1) * 128].rearrange("h c q -> c h q"),
                dqc, reads=[d_qcat], writes=[dqc])
            dsl = slice(128 * i, 128 * (i + 1))
            op("dve", lambda e: e.tensor_tensor(out=Ssc[:, dsl], in0=Ssc[:, dsl], in1=causq[:], op=ALU.mult), reads=dSall + [dcq], writes=dSall)
            op("dve", lambda e: e.tensor_tensor(out=Ssc[:, dsl], in0=Ssc[:, dsl], in1=negm[:], op=ALU.add), reads=dSall + [dng], writes=dSall)
            osl = slice(128 * nb_ + 128 * i, 128 * nb_ + 128 * (i + 1))
            op("dve", lambda e: e.tensor_scalar(out=Ssc[:, osl], in0=Ssc[:, osl], scalar1=visnb[:, i, 0:1], scalar2=visnb[:, i, 2:3],
                                                op0=ALU.mult, op1=ALU.add), reads=dSall + [d_vis], writes=dSall)
            yield
            for r in range(32):
                src = Ssc if r == 0 else wk
                dsrc = dSall if r == 0 else [dwk]
                op("dve", lambda e: e.max(out=m8[:], in_=src[:, 0:L]), reads=dsrc, writes=[dm8])
                yield
                if r < 31:
                    op("dve", lambda e: e.match_replace(out=wk[:, 0:L], in_to_replace=m8[:], in_values=src[:, 0:L], imm_value=-3e38),
                       reads=dsrc + [dm8], writes=[dwk])
                    yield
            op("dve", lambda e: e.tensor_scalar(out=m8[:, 7:8], in0=m8[:, 7:8], scalar1=-1e29, scalar2=None, op0=ALU.max),
               reads=[dm8], writes=[dm8])
            op("dve", lambda e: e.tensor_scalar(out=Mq[:, 0:L], in0=Ssc[:, 0:L], scalar1=m8[:, 7:8], scalar2=None, op0=ALU.is_ge),
               reads=dSall + [dm8], writes=[dMq])
            yield
            nkb = 2 * nb_
            for k0 in range(0, nkb, 8):
                kn = min(8, nkb - k0)
                for j in range(kn):
                    op("pe", lambda e: e.transpose(ptm[:, j * 128:(j + 1) * 128], Mq[:, (k0 + j) * 128:(k0 + j + 1) * 128], ident_bf[:]),
                       reads=[dMq, d_idb], writes=[dptm], inc=(j == kn - 1))
                evac(MT[:, k0:k0 + kn, :], ptm[:, 0:kn * 128].rearrange("p (a b) -> p a b", a=kn), [dptm], [dMT])
                yield
            blks = list(range(nb_)) + [8 + m for m in range(nb_)]
            steps = [(hg, kb, bl) for hg in range(4) for kb, bl in enumerate(blks)]

            def front(stp):
                hg, kb, bl = stp
                t0 = bl * 128
                ps, dp = pp.next()
                for ch in range(3):
                    kp = 64 if ch == 2 else 128
                    op("pe", lambda e: e.matmul(ps[:, :], lhsT=kvcT[0:kp, ch, t0:t0 + 128],
                                                rhs=qc[0:kp, ch, hg * 4:(hg + 1) * 4, :].rearrange("p a b -> p (a b)"),
                                                start=(ch == 0), stop=(ch == 2)),
                       reads=[d_kvcT, dqc] if ch == 0 else (), writes=[dp], inc=(ch == 2))
                b = cnts["ex"] % 3
                cnts["ex"] += 1
                op("act", lambda e: e.activation(out=exs[b][:], in_=ps[:, :], func=AF.Exp, scale=SC_DSA), reads=[dp], writes=[dexs[b]])
                for hh in range(4):
                    eng = "dve" if hh % 2 == 0 else "pool"
                    op(eng, lambda e: e.tensor_tensor(out=PT[b][:, hh * 128:(hh + 1) * 128], in0=exs[b][:, hh * 128:(hh + 1) * 128],
                                                      in1=MT[:, kb, :], op=ALU.mult), reads=[dexs[b], dMT], writes=[dPT[b]])
                return b

            deferred = []

            def tail_pe(hg):
                ps, dp = pp.next()
                for hh in range(4):
                    h = hg * 4 + hh
                    for rc in range(2):
                        op("pe", lambda e: e.matmul(ps[:, hh * 128:(hh + 1) * 128], lhsT=wuv[:, h, rc, :], rhs=ol[:, rc, hh * 128:(hh + 1) * 128],
                                                    start=(rc == 0), stop=(rc == 1)),
                           reads=[dwuv, dol], writes=[dp], inc=(hh == 3 and rc == 1))
                evac(obst[:], ps[:, :], [dp], [dob])
                dma("sp", obT_d[hg * 4:(hg + 1) * 4, :, i * 128:(i + 1) * 128].rearrange("h v q -> v h q"),
                    obst[:].rearrange("p (a b) -> p a b", a=4), dob, reads=[dob], writes=[d_obT])

            def back(stp, b):
                hg, kb, bl = stp
                for a in range(3):
                    lh = ones_bf[:] if a == 2 else kvlat[:, bl, a * 128:(a + 1) * 128]
                    op("pe", lambda e: e.matmul(acc[a][:, :], lhsT=lh, rhs=PT[b][:], start=(kb == 0), stop=(kb == len(blks) - 1)),
                       reads=[dPT[b], d_kvlat, d_onb], writes=[dacc[a]], inc=(kb == len(blks) - 1))
                if kb == len(blks) - 1:
                    op("dve", lambda e: e.reciprocal(out=rden[:], in_=acc[2][:, :]), reads=[dacc[2]], writes=[drd])
                    for a in range(2):
                        op("dve", lambda e: e.tensor_tensor(out=ol[:, a, :], in0=acc[a][:, :], in1=rden[:], op=ALU.mult),
                           reads=[dacc[a], drd], writes=[dol])
                    deferred.append(hg)

            pend = None
            for stp in steps:
                b = front(stp)
                if pend is not None:
                    back(*pend)
                    if deferred and pend[0][0] != deferred[0]:
                        pass
                pend = (stp, b)
                while deferred and (deferred[0] != stp[0]):
                    tail_pe(deferred.pop(0))
                yield
            back(*pend)
            while deferred:
                tail_pe(deferred.pop(0))
            yield

        run_interleaved([stage1(0)])
        for i in range(8):
            gens = [stage23(i)]
            if i < 7:
                gens.append(stage1(i + 1))
            run_interleaved(gens)
        fw.barrier()
    if stop_after <= 3:
        fw.barrier()
        return nc, fw, dbg_outs, {}

    SC_FOX = float(128 ** -0.5)
    with ExitStack() as st:
        pp = PsPool("pe", 4, st, shape=(128, 128))
        ao = [fw.ps("ao%d" % i, [128, 128], F32, st) for i in range(2)]; dao = [Dep(), Dep()]
        ad = [fw.ps("ad%d" % i, [128, 128], F32, st) for i in range(2)]; dad = [Dep(), Dep()]
        Bias = fw.sb("Bias", [128, 8, 16, 16], F32, st); dB = Dep()
        for i in range(8):
            for bl in range(16):
                op("dve" if bl % 2 else "pool", lambda e: e.tensor_tensor(out=Bias[:, i, bl, :], in0=negck[:, bl, :], in1=Rref[:, i, :],
                                                                          op=ALU.subtract), reads=[d_negck, d_Rref], writes=[dB])
            op("dve", lambda e: e.tensor_scalar(out=Bias[:, i, 8 + i, :], in0=Bias[:, i, 8 + i, :], scalar1=visnb[:, i, 1:2], scalar2=None,
                                                op0=ALU.add), reads=[dB, d_vis], writes=[dB])
        kh = [fw.sb("kh%d" % i, [128, S], BF16, st) for i in range(2)]; dkh = [Dep(), Dep()]
        qh = [fw.sb("qh%d" % i, [128, NQ], BF16, st) for i in range(2)]; dqh = [Dep(), Dep()]
        vh = [fw.sb("vh%d" % i, [128, 16, 128], BF16, st) for i in range(2)]; dvh = [Dep(), Dep()]
        fost = [fw.sb("fost%d" % i, [128, NQ], BF16, st) for i in range(2)]; dfo = [Dep(), Dep()]
        ex = [[fw.sb("ex%d_%d" % (p_, i), [128, 128], BF16, st) for i in range(3)] for p_ in range(2)]
        dex = [[Dep(), Dep(), Dep()] for p_ in range(2)]
        rdn = [fw.sb("rdn%d" % i, [128, 128], F32, st) for i in range(2)]; drdn = [Dep(), Dep()]

        cnt_e = [0, 0]

        def fox_thread(hb):
            for h in range(hb, 16, 2):
                dma("sp", kh[hb][:], kT_d[h, :, :], dkh[hb], reads=[d_kT], writes=[dkh[hb]])
                dma("sp", qh[hb][:], qT_d[h, :, :], dqh[hb], reads=[d_qT], writes=[dqh[hb]])
                dma("sp", vh[hb][:], v_d[:, h * 128:(h + 1) * 128].rearrange("(b k) d -> k b d", k=128), dvh[hb], reads=[d_v], writes=[dvh[hb]])
                steps = [(i, kb, bl, len(range(i + 1)) * 2) for i in range(8) for kb, bl in enumerate(list(range(i + 1)) + [8 + m for m in range(i + 1)])]

                def front(stp):
                    i, kb, bl, nk = stp
                    ps, dp = pp.next()
                    op("pe", lambda e: e.matmul(ps[:, :], lhsT=kh[hb][:, bl * 128:(bl + 1) * 128], rhs=qh[hb][:, i * 128:(i + 1) * 128],
                                                start=True, stop=True), reads=[dkh[hb], dqh[hb]], writes=[dp])
                    b = cnt_e[hb] % 3
                    cnt_e[hb] += 1
                    op("act", lambda e: e.activation(out=ex[hb][b][:], in_=ps[:, :], func=AF.Exp, scale=SC_FOX, bias=Bias[:, i, bl, h:h + 1]),
                       reads=[dp, dB], writes=[dex[hb][b]])
                    if bl == i:
                        op("dve", lambda e: e.tensor_tensor(out=ex[hb][b][:], in0=ex[hb][b][:], in1=tri_bf[:], op=ALU.mult),
                           reads=[dex[hb][b], d_tri], writes=[dex[hb][b]])
                    return b

                def back(stp, b):
                    i, kb, bl, nk = stp
                    last = (kb == nk - 1)
                    op("pe", lambda e: e.matmul(ao[hb][:, :], lhsT=vh[hb][:, bl, :], rhs=ex[hb][b][:], start=(kb == 0), stop=last),
                       reads=[dvh[hb], dex[hb][b]], writes=[dao[hb]], inc=last)
                    op("pe", lambda e: e.matmul(ad[hb][:, :], lhsT=ones_bf[:], rhs=ex[hb][b][:], start=(kb == 0), stop=last),
                       reads=[d_onb, dex[hb][b]], writes=[dad[hb]], inc=last)
                    if last:
                        op("dve", lambda e: e.reciprocal(out=rdn[hb][:], in_=ad[hb][:, :]), reads=[dad[hb]], writes=[drdn[hb]])
                        op("dve", lambda e: e.tensor_tensor(out=fost[hb][:, i * 128:(i + 1) * 128], in0=ao[hb][:, :], in1=rdn[hb][:], op=ALU.mult),
                           reads=[dao[hb], drdn[hb]], writes=[dfo[hb]])

                pend = None
                for stp in steps:
                    b = front(stp)
                    if pend is not None:
                        back(*pend)
                    pend = (stp, b)
                    yield
                back(*pend)
                yield
                dma("sp", foT_d[h, :, :], fost[hb][:], dfo[hb], reads=[dfo[hb]], writes=[d_foT])

        run_interleaved([fox_thread(0), fox_thread(1)])
        fw.barrier()
    MS.close()
    if stop_after <= 4:
        fw.barrier()
        return nc, fw, dbg_outs, {}

    with ExitStack() as st:
        pp = PsPool("pf", 6, st)
        wp = WPool("wf", st, 3)
        foT = fw.sb("foT", [128, 16, NQ], BF16, st); dfoT = Dep()
        obT = fw.sb("obT", [128, 16, NQ], BF16, st); dobT = Dep()
        mg = fw.sb("mg", [128, 32, NQ], BF16, st); dmg = [Dep() for _ in range(32)]
        dma("sp", foT[:], foT_d.rearrange("h d q -> d h q"), dfoT, reads=[d_foT], writes=[dfoT])
        dma("sp", obT[:], obT_d.rearrange("h d q -> d h q"), dobT, reads=[d_obT], writes=[dobT])
        gt = [fw.sb("gt%d" % i, [128, NQ], BF16, st) for i in range(4)]; dgt = [Dep() for _ in range(4)]

        def gload(gi):
            dma("sp", gt[gi % 4][:], gT_d[gi, :, :], dgt[gi % 4], reads=[d_gT], writes=[dgt[gi % 4]])
        gload(0)
        gload(1)
        tmpb = fw.sb("tmpb", [128, 512], BF16, st); dtb = Dep()
        xfo = [(lambda c: foT[:, c, 0:512], [dfoT], 512), (lambda c: foT[:, c, 512:1024], [dfoT], 512)]
        xob = [(lambda c: obT[:, c, 0:512], [dobT], 512), (lambda c: obT[:, c, 512:1024], [dobT], 512)]

        def cons_ya(col, gw, xi, pss, dps):
            g = col // 128
            b = g % 4
            if xi == 0:
                gload(g + 2)
            op("dve", lambda e: e.tensor_tensor(out=mg[:, g, xi * 512:(xi + 1) * 512], in0=pss[0], in1=gt[b][:, xi * 512:(xi + 1) * 512],
                                                op=ALU.mult), reads=[dps[0], dgt[b]], writes=[dmg[g]])
        lin_fm(wp, [W["w_up_a"]], 2048, 0, D, xfo, pp, cons_ya)

        def cons_yb(col, gw, xi, pss, dps):
            g = col // 128
            b = (32 + g) % 4
            if xi == 0 and 32 + g + 2 < 64:
                gload(32 + g + 2)
            op("dve", lambda e: e.tensor_tensor(out=tmpb[:], in0=pss[0], in1=gt[b][:, xi * 512:(xi + 1) * 512], op=ALU.mult),
               reads=[dps[0], dgt[b]], writes=[dtb])
            op("pool", lambda e: e.tensor_tensor(out=mg[:, g, xi * 512:(xi + 1) * 512], in0=mg[:, g, xi * 512:(xi + 1) * 512], in1=tmpb[:],
                                                 op=ALU.add), reads=[dtb, dmg[g]], writes=[dmg[g]])
        lin_fm(wp, [W["w_up_b"]], 2048, 0, D, xob, pp, cons_yb)
        xt = [fw.sb("xt%d" % i, [128, 256], F32, st) for i in range(4)]; dxt = [Dep() for _ in range(4)]
        cn = [0]

        def cons_o(lo, n, t, ps, dp):
            b = cn[0] % 4
            cn[0] += 1
            dma("sp", xt[b][:, 0:n], x_seq[t * 128:(t + 1) * 128, lo:lo + n], dxt[b], writes=[dxt[b]])
            op("dve", lambda e: e.tensor_tensor(out=xt[b][:, 0:n], in0=ps, in1=xt[b][:, 0:n], op=ALU.add), reads=[dp, dxt[b]], writes=[dxt[b]])
            dma("act", x1_d[t * 128:(t + 1) * 128, lo:lo + n], xt[b][:, 0:n], dxt[b], reads=[dxt[b]], writes=[d_x1])
        lin_tm(wp, W["w_out"], D, 0, D, lambda c, t: mg[:, c, t * 128:(t + 1) * 128], lambda t: dmg, 8, pp, cons_o)
        fw.barrier()
    if stop_after <= 5:
        fw.barrier()
        return nc, fw, dbg_outs, {}

    SC_MEM = float(128 ** -0.5)
    with ExitStack() as st:
        memT = fw.sb("memT", [128, 32, 256], BF16, st); dmemT = [Dep(), Dep()]
        hxT = fw.sb("hxT", [128, 32, NQ], BF16, st); dhx = [Dep() for _ in range(8)]
        with ExitStack() as st2:
            norm_T("nM", st2, mem_in, 256, W["g_mem"], lambda t: (memT[:, :, t * 128:(t + 1) * 128], dmemT[t]))
            fw.barrier()
        with ExitStack() as st2:
            norm_T("nX", st2, x1_d, NQ, W["g_norm_mem_x"], lambda t: (hxT[:, :, t * 128:(t + 1) * 128], dhx[t]), src_deps=[d_x1])
            fw.barrier()
        pp = PsPool("pg", 4, st)
        wp = WPool("wg", st, 3)
        kmT = fw.sb("kmT", [128, 4, 256], BF16, st); dkm = Dep(multi=True)
        vm = fw.sb("vm", [128, 2, 512], BF16, st); dvm = Dep(multi=True)
        qmT = fw.sb("qmT", [128, 4, NQ], BF16, st); dqm = Dep(multi=True)
        omT = fw.sb("omT", [128, 4, NQ], BF16, st); dom = Dep(multi=True)
        lin_fm(wp, [W["w_km"]], D, 0, 512, [(lambda c: memT[:, c, :], dmemT, 256)], pp,
               lambda col, gw, xi, pss, dps: evac(kmT[:, col // 128, :], pss[0], [dps[0]], [dkm]))
        lin_tm(wp, W["w_vm"], D, 0, 512, lambda c, t: memT[:, c, t * 128:(t + 1) * 128], lambda t: [dmemT[t]], 2, pp,
               lambda lo, n, t, ps, dp: evac(vm[:, t, lo:lo + n], ps, [dp], [dvm]))
        xhx = [(lambda c: hxT[:, c, 0:512], dhx[0:4], 512), (lambda c: hxT[:, c, 512:1024], dhx[4:8], 512)]
        lin_fm(wp, [W["w_qm"]], D, 0, 512, xhx, pp,
               lambda col, gw, xi, pss, dps: evac(qmT[:, col // 128, xi * 512:(xi + 1) * 512], pss[0], [dps[0]], [dqm]))
        accm = [fw.ps("accm%d" % i, [128, 512], F32, st) for i in range(2)]; daccm = [Dep(), Dep()]
        exm = [fw.sb("exm%d" % i, [128, 512], BF16, st) for i in range(2)]; dexm = [Dep(), Dep()]
        rdm = fw.sb("rdm", [128, 512], F32, st); drdm = Dep()
        nex = 0
        for h in range(4):
            for tc in range(2):
                for mb in range(2):
                    ps, dp = pp.next()
                    op("pe", lambda e: e.matmul(ps[:, :], lhsT=kmT[:, h, mb * 128:(mb + 1) * 128], rhs=qmT[:, h, tc * 512:(tc + 1) * 512],
                                                start=True, stop=True), reads=[dkm, dqm], writes=[dp])
                    b = nex % 2
                    nex += 1
                    op("act", lambda e: e.activation(out=exm[b][:], in_=ps[:, :], func=AF.Exp, scale=SC_MEM), reads=[dp], writes=[dexm[b]])
                    op("pe", lambda e: e.matmul(accm[0][:, :], lhsT=vm[:, mb, h * 128:(h + 1) * 128], rhs=exm[b][:], start=(mb == 0), stop=(mb == 1)),
                       reads=[dvm, dexm[b]], writes=[daccm[0]], inc=(mb == 1))
                    op("pe", lambda e: e.matmul(accm[1][:, :], lhsT=ones_bf[:], rhs=exm[b][:], start=(mb == 0), stop=(mb == 1)),
                       reads=[d_onb, dexm[b]], writes=[daccm[1]], inc=(mb == 1))
                op("dve", lambda e: e.reciprocal(out=rdm[:], in_=accm[1][:, :]), reads=[daccm[1]], writes=[drdm])
                op("dve", lambda e: e.tensor_tensor(out=omT[:, h, tc * 512:(tc + 1) * 512], in0=accm[0][:, :], in1=rdm[:], op=ALU.mult),
                   reads=[daccm[0], drdm], writes=[dom])
        xt = [fw.sb("xtg%d" % i, [128, 256], F32, st) for i in range(4)]; dxt = [Dep() for _ in range(4)]
        cn = [0]

        def cons_om(lo, n, t, ps, dp):
            b = cn[0] % 4
            cn[0] += 1
            dma("sp", xt[b][:, 0:n], x1_d[t * 128:(t + 1) * 128, lo:lo + n], dxt[b], reads=[d_x1], writes=[dxt[b]])
            op("dve", lambda e: e.tensor_tensor(out=xt[b][:, 0:n], in0=ps, in1=xt[b][:, 0:n], op=ALU.add), reads=[dp, dxt[b]], writes=[dxt[b]])
            dma("act", x2_d[t * 128:(t + 1) * 128, lo:lo + n], xt[b][:, 0:n], dxt[b], reads=[dxt[b]], writes=[d_x2])
        lin_tm(wp, W["w_om"], 512, 0, D, lambda c, t: omT[:, c, t * 128:(t + 1) * 128], lambda t: [dom], 8, pp, cons_om)
        fw.barrier()
    if stop_after <= 6:
        fw.barrier()
        return nc, fw, dbg_outs, {}

    with ExitStack() as st:
        hf = fw.sb("hf", [128, 8, D], BF16, st); dhf = [Dep() for _ in range(8)]
        Sel = fw.sb("Sel", [128, 8, 8, CAP], BF16, st); dSel = Dep(multi=True)
        RWg = fw.sb("RWg", [128, 8, 2, 8], F32, st); dRWg = Dep(multi=True)
        oh = fw.sb("oh", [128, 8, 8], F32, st); doh = Dep(multi=True)
        RW = fw.sb("RW", [128, 8, 64], F32, st); dRW = Dep(multi=True)
        pp = PsPool("ph", 4, st)
        ptb = fw.ps("ptb", [128, 1024], BF16, st); dptb = Dep()
        with ExitStack() as s2:
            gbc = fw.sb("hgbc", [128, D], F32, s2); dg = Dep()
            dma("sp", gbc[:], W["g_norm_ffn"].partition_broadcast(128), dg, writes=[dg])
            xin = fw.sb("hxin", [128, D], F32, s2); dx = Dep()
            hT32 = fw.sb("hT32", [128, 32, 128], F32, s2); dh32 = Dep()
            wr = fw.sb("wr", [128, 32, 72], F32, s2); dwr = Dep()
            dma("sp", wr[:], W["w_r"].rearrange("(c p) n -> p c n", p=128), dwr, writes=[dwr])
            brb = fw.sb("brb", [128, 72], F32, s2); dbr = Dep()
            dma("sp", brb[:], W["b_r"].partition_broadcast(128), dbr, writes=[dbr])
            lg = fw.sb("lg", [128, 72], F32, s2); dlg = Dep()
            sm = fw.sb("sm", [128, 16], F32, s2); dsm = Dep()
            e8 = fw.sb("e8", [128, 8], F32, s2); de8 = Dep()
            les = fw.sb("les", [128, 8], F32, s2); dles = Dep()
            m8 = fw.sb("hm8", [128, 8], F32, s2); dm8 = Dep()
            o12 = fw.sb("o12", [128, 2, 8], F32, s2); do12 = Dep()
            rwl = fw.sb("rwl", [128, 8], F32, s2); drwl = Dep()
            plg = fw.ps("plg", [128, 72], F32, s2); dplg = Dep()
            ptr = [fw.ps("ptr%d" % i, [128, 512], F32, s2) for i in range(2)]; dptr = [Dep(), Dep()]
            ntr = 0
            for m in range(8):
                dma("sp", xin[:], x2_d[m * 128:(m + 1) * 128, :], dx, reads=[d_x2], writes=[dx])
                op("act", lambda e: e.activation(out=hf[:, m, :], in_=xin[:], func=AF.Square, accum_out=sm[:, 0:1]),
                   reads=[dx], writes=[dhf[m], dsm])
                op("dve", lambda e: e.tensor_scalar(out=sm[:, 1:2], in0=sm[:, 0:1], scalar1=1.0 / D, scalar2=EPS, op0=ALU.mult, op1=ALU.add),
                   reads=[dsm], writes=[dsm])
                op("act", lambda e: e.activation(out=sm[:, 1:2], in_=sm[:, 1:2], func=AF.Sqrt), reads=[dsm], writes=[dsm])
                op("dve", lambda e: e.reciprocal(out=sm[:, 1:2], in_=sm[:, 1:2]), reads=[dsm], writes=[dsm])
                op("dve", lambda e: e.scalar_tensor_tensor(out=xin[:], in0=xin[:], scalar=sm[:, 1:2], in1=gbc[:], op0=ALU.mult, op1=ALU.mult),
                   reads=[dx, dsm, dg], writes=[dx])
                op("act", lambda e: e.activation(out=hf[:, m, :], in_=xin[:], func=AF.Copy), reads=[dx], writes=[dhf[m]])
                for q in range(8):
                    b = ntr % 2
                    ntr += 1
                    for j in range(4):
                        c = q * 4 + j
                        op("pe", lambda e: e.transpose(ptr[b][:, j * 128:(j + 1) * 128], xin[:, c * 128:(c + 1) * 128], ident_f[:]),
                           reads=[dx, d_idf], writes=[dptr[b]], inc=(j == 3))
                    evac(hT32[:, q * 4:(q + 1) * 4, :], ptr[b][:, :].rearrange("p (a b) -> p a b", a=4), [dptr[b]], [dh32])
                for c in range(32):
                    op("pe", lambda e: e.matmul(plg[:, :], lhsT=hT32[:, c, :], rhs=wr[:, c, :], start=(c == 0), stop=(c == 31)),
                       reads=[dh32, dwr], writes=[dplg], inc=(c == 31))
                op("dve", lambda e: e.tensor_tensor(out=lg[:], in0=plg[:, :], in1=brb[:], op=ALU.add), reads=[dplg, dbr], writes=[dlg])
                op("dve", lambda e: e.tensor_reduce(out=sm[:, 2:3], in_=lg[:, 0:8], axis=AX.X, op=ALU.max), reads=[dlg], writes=[dsm])
                op("dve", lambda e: e.tensor_scalar(out=sm[:, 3:4], in0=sm[:, 2:3], scalar1=-1.0, scalar2=None, op0=ALU.mult), reads=[dsm], writes=[dsm])
                op("act", lambda e: e.activation(out=e8[:], in_=lg[:, 0:8], func=AF.Exp, bias=sm[:, 3:4], scale=1.0, accum_out=sm[:, 4:5]),
                   reads=[dlg, dsm], writes=[de8, dsm])
                op("dve", lambda e: e.reciprocal(out=sm[:, 5:6], in_=sm[:, 4:5]), reads=[dsm], writes=[dsm])
                op("dve", lambda e: e.tensor_scalar(out=oh[:, m, :], in0=lg[:, 0:8], scalar1=sm[:, 2:3], scalar2=None, op0=ALU.is_equal),
                   reads=[dlg, dsm], writes=[doh])
                op("dve", lambda e: e.tensor_scalar(out=les[:], in0=lg[:, 8:16], scalar1=oh[:, m, 0:1], scalar2=None, op0=ALU.mult),
                   reads=[dlg, doh], writes=[dles])
                for g in range(1, 8):
                    op("dve", lambda e: e.scalar_tensor_tensor(out=les[:], in0=lg[:, 8 + g * 8:16 + g * 8], scalar=oh[:, m, g:g + 1], in1=les[:],
                                                               op0=ALU.mult, op1=ALU.add), reads=[dlg, doh, dles], writes=[dles])
                op("dve", lambda e: e.max(out=m8[:], in_=les[:]), reads=[dles], writes=[dm8])
                op("dve", lambda e: e.tensor_tensor(out=sm[:, 6:7], in0=m8[:, 1:2], in1=m8[:, 0:1], op=ALU.subtract), reads=[dm8], writes=[dsm])
                op("act", lambda e: e.activation(out=sm[:, 7:8], in_=sm[:, 6:7], func=AF.Exp), reads=[dsm], writes=[dsm])
                op("dve", lambda e: e.tensor_scalar(out=sm[:, 7:8], in0=sm[:, 7:8], scalar1=1.0, scalar2=None, op0=ALU.add), reads=[dsm], writes=[dsm])
                op("dve", lambda e: e.reciprocal(out=sm[:, 8:9], in_=sm[:, 7:8]), reads=[dsm], writes=[dsm])
                op("dve", lambda e: e.tensor_tensor(out=sm[:, 9:10], in0=sm[:, 8:9], in1=sm[:, 5:6], op=ALU.mult), reads=[dsm], writes=[dsm])
                op("dve", lambda e: e.tensor_tensor(out=sm[:, 10:11], in0=sm[:, 5:6], in1=sm[:, 9:10], op=ALU.subtract), reads=[dsm], writes=[dsm])
                op("dve", lambda e: e.tensor_scalar(out=o12[:, 0, :], in0=les[:], scalar1=m8[:, 0:1], scalar2=sm[:, 9:10], op0=ALU.is_equal, op1=ALU.mult),
                   reads=[dles, dm8, dsm], writes=[do12])
                op("dve", lambda e: e.tensor_scalar(out=o12[:, 1, :], in0=les[:], scalar1=m8[:, 1:2], scalar2=sm[:, 10:11], op0=ALU.is_equal, op1=ALU.mult),
                   reads=[dles, dm8, dsm], writes=[do12])
                op("dve", lambda e: e.tensor_tensor(out=rwl[:], in0=o12[:, 0, :], in1=o12[:, 1, :], op=ALU.add), reads=[do12], writes=[drwl])
                for g in range(8):
                    op("dve", lambda e: e.tensor_scalar(out=RW[:, m, g * 8:(g + 1) * 8], in0=rwl[:], scalar1=oh[:, m, g:g + 1], scalar2=None, op0=ALU.mult),
                       reads=[drwl, doh], writes=[dRW])
            tril, dtril = fw.sb("tril", [128, 128], BF16, s2), Dep()
            dma("sp", tril[:], C["c_tril_bf"][:, :], dtril, writes=[dtril])
            iota, diota = fw.sb("iota", [128, CAP], F32, s2), Dep()
            dma("sp", iota[:], C["c_iota"][:, :], diota, writes=[diota])
            ohb = fw.sb("ohb", [128, 8, 8], BF16, s2); dohb = Dep()
            op("dve", lambda e: e.tensor_copy(out=ohb[:], in_=oh[:]), reads=[doh], writes=[dohb])
            RWh = fw.sb("RWh", [128, 8, 64], BF16, s2); dRWh = Dep()
            RWl = fw.sb("RWl", [128, 8, 64], BF16, s2); dRWl = Dep()
            op("dve", lambda e: e.tensor_copy(out=RWh[:], in_=RW[:]), reads=[dRW], writes=[dRWh])
            op("dve", lambda e: e.tensor_tensor(out=RWl[:], in0=RW[:], in1=RWh[:], op=ALU.subtract), reads=[dRW, dRWh], writes=[dRWl])
            t8 = fw.sb("t8", [128, 8], F32, s2); dt8 = Dep()
            posv = fw.sb("posv", [128, 1], F32, s2); dpos = Dep()
            pcn = plg; dpcn = dplg
            for m in range(8):
                for m2 in range(m + 1):
                    op("pe", lambda e: e.matmul(pcn[:, 0:8], lhsT=(ones_bf[:] if m2 < m else tril[:]), rhs=ohb[:, m2, :], start=(m2 == 0), stop=(m2 == m)),
                       reads=[d_onb, dtril, dohb], writes=[dpcn], inc=(m2 == m))
                op("dve", lambda e: e.tensor_tensor(out=t8[:], in0=pcn[:, 0:8], in1=oh[:, m, :], op=ALU.mult), reads=[dpcn, doh], writes=[dt8])
                op("dve", lambda e: e.tensor_reduce(out=posv[:], in_=t8[:], axis=AX.X, op=ALU.add), reads=[dt8], writes=[dpos])
                for g in range(8):
                    op("dve" if g % 2 else "pool", lambda e: e.tensor_scalar(out=Sel[:, m, g, :], in0=iota[:], scalar1=posv[:, 0:1], scalar2=oh[:, m, g:g + 1],
                                                                             op0=ALU.is_equal, op1=ALU.mult), reads=[diota, dpos, doh], writes=[dSel])
            for g in range(8):
                for sc in range(2):
                    ps, dp = pp.next()
                    for m in range(8):
                        for hl, (Rt, dRt) in enumerate(((RWh, dRWh), (RWl, dRWl))):
                            op("pe", lambda e: e.matmul(ps[:, 0:8], lhsT=Sel[:, m, g, sc * 128:(sc + 1) * 128], rhs=Rt[:, m, g * 8:(g + 1) * 8],
                                                        start=(m == 0 and hl == 0), stop=(m == 7 and hl == 1)),
                               reads=[dSel, dRt], writes=[dp], inc=(m == 7 and hl == 1))
                    evac(RWg[:, g, sc, :], ps[:, 0:8], [dp], [dRWg], eng="dve")
            fw.barrier()
        with ExitStack() as s2:
            wp = WPool("wh", s2, 3)
            xgT = fw.sb("xgT", [128, 32, CAP], BF16, s2); dxg = Dep()
            actT = fw.sb("actT", [128, 8, 4, CAP], BF16, s2); daT = Dep(multi=True)
            sg = fw.sb("sg", [128, 256], F32, s2); dsg = Dep()
            asb = [fw.sb("asb%d" % i, [128, 512], BF16, s2) for i in range(2)]; dasb = [Dep(), Dep()]
            ygst = [fw.sb("ygst%d" % i, [128, 256], F32, s2) for i in range(2)]; dyg = [Dep(), Dep()]
            ny = 0
            for g in range(8):
                for c in range(32):
                    ps, dp = pp.next()
                    for m in range(8):
                        op("pe", lambda e: e.matmul(ps[:, 0:CAP], lhsT=hf[:, m, c * 128:(c + 1) * 128], rhs=Sel[:, m, g, :], start=(m == 0), stop=(m == 7)),
                           reads=[dhf[m], dSel] if c == 0 else (), writes=[dp], inc=(m == 7))
                    evac(xgT[:, c, :], ps[:, 0:CAP], [dp], [dxg])
                for e8i in range(8):
                    ex_ = g * 8 + e8i
                    for fh in range(2):
                        wg, dwg = wp.load(W["w_gate"][ex_], D, fh * 256, 256, 256)
                        wu, dwu = wp.load(W["w_up"][ex_], D, fh * 256, 256, 256)
                        for s_ in range(2):
                            psg, dpg = pp.next()
                            for c in range(32):
                                op("pe", lambda e: e.matmul(psg[:, 0:256], lhsT=xgT[:, c, s_ * 128:(s_ + 1) * 128], rhs=wg[:, c, :], start=(c == 0), stop=(c == 31)),
                                   reads=[dxg, dwg] if c == 0 else (), writes=[dpg], inc=(c == 31))
                            psu, dpu = pp.next()
                            for c in range(32):
                                op("pe", lambda e: e.matmul(psu[:, 0:256], lhsT=xgT[:, c, s_ * 128:(s_ + 1) * 128], rhs=wu[:, c, :], start=(c == 0), stop=(c == 31)),
                                   reads=[dxg, dwu] if c == 0 else (), writes=[dpu], inc=(c == 31))
                            op("act", lambda e: e.activation(out=sg[:], in_=psg[:, 0:256], func=AF.Silu), reads=[dpg], writes=[dsg])
                            op("dve", lambda e: e.scalar_tensor_tensor(out=asb[s_][:, fh * 256:(fh + 1) * 256], in0=psu[:, 0:256],
                                                                       scalar=RWg[:, g, s_, e8i:e8i + 1], in1=sg[:], op0=ALU.mult, op1=ALU.mult),
                               reads=[dpu, dRWg, dsg], writes=[dasb[s_]])
                    for s_ in range(2):
                        for fc in range(4):
                            op("pe", lambda e: e.transpose(ptb[:, fc * 128:(fc + 1) * 128], asb[s_][:, fc * 128:(fc + 1) * 128], ident_bf[:]),
                               reads=[dasb[s_], d_idb], writes=[dptb], inc=(fc == 3))
                        evac(actT[:, e8i, :, s_ * 128:(s_ + 1) * 128], ptb[:, 0:512].rearrange("p (a b) -> p a b", a=4), [dptb], [daT])
                wdsrc = W["w_down"][g * 8:(g + 1) * 8].rearrange("e f n -> (e f) n")
                for ct in range(16):
                    wd, dwd = wp.load(wdsrc, D, ct * 256, 256, 256)
                    for s_ in range(2):
                        ps, dp = pp.next()
                        for kc in range(32):
                            op("pe", lambda e: e.matmul(ps[:, 0:256], lhsT=actT[:, kc // 4, kc % 4, s_ * 128:(s_ + 1) * 128], rhs=wd[:, kc, :],
                                                        start=(kc == 0), stop=(kc == 31)),
                               reads=[daT, dwd] if kc == 0 else (), writes=[dp], inc=(kc == 31))
                        b = ny % 2
                        ny += 1
                        evac(ygst[b][:], ps[:, 0:256], [dp], [dyg[b]])
                        dma("sp", yg_d[g, s_ * 128:(s_ + 1) * 128, ct * 256:(ct + 1) * 256], ygst[b][:], dyg[b], reads=[dyg[b]], writes=[d_yg])
            fw.barrier()
        with ExitStack() as s2:
            SelT = fw.sb("SelT", [128, 8, 2, NQ], BF16, s2); dST = Dep(multi=True)
            for g in range(8):
                for sc in range(2):
                    for m in range(8):
                        op("pe", lambda e: e.transpose(ptb[:, m * 128:(m + 1) * 128], Sel[:, m, g, sc * 128:(sc + 1) * 128], ident_bf[:]),
                           reads=[dSel, d_idb], writes=[dptb], inc=(m == 7))
                    evac(SelT[:, g, sc, :], ptb[:, :], [dptb], [dST])
            ygf = fw.sb("ygf", [128, 16, 512], F32, s2); dygf = Dep()
            yh = fw.sb("yh", [128, 16, 512], BF16, s2); dyh = Dep()
            yl = fw.sb("yl", [128, 16, 512], BF16, s2); dyl = Dep()
            xz = [fw.sb("xz%d" % i, [128, 512], F32, s2) for i in range(2)]; dxz = [Dep(), Dep()]
            nz = 0
            for ct in range(8):
                dma("sp", ygf[:], yg_d[:, :, ct * 512:(ct + 1) * 512].rearrange("g (sc s) n -> s (g sc) n", sc=2), dygf, reads=[d_yg], writes=[dygf])
                op("act", lambda e: e.activation(out=yh[:], in_=ygf[:], func=AF.Copy), reads=[dygf], writes=[dyh])
                op("dve", lambda e: e.tensor_tensor(out=yl[:], in0=ygf[:], in1=yh[:], op=ALU.subtract), reads=[dygf, dyh], writes=[dyl])
                for m in range(8):
                    ps, dp = pp.next()
                    k = 0
                    for gs in range(16):
                        for (Yt, dY) in ((yh, dyh), (yl, dyl)):
                            op("pe", lambda e: e.matmul(ps[:, :], lhsT=SelT[:, gs // 2, gs % 2, m * 128:(m + 1) * 128], rhs=Yt[:, gs, :],
                                                        start=(k == 0), stop=(k == 31)), reads=[dST, dY], writes=[dp], inc=(k == 31))
                            k += 1
                    b = nz % 2
                    nz += 1
                    dma("sp", xz[b][:], x2_d[m * 128:(m + 1) * 128, ct * 512:(ct + 1) * 512], dxz[b], reads=[d_x2], writes=[dxz[b]])
                    op("dve", lambda e: e.tensor_tensor(out=xz[b][:], in0=ps[:, :], in1=xz[b][:], op=ALU.add), reads=[dp, dxz[b]], writes=[dxz[b]])
                    dma("act", x1_d[m * 128:(m + 1) * 128, ct * 512:(ct + 1) * 512], xz[b][:], dxz[b], reads=[dxz[b]], writes=[d_x1])
            fw.barrier()
    with ExitStack() as st:
        gbc = fw.sb("fgbc", [128, D], F32, st); dg = Dep()
        dma("sp", gbc[:], W["g_final"].partition_broadcast(128), dg, writes=[dg])
        zin = [fw.sb("zin%d" % i, [128, D], F32, st) for i in range(2)]; dz = [Dep(), Dep()]
        zo = [fw.sb("zo%d" % i, [128, D], F32, st) for i in range(2)]; dzo = [Dep(), Dep()]
        sm = [fw.sb("fsm%d" % i, [128, 2], F32, st) for i in range(2)]; dsm = [Dep(), Dep()]
        for m in range(8):
            b = m % 2
            dma("sp", zin[b][:], x1_d[m * 128:(m + 1) * 128, :], dz[b], reads=[d_x1], writes=[dz[b]])
            op("act", lambda e: e.activation(out=zo[b][:], in_=zin[b][:], func=AF.Square, accum_out=sm[b][:, 0:1]), reads=[dz[b]], writes=[dzo[b], dsm[b]])
            op("dve", lambda e: e.tensor_scalar(out=sm[b][:, 1:2], in0=sm[b][:, 0:1], scalar1=1.0 / D, scalar2=EPS, op0=ALU.mult, op1=ALU.add),
               reads=[dsm[b]], writes=[dsm[b]])
            op("act", lambda e: e.activation(out=sm[b][:, 1:2], in_=sm[b][:, 1:2], func=AF.Sqrt), reads=[dsm[b]], writes=[dsm[b]])
            op("dve", lambda e: e.reciprocal(out=sm[b][:, 1:2], in_=sm[b][:, 1:2]), reads=[dsm[b]], writes=[dsm[b]])
            op("dve", lambda e: e.scalar_tensor_tensor(out=zo[b][:], in0=zin[b][:], scalar=sm[b][:, 1:2], in1=gbc[:], op0=ALU.mult, op1=ALU.mult),
               reads=[dz[b], dsm[b], dg], writes=[dzo[b]])
            dma("act", out_d[m * 128:(m + 1) * 128, :], zo[b][:], dzo[b], reads=[dzo[b]], writes=[d_out])
        fw.barrier()
    return nc, fw, dbg_outs, {}


_PROG = {}


def kernel(**inputs):
    hw = host_weights(inputs)
    x = np.asarray(inputs["x"], np.float32)
    mem = np.asarray(inputs["mem"], np.float32)
    if "nc" not in _PROG:
        _PROG["nc"] = build_program()[0]
    nc = _PROG["nc"]
    csts = [host_consts(0), host_consts(1)]
    in_maps = []
    for core in range(8):
        b, par = core // 2, core % 2
        perm = OWN[par] + OWN[1 - par]
        xb = np.ascontiguousarray(x[b].reshape(16, 128, D)[perm].reshape(S, D))
        m = {"x_seq": xb, "mem_b": np.ascontiguousarray(mem[b])}
        m.update(hw)
        m.update(csts[par])
        in_maps.append(m)
    res = run_bass_kernel_spmd(nc, in_maps, core_ids=list(range(8)))
    out = np.empty((4, S, D), np.float32)
    for core in range(8):
        b, par = core // 2, core % 2
        o = np.asarray(res.results[core]["out"], np.float32).reshape(8, 128, D)
        for i, blk in enumerate(OWN[par]):
            out[b, blk * 128:(blk + 1) * 128, :] = o[i]
    return out
```

```python
import numpy as np
import ml_dtypes
from contextlib import ExitStack
import concourse.bass as bass
import concourse.mybir as mybir
from concourse.bass_utils import run_bass_kernel_spmd

F32 = mybir.dt.float32
BF16 = mybir.dt.bfloat16
AF = mybir.ActivationFunctionType
ALU = mybir.AluOpType
AX = mybir.AxisListType

D = 4096
S = 2048
NQ = 1024
EPS = 1e-6
NBLK = 16
OWN = ([0, 3, 4, 7, 8, 11, 12, 15], [1, 2, 5, 6, 9, 10, 13, 14])
IN_SPLITS = (2048, 2048, 2048, 16, 1024, 256, 64, 128, 32, 4096, 4096)
OFF = np.concatenate([[0], np.cumsum(IN_SPLITS)]).tolist()
O_QA, O_KA, O_VA, O_FA, O_CQ, O_CKV, O_KR, O_IK, O_IW, O_GA, O_GB = OFF[:11]
CAP = 256


class Dep:
    __slots__ = ("w", "r", "dsem", "dval", "multi")

    def __init__(self, multi=False):
        self.multi = multi
        self.w = {}
        self.r = {}
        self.dsem = None
        self.dval = 0


class EngS:
    def __init__(self, name, eng, sem):
        self.name, self.eng, self.sem = name, eng, sem
        self.cnt = 0
        self.waited = {}
        self.pend = []
        self.ninst = 0


class FW:
    def __init__(self, nc):
        self.nc = nc
        self.es = ExitStack()
        self.E = {}
        for name, eng in (("pe", nc.tensor), ("act", nc.scalar), ("dve", nc.vector),
                          ("pool", nc.gpsimd), ("sp", nc.sync)):
            sem = self.es.enter_context(nc.semaphore("s_" + name))
            self.E[name] = EngS(name, eng, sem)
        self.dma_deps = []
        self.free_sems = []

    def sb(self, name, shape, dtype, st=None):
        self.uid = getattr(self, "uid", 0) + 1
        return (st or self.es).enter_context(self.nc.sbuf_tensor("%s_u%d" % (name, self.uid), list(shape), dtype))

    def ps(self, name, shape, dtype=F32, st=None):
        self.uid = getattr(self, "uid", 0) + 1
        return (st or self.es).enter_context(self.nc.psum_tensor("%s_u%d" % (name, self.uid), list(shape), dtype))

    def _wait(self, e, sem, val):
        if val <= 0:
            return
        k = id(sem)
        if e.waited.get(k, (None, 0))[1] >= val:
            return
        e.eng.wait_ge(sem, val)
        e.waited[k] = (sem, val)
        e.ninst += 1

    def _pre(self, e, reads, writes):
        for d in reads:
            for k, (sem, val) in d.w.items():
                self._wait(e, sem, val)
        for d in writes:
            if not d.multi:
                for k, (sem, val) in d.w.items():
                    if sem is e.sem:
                        continue
                    self._wait(e, sem, val)
            for k, (sem, val) in d.r.items():
                if sem is e.sem:
                    continue
                self._wait(e, sem, val)

    def op(self, ename, fn, reads=(), writes=(), inc=True):
        e = self.E[ename]
        self._pre(e, reads, writes)
        ins = fn(e.eng)
        e.ninst += 1
        if inc:
            e.cnt += 1
            ins.then_inc(e.sem, 1)
            k = id(e.sem)
            for d in list(reads) + e.pend:
                if d.r.get(k, (None, 0))[1] < e.cnt:
                    d.r[k] = (e.sem, e.cnt)
            e.pend = []
            for d in writes:
                if d.multi:
                    d.w[k] = (e.sem, e.cnt)
                else:
                    d.w = {k: (e.sem, e.cnt)}
                    d.r = {}
        else:
            e.pend.extend(reads)
        return ins

    def dma(self, q, out, in_, sbd, reads=(), writes=(), **kw):
        e = self.E[q]
        if sbd.dsem is None:
            if self.free_sems:
                sbd.dsem, sbd.dval = self.free_sems.pop()
            else:
                self.nsem = getattr(self, "nsem", 0) + 1
                sbd.dsem = self.es.enter_context(self.nc.semaphore("d%d" % self.nsem))
                sbd.dval = 0
            self.dma_deps.append(sbd)
        sem = sbd.dsem
        k = id(sem)
        for d in reads:
            for kk, (s, v) in d.w.items():
                self._wait(e, s, v)
        for d in writes:
            if not d.multi:
                for kk, (s, v) in list(d.w.items()):
                    if s is sem:
                        continue
                    self._wait(e, s, v)
            for kk, (s, v) in list(d.r.items()):
                self._wait(e, s, v)
        ins = e.eng.dma_start(out=out, in_=in_, **kw)
        e.ninst += 1
        sbd.dval += 16
        ins.then_inc(sem, 16)
        val = sbd.dval
        for d in reads:
            if d.r.get(k, (None, 0))[1] < val:
                d.r[k] = (sem, val)
        for d in writes:
            if d.multi or set(d.w.keys()) <= {k}:
                d.w[k] = (sem, val)
            else:
                d.w = {k: (sem, val)}
            if not d.multi:
                d.r = {}
        return ins

    def barrier(self):
        names = ["pe", "act", "dve", "pool", "sp"]
        for n in names:
            e = self.E[n]
            for m in names:
                o = self.E[m]
                if o is not e:
                    self._wait(e, o.sem, o.cnt)
            for d in self.dma_deps:
                self._wait(e, d.dsem, d.dval)
        for d in self.dma_deps:
            self.free_sems.append((d.dsem, d.dval))
            d.dsem = None
        self.dma_deps = []


def _rope_tab(pos, d):
    inv = np.power(np.float32(10000.0), -np.arange(0, d, 2, dtype=np.float32) / np.float32(d)).astype(np.float32)
    ang = pos.astype(np.float32)[:, None] * inv[None, :]
    c, s = np.cos(ang).astype(np.float32), np.sin(ang).astype(np.float32)
    return np.concatenate([c, c], 1), np.concatenate([-s, s], 1)


def host_consts(par):
    own = OWN[par]
    oth = OWN[1 - par]
    perm = own + oth
    pos = (np.array(perm)[:, None] * 128 + np.arange(128)[None, :]).reshape(-1)
    c64, s64 = _rope_tab(pos, 64)
    c128, s128 = _rope_tab(pos, 128)
    prec = np.zeros((16, 16, 16), np.float32)
    for b1 in range(16):
        for b2 in range(16):
            prec[:, b1, b2] = 1.0 if perm[b2] < perm[b1] else 0.0
    visnb = np.zeros((128, 8, 4), np.float32)
    for i in range(8):
        v = 1.0 if oth[i] < own[i] else 0.0
        visnb[:, i, 0] = v
        visnb[:, i, 1] = -30000.0 * (1 - v)
        visnb[:, i, 2] = -1e30 * (1 - v)
    k = np.arange(128)
    sel63 = np.zeros((128, 128), np.float32)
    sel63[63, :] = 1.0
    return {
        "c_ident_bf": np.eye(128).astype(ml_dtypes.bfloat16),
        "c_ident_f": np.eye(128, dtype=np.float32),
        "c_tri_bf": (k[:, None] <= k[None, :]).astype(ml_dtypes.bfloat16),
        "c_tril_bf": (k[:, None] < k[None, :]).astype(ml_dtypes.bfloat16),
        "c_ones_bf": np.ones((128, 128), ml_dtypes.bfloat16),
        "c_ones_f": np.ones((128, 128), np.float32),
        "c_sel63": sel63,
        "c_cos64T": np.ascontiguousarray(c64.T), "c_sin64T": np.ascontiguousarray(s64.T),
        "c_cos128T": np.ascontiguousarray(c128[:NQ].T), "c_sin128T": np.ascontiguousarray(s128[:NQ].T),
        "c_cos128tm": np.ascontiguousarray(c128.reshape(16, 128, 128).transpose(1, 0, 2)),
        "c_sin128tm": np.ascontiguousarray(s128.reshape(16, 128, 128).transpose(1, 0, 2)),
        "c_prec": np.ascontiguousarray(prec.reshape(16, 256)),
        "c_visnb": visnb,
        "c_iota": np.tile(np.arange(CAP, dtype=np.float32)[None, :], (128, 1)),
        "c_causq": (k[:, None] >= k[None, :]).astype(np.float32),
    }


CONST_SPECS = {
    "c_ident_bf": ([128, 128], BF16), "c_ident_f": ([128, 128], F32), "c_tri_bf": ([128, 128], BF16),
    "c_tril_bf": ([128, 128], BF16), "c_ones_bf": ([128, 128], BF16), "c_ones_f": ([128, 128], F32),
    "c_sel63": ([128, 128], F32), "c_cos64T": ([64, S], F32), "c_sin64T": ([64, S], F32),
    "c_cos128T": ([128, NQ], F32), "c_sin128T": ([128, NQ], F32),
    "c_cos128tm": ([128, 16, 128], F32), "c_sin128tm": ([128, 16, 128], F32),
    "c_prec": ([16, 256], F32), "c_visnb": ([128, 8, 4], F32), "c_iota": ([128, CAP], F32),
    "c_causq": ([128, 128], F32),
}

W_SPECS = {
    "g_norm_mix": [D], "w_in": [D, 15856], "w_kr_sw": [D, 64], "b_f": [16, 1], "g_q_lat": [128, 8], "g_kv_lat": [128, 2],
    "g_idx_k": [128], "b_idx_k": [128], "w_uq_nope": [1024, 2048], "w_uq_rope": [1024, 1024],
    "w_uq_rope_sw": [1024, 1024], "w_idx_q": [1024, 4096], "w_idx_q_sw": [1024, 4096],
    "w_uk": [16, 128, 256], "w_uv": [16, 256, 128], "w_up_a": [2048, D], "w_up_b": [2048, D], "w_out": [D, D],
    "g_norm_mem_x": [D], "g_mem": [D], "w_qm": [D, 512], "w_km": [D, 512], "w_vm": [D, 512], "w_om": [512, D],
    "g_norm_ffn": [D], "w_r": [D, 72], "b_r": [72], "w_gate": [64, D, 512], "w_up": [64, D, 512],
    "w_down": [64, 512, D], "g_final": [D],
}


def _swap_halves(w, nheads, hd):
    k = w.shape[0]
    w3 = w.reshape(k, nheads, hd)
    return np.ascontiguousarray(np.concatenate([w3[:, :, hd // 2:], w3[:, :, :hd // 2]], axis=2).reshape(k, nheads * hd))


def host_weights(inp):
    g = lambda n: np.ascontiguousarray(np.asarray(inp[n], np.float32)[0])
    w_uq = g("w_uq").reshape(1024, 16, 192)
    w_uq_rope = np.ascontiguousarray(w_uq[:, :, 128:].reshape(1024, 1024))
    w_in = g("w_in")
    out = {
        "g_norm_mix": g("g_norm_mix"), "w_in": w_in,
        "w_kr_sw": _swap_halves(np.ascontiguousarray(w_in[:, O_KR:O_KR + 64]), 1, 64),
        "b_f": np.ascontiguousarray(g("b_f").reshape(16, 1)), "g_q_lat": np.ascontiguousarray(g("g_q_lat").reshape(8, 128).T), "g_kv_lat": np.ascontiguousarray(g("g_kv_lat").reshape(2, 128).T), "g_idx_k": g("g_idx_k"),
        "b_idx_k": g("b_idx_k"),
        "w_uq_nope": np.ascontiguousarray(w_uq[:, :, :128].reshape(1024, 2048)),
        "w_uq_rope": w_uq_rope, "w_uq_rope_sw": _swap_halves(w_uq_rope, 16, 64),
        "w_idx_q": g("w_idx_q"), "w_idx_q_sw": _swap_halves(g("w_idx_q"), 32, 128),
        "w_uk": g("w_uk"), "w_uv": g("w_uv"), "w_up_a": g("w_up_a"), "w_up_b": g("w_up_b"), "w_out": g("w_out"),
        "g_norm_mem_x": g("g_norm_mem_x"), "g_mem": g("g_mem"), "w_qm": g("w_qm"), "w_km": g("w_km"),
        "w_vm": g("w_vm"), "w_om": g("w_om"), "g_norm_ffn": g("g_norm_ffn"),
        "w_r": np.ascontiguousarray(np.concatenate([g("w_rg"), g("w_re")], axis=1)),
        "b_r": np.ascontiguousarray(np.concatenate([g("b_rg"), g("b_re")], axis=0)),
        "w_gate": g("w_gate"), "w_up": g("w_up"), "w_down": g("w_down"),
        "g_final": np.ascontiguousarray(np.asarray(inp["g_final"], np.float32)),
    }
    return out


def build_program(stop_after=99, dbg=False):
    nc = bass.Bass("TRN2", target_bir_lowering=False)
    fw = FW(nc)
    op, dma = fw.op, fw.dma

    def din(name, shape, dt=F32):
        return nc.dram_tensor(name, list(shape), dt, kind="ExternalInput").ap()

    dbg_outs = {}

    def dscr(name, shape, dt):
        kind = "ExternalOutput" if dbg else "Internal"
        t = nc.dram_tensor(name, list(shape), dt, kind=kind).ap()
        if dbg:
            dbg_outs[name] = t
        return t

    x_seq = din("x_seq", [S, D])
    mem_in = din("mem_b", [256, D])
    W = {n: din(n, s) for n, s in W_SPECS.items()}
    C = {n: din(n, s, dt) for n, (s, dt) in CONST_SPECS.items()}
    out_d = nc.dram_tensor("out", [NQ, D], F32, kind="ExternalOutput").ap()
    d_out = Dep(multi=True)

    kT_d = dscr("kT_d", [16, 128, S], BF16); d_kT = Dep(multi=True)
    v_d = dscr("v_d", [S, 2048], BF16); d_v = Dep(multi=True)
    qT_d = dscr("qT_d", [16, 128, NQ], BF16); d_qT = Dep(multi=True)
    gT_d = dscr("gT_d", [64, 128, NQ], BF16); d_gT = Dep(multi=True)
    qcat_d = dscr("qcat_d", [16, 320, NQ], BF16); d_qcat = Dep(multi=True)
    qidx_d = dscr("qidx_d", [32, 128, NQ], BF16); d_qidx = Dep(multi=True)
    foT_d = dscr("foT_d", [16, 128, NQ], BF16); d_foT = Dep(multi=True)
    obT_d = dscr("obT_d", [16, 128, NQ], BF16); d_obT = Dep(multi=True)
    x1_d = dscr("x1_d", [NQ, D], F32); d_x1 = Dep(multi=True)
    x2_d = dscr("x2_d", [NQ, D], F32); d_x2 = Dep(multi=True)
    yg_d = dscr("yg_d", [8, CAP, D], F32); d_yg = Dep(multi=True)

    G = fw.es

    def cload(name, shape, dt, src, q="sp"):
        t = fw.sb(name, shape, dt, G)
        d = Dep()
        dma(q, t[:], src, d, writes=[d])
        return t, d

    ident_bf, d_idb = cload("ident_bf", [128, 128], BF16, C["c_ident_bf"][:, :])
    ident_f, d_idf = cload("ident_f", [128, 128], F32, C["c_ident_f"][:, :])
    tri_bf, d_tri = cload("tri_bf", [128, 128], BF16, C["c_tri_bf"][:, :])
    ones_bf, d_onb = cload("ones_bf", [128, 128], BF16, C["c_ones_bf"][:, :])
    ones_f, d_onf = cload("ones_f", [128, 128], F32, C["c_ones_f"][:, :])
    visnb, d_vis = cload("visnb", [128, 8, 4], F32, C["c_visnb"][:, :, :])
    epsc = fw.sb("epsc", [128, 1], F32, G); d_eps = Dep()
    op("dve", lambda e: e.memset(epsc[:], EPS), writes=[d_eps])

    MS = ExitStack()
    G = MS
    kvcT = fw.sb("kvcT", [128, 3, S], BF16, G); d_kvcT = Dep(multi=True)
    kvlat = fw.sb("kvlat", [128, 16, 256], BF16, G); d_kvlat = Dep(multi=True)
    kidxT = fw.sb("kidxT", [128, S], BF16, G); d_kidxT = Dep(multi=True)
    negck = fw.sb("negck", [128, 16, 16], F32, G); d_negck = Dep()
    Rref = fw.sb("Rref", [128, 16, 16], F32, G); d_Rref = Dep()
    cqT = fw.sb("cqT", [128, 8, NQ], BF16, G); d_cqT = Dep(multi=True)
    Lsb = fw.sb("Lsb", [16, S], F32, G); dL = Dep(multi=True)
    wi_sb = fw.sb("wi_sb", [128, 8, 32], F32, G); d_wi = Dep(multi=True)

    def norm_T(tag, st, src, ntok, g_ap, dst_fn, f32_fn=None, src_deps=()):
        gbc = fw.sb(tag + "gbc", [128, D], BF16, st); dg = Dep()
        dma("pool", gbc[:], g_ap.partition_broadcast(128), dg, writes=[dg])
        xin = [fw.sb(tag + "xin%d" % i, [128, D], F32, st) for i in range(2)]
        dx = [Dep(), Dep()]
        xs = [fw.sb(tag + "xs%d" % i, [128, D], BF16, st) for i in range(2)]
        dxs = [Dep(), Dep()]
        ss = [fw.sb(tag + "ss%d" % i, [128, 2], F32, st) for i in range(2)]
        dss = [Dep(), Dep()]
        pst = [fw.ps(tag + "pt%d" % i, [128, 1024], BF16, st) for i in range(2)]
        dpt = [Dep(), Dep()]
        nev = 0
        for t in range(ntok // 128):
            b = t % 2
            dma("sp", xin[b][:], src[t * 128:(t + 1) * 128, :], dx[b], reads=list(src_deps), writes=[dx[b]])
            op("act", lambda e: e.activation(out=xs[b][:], in_=xin[b][:], func=AF.Square, accum_out=ss[b][:, 0:1]),
               reads=[dx[b]], writes=[dxs[b], dss[b]])
            op("dve", lambda e: e.tensor_scalar(out=ss[b][:, 1:2], in0=ss[b][:, 0:1], scalar1=1.0 / D, scalar2=EPS,
                                                op0=ALU.mult, op1=ALU.add), reads=[dss[b]], writes=[dss[b]])
            op("act", lambda e: e.activation(out=ss[b][:, 1:2], in_=ss[b][:, 1:2], func=AF.Sqrt),
               reads=[dss[b]], writes=[dss[b]])
            op("dve", lambda e: e.reciprocal(out=ss[b][:, 1:2], in_=ss[b][:, 1:2]), reads=[dss[b]], writes=[dss[b]])
            if f32_fn is not None:
                f32_fn(t, xin[b], dx[b], ss[b], dss[b], gbc, dg)
            op("dve", lambda e: e.scalar_tensor_tensor(out=xs[b][:], in0=xin[b][:], scalar=ss[b][:, 1:2], in1=gbc[:],
                                                       op0=ALU.mult, op1=ALU.mult),
               reads=[dx[b], dss[b], dg], writes=[dxs[b]])
            dst, ddst = dst_fn(t)
            for q4 in range(4):
                pb = nev % 2
                nev += 1
                for c8 in range(8):
                    c = q4 * 8 + c8
                    op("pe", lambda e: e.transpose(pst[pb][:, c8 * 128:(c8 + 1) * 128], xs[b][:, c * 128:(c + 1) * 128],
                                                   ident_bf[:]),
                       reads=[dxs[b], d_idb], writes=[dpt[pb]], inc=(c8 == 7))
                src_ps = pst[pb][:].rearrange("p (c t) -> p c t", c=8)
                if q4 % 2 == 0:
                    op("act", lambda e: e.activation(out=dst[:, q4 * 8:(q4 + 1) * 8, :], in_=src_ps, func=AF.Copy),
                       reads=[dpt[pb]], writes=[ddst])
                else:
                    op("dve", lambda e: e.tensor_copy(out=dst[:, q4 * 8:(q4 + 1) * 8, :], in_=src_ps),
                       reads=[dpt[pb]], writes=[ddst])

    class PsPool:
        def __init__(self, tag, n, st, shape=(128, 512), dt=F32):
            self.t = [fw.ps("%s%d" % (tag, i), list(shape), dt, st) for i in range(n)]
            self.d = [Dep() for _ in range(n)]
            self.i = 0

        def next(self):
            j = self.i % len(self.t)
            self.i += 1
            return self.t[j], self.d[j]

    class WPool:
        def __init__(self, tag, st, nbuf, nelem=8192):
            self.t = [fw.sb("%s%d" % (tag, i), [128, nelem], BF16, st) for i in range(nbuf)]
            self.d = [Dep() for _ in range(nbuf)]
            self.i = 0
            self.nelem = nelem

        def load(self, Wap, K, lo, n, ct):
            kc = K // 128
            assert kc * ct <= self.nelem
            j = self.i % len(self.t)
            self.i += 1
            v = self.t[j][:, 0:kc * ct].rearrange("p (c n) -> p c n", c=kc)
            dma("pool", v[:, :, 0:n], Wap.rearrange("(c p) n -> p c n", p=128)[:, :, lo:lo + n], self.d[j],
                writes=[self.d[j]])
            return v, self.d[j]

    def lin_fm(wp, Ws, K, col_lo, ncols, xchunks, pspool, consume, CT=256):
        kc = K // 128
        nw = len(Ws)
        ntile = (ncols + CT - 1) // CT
        for ti in range(ntile):
            lo = col_lo + ti * CT
            n = min(CT, col_lo + ncols - lo)
            wv = [wp.load(Ws[j], K, lo, n, CT) for j in range(nw)]
            for g0 in range(0, n, 128):
                gw = min(128, n - g0)
                for xi, (xfn, xdeps, xn) in enumerate(xchunks):
                    pss, dps = [], []
                    for j in range(nw):
                        ps, dp = pspool.next()
                        for c in range(kc):
                            op("pe", lambda e: e.matmul(ps[0:gw, 0:xn], lhsT=wv[j][0][:, c, g0:g0 + gw], rhs=xfn(c),
                                                        start=(c == 0), stop=(c == kc - 1)),
                               reads=([wv[j][1]] + list(xdeps)) if c == 0 else (), writes=[dp], inc=(c == kc - 1))
                        pss.append(ps[0:gw, 0:xn])
                        dps.append(dp)
                    consume(lo + g0, gw, xi, pss, dps)

    def lin_tm(wp, Wap, K, col_lo, ncols, xT_fn, xdeps_fn, ntiles, pspool, consume, CT=256):
        kc = K // 128
        ntile = (ncols + CT - 1) // CT
        for ti in range(ntile):
            lo = col_lo + ti * CT
            n = min(CT, col_lo + ncols - lo)
            wv, dw = wp.load(Wap, K, lo, n, CT)
            for t in range(ntiles):
                ps, dp = pspool.next()
                for c in range(kc):
                    op("pe", lambda e: e.matmul(ps[:, 0:n], lhsT=xT_fn(c, t), rhs=wv[:, c, 0:n],
                                                start=(c == 0), stop=(c == kc - 1)),
                       reads=([dw] + list(xdeps_fn(t))) if c == 0 else (), writes=[dp], inc=(c == kc - 1))
                consume(lo, n, t, ps[:, 0:n], dp)

    evac_rr = [0]

    def evac(out, in_, reads, writes, eng=None):
        if eng is None:
            eng = ("act", "dve")[evac_rr[0] % 2]
            evac_rr[0] += 1
        if eng == "act":
            op("act", lambda e: e.activation(out=out, in_=in_, func=AF.Copy), reads=reads, writes=writes)
        else:
            op("dve", lambda e: e.tensor_copy(out=out, in_=in_), reads=reads, writes=writes)

    hst = ExitStack()
    hT = fw.sb("hT", [128, 32, NQ], BF16, hst)
    dh = [Dep() for _ in range(8)]

    def load_hT(half):
        with ExitStack() as st:
            norm_T("nA%d" % half, st, x_seq[half * NQ:(half + 1) * NQ, :], NQ, W["g_norm_mix"],
                   lambda t: (hT[:, :, t * 128:(t + 1) * 128], dh[t]))
            fw.barrier()

    xch = [(lambda c: hT[:, c, 0:512], dh[0:4], 512), (lambda c: hT[:, c, 512:1024], dh[4:8], 512)]

    def hT_tile(c, t):
        return hT[:, c, t * 128:(t + 1) * 128]

    def kside(half):
        T0 = half * NQ
        with ExitStack() as st:
            pp = PsPool("pk%d" % half, 6, st)
            wp = WPool("wk", st, 3)
            kstage = [fw.sb("kstg%d" % i, [128, NQ], BF16, st) for i in range(2)]
            dks = [Dep(), Dep()]

            def cons_k(col, gw, xi, pss, dps):
                h = (col - O_KA) // 128
                b = h % 2
                evac(kstage[b][:, xi * 512:(xi + 1) * 512], pss[0], [dps[0]], [dks[b]])
                if xi == 1:
                    dma("sp", kT_d[h, :, T0:T0 + NQ], kstage[b][:], dks[b], reads=[dks[b]], writes=[d_kT])
            lin_fm(wp, [W["w_in"]], D, O_KA, 2048, xch, pp, cons_k)

            nbf = fw.sb("nbf", [16, 1], F32, st); dnbf = Dep()
            dma("sp", nbf[:], W["b_f"][:, :], dnbf, writes=[dnbf])
            op("act", lambda e: e.mul(nbf[:], nbf[:], -1.0), reads=[dnbf], writes=[dnbf])

            def cons_f(col, gw, xi, pss, dps):
                sl = Lsb[:, T0 + xi * 512:T0 + (xi + 1) * 512]
                op("act", lambda e: e.activation(out=sl, in_=pss[0], func=AF.Exp, bias=nbf[:, 0:1], scale=-1.0),
                   reads=[dps[0], dnbf], writes=[dL])
                op("act", lambda e: e.activation(out=sl, in_=sl, func=AF.Ln, bias=1.0, scale=1.0), reads=[dL], writes=[dL])
            lin_fm(wp, [W["w_in"]], D, O_FA, 16, xch, pp, cons_f, CT=16)

            xc = fw.sb("xckv", [128, 2, NQ], BF16, st); dxc = Dep(multi=True)
            sq = fw.sb("sqkv", [128, 2, NQ], BF16, st); dsq = Dep(multi=True)

            def cons_ckv(col, gw, xi, pss, dps):
                g = (col - O_CKV) // 128
                op("act", lambda e: e.activation(out=xc[:, g, xi * 512:(xi + 1) * 512], in_=pss[0], func=AF.Copy),
                   reads=[dps[0]], writes=[dxc])
                op("dve", lambda e: e.tensor_tensor(out=sq[:, g, xi * 512:(xi + 1) * 512], in0=pss[0],
                                                    in1=xc[:, g, xi * 512:(xi + 1) * 512], op=ALU.mult),
                   reads=[dps[0], dxc], writes=[dsq])
            lin_fm(wp, [W["w_in"]], D, O_CKV, 256, xch, pp, cons_ckv)
            gkv = fw.sb("gkv", [128, 2], F32, st); dgkv = Dep()
            dma("sp", gkv[:], W["g_kv_lat"][:, :], dgkv, writes=[dgkv])
            rs = fw.sb("rskv", [128, 512], F32, st); drs = Dep()
            for xi in range(2):
                ps, dp = pp.next()
                for g in range(2):
                    op("pe", lambda e: e.matmul(ps[:, :], lhsT=ones_bf[:], rhs=sq[:, g, xi * 512:(xi + 1) * 512],
                                                start=(g == 0), stop=(g == 1)), reads=[d_onb, dsq], writes=[dp], inc=(g == 1))
                op("act", lambda e: e.activation(out=rs[:], in_=ps[:, :], func=AF.Sqrt, scale=1.0 / 256, bias=epsc[:, 0:1]),
                   reads=[dp, d_eps], writes=[drs])
                op("dve", lambda e: e.reciprocal(out=rs[:], in_=rs[:]), reads=[drs], writes=[drs])
                for g in range(2):
                    op("dve", lambda e: e.scalar_tensor_tensor(out=kvcT[:, g, T0 + xi * 512:T0 + (xi + 1) * 512],
                                                               in0=xc[:, g, xi * 512:(xi + 1) * 512], scalar=gkv[:, g:g + 1],
                                                               in1=rs[:], op0=ALU.mult, op1=ALU.mult),
                       reads=[dxc, dgkv, drs], writes=[d_kvcT])
            ptk = fw.ps("ptk", [128, 1024], BF16, st); dptk = Dep()
            for bl4 in range(2):
                for j in range(4):
                    bl = half * 8 + bl4 * 4 + j
                    for g in range(2):
                        last = (j == 3 and g == 1)
                        op("pe", lambda e: e.transpose(ptk[:, (j * 2 + g) * 128:(j * 2 + g + 1) * 128],
                                                       kvcT[:, g, bl * 128:(bl + 1) * 128], ident_bf[:]),
                           reads=[d_kvcT, d_idb], writes=[dptk], inc=last)
                b0 = half * 8 + bl4 * 4
                evac(kvlat[:, b0:b0 + 4, :], ptk[:].rearrange("p (a b) -> p a b", a=4), [dptk], [d_kvlat])

            cos64 = fw.sb("cos64", [64, NQ], F32, st); dc64 = Dep()
            sin64 = fw.sb("sin64", [64, NQ], F32, st); ds64 = Dep()
            dma("sp", cos64[:], C["c_cos64T"][:, T0:T0 + NQ], dc64, writes=[dc64])
            dma("sp", sin64[:], C["c_sin64T"][:, T0:T0 + NQ], ds64, writes=[ds64])
            tA = fw.sb("tA", [64, 512], F32, st); dtA = Dep()
            tB = fw.sb("tB", [64, 512], F32, st); dtB = Dep()

            def cons_kr(col, gw, xi, pss, dps):
                op("dve", lambda e: e.tensor_tensor(out=tA[:, :], in0=pss[0], in1=cos64[:, xi * 512:(xi + 1) * 512],
                                                    op=ALU.mult), reads=[dps[0], dc64], writes=[dtA])
                op("dve", lambda e: e.tensor_tensor(out=tB[:, :], in0=pss[1], in1=sin64[:, xi * 512:(xi + 1) * 512],
                                                    op=ALU.mult), reads=[dps[1], ds64], writes=[dtB])
                op("pool", lambda e: e.tensor_tensor(out=kvcT[0:64, 2, T0 + xi * 512:T0 + (xi + 1) * 512], in0=tA[:, :],
                                                     in1=tB[:, :], op=ALU.add), reads=[dtA, dtB], writes=[d_kvcT])
            lin_fm(wp, [W["w_in"][:, O_KR:O_KR + 64], W["w_kr_sw"]], D, 0, 64, xch, pp, cons_kr, CT=64)

            vstage = [fw.sb("vstg%d" % i, [128, 256], BF16, st) for i in range(2)]
            dvs = [Dep(), Dep()]
            vcnt = [0]

            def cons_v(lo, n, t, ps, dp):
                b = vcnt[0] % 2
                vcnt[0] += 1
                evac(vstage[b][:, 0:n], ps, [dp], [dvs[b]])
                dma("sp", v_d[T0 + t * 128:T0 + (t + 1) * 128, lo - O_VA:lo - O_VA + n], vstage[b][:, 0:n], dvs[b],
                    reads=[dvs[b]], writes=[d_v])
            lin_tm(wp, W["w_in"], D, O_VA, 2048, hT_tile, lambda t: [dh[t]], 8, pp, cons_v, CT=256)

            gi = fw.sb("gi", [128, 128], F32, st); dgi = Dep()
            bi = fw.sb("bi", [128, 128], F32, st); dbi = Dep()
            dma("sp", gi[:], W["g_idx_k"].partition_broadcast(128), dgi, writes=[dgi])
            dma("sp", bi[:], W["b_idx_k"].partition_broadcast(128), dbi, writes=[dbi])
            c128 = fw.sb("c128tm", [128, 8, 128], F32, st); dc128 = Dep()
            s128 = fw.sb("s128tm", [128, 8, 128], F32, st); ds128 = Dep()
            dma("sp", c128[:], C["c_cos128tm"][:, half * 8:(half + 1) * 8, :], dc128, writes=[dc128])
            dma("sp", s128[:], C["c_sin128tm"][:, half * 8:(half + 1) * 8, :], ds128, writes=[ds128])
            bst = fw.sb("bst", [128, 8], F32, st); dbst = Dep()
            xk = fw.sb("xk", [128, 128], F32, st); dxk = Dep()
            xr = fw.sb("xr", [128, 128], F32, st); dxr = Dep()
            xo = fw.sb("xo", [128, 128], BF16, st); dxo = Dep()
            ptx = fw.ps("ptx", [128, 128], BF16, st); dptx = Dep()

            def cons_ik(lo, n, t, ps, dp):
                op("dve", lambda e: e.bn_stats(out=bst[:, 0:6], in_=ps), reads=[dp], writes=[dbst])
                op("dve", lambda e: e.bn_aggr(out=bst[:, 6:8], in_=bst[:, 0:6]), reads=[dbst], writes=[dbst])
                op("act", lambda e: e.activation(out=bst[:, 7:8], in_=bst[:, 7:8], func=AF.Sqrt, bias=epsc[:, 0:1], scale=1.0),
                   reads=[dbst, d_eps], writes=[dbst])
                op("dve", lambda e: e.reciprocal(out=bst[:, 7:8], in_=bst[:, 7:8]), reads=[dbst], writes=[dbst])
                op("dve", lambda e: e.tensor_scalar(out=xk[:], in0=ps, scalar1=bst[:, 6:7], scalar2=bst[:, 7:8],
                                                    op0=ALU.subtract, op1=ALU.mult), reads=[dp, dbst], writes=[dxk])
                op("dve", lambda e: e.tensor_tensor(out=xk[:], in0=xk[:], in1=gi[:], op=ALU.mult), reads=[dxk, dgi], writes=[dxk])
                op("dve", lambda e: e.tensor_tensor(out=xk[:], in0=xk[:], in1=bi[:], op=ALU.add), reads=[dxk, dbi], writes=[dxk])
                op("pool", lambda e: e.tensor_tensor(out=xr[:, 0:64], in0=xk[:, 64:128], in1=s128[:, t, 0:64], op=ALU.mult),
                   reads=[dxk, ds128], writes=[dxr])
                op("pool", lambda e: e.tensor_tensor(out=xr[:, 64:128], in0=xk[:, 0:64], in1=s128[:, t, 64:128], op=ALU.mult),
                   reads=[dxk, ds128], writes=[dxr])
                op("dve", lambda e: e.tensor_tensor(out=xk[:], in0=xk[:], in1=c128[:, t, :], op=ALU.mult),
                   reads=[dxk, dc128, dxr], writes=[dxk])
                op("dve", lambda e: e.tensor_tensor(out=xo[:], in0=xk[:], in1=xr[:], op=ALU.add), reads=[dxk, dxr], writes=[dxo])
                op("pe", lambda e: e.transpose(ptx[:], xo[:], ident_bf[:]), reads=[dxo, d_idb], writes=[dptx])
                evac(kidxT[:, T0 + t * 128:T0 + (t + 1) * 128], ptx[:], [dptx], [d_kidxT])
            lin_tm(wp, W["w_in"], D, O_IK, 128, hT_tile, lambda t: [dh[t]], 8, pp, cons_ik, CT=128)
            fw.barrier()

    def qside():
        with ExitStack() as st:
            pp = PsPool("pq", 6, st)
            wp = WPool("wq", st, 3)
            qstage = [fw.sb("qstg%d" % i, [128, NQ], BF16, st) for i in range(2)]
            dqs = [Dep(), Dep()]

            def cons_q(col, gw, xi, pss, dps):
                h = (col - O_QA) // 128
                b = h % 2
                evac(qstage[b][:, xi * 512:(xi + 1) * 512], pss[0], [dps[0]], [dqs[b]])
                if xi == 1:
                    dma("sp", qT_d[h, :, :], qstage[b][:], dqs[b], reads=[dqs[b]], writes=[d_qT])
            lin_fm(wp, [W["w_in"]], D, O_QA, 2048, xch, pp, cons_q)

            def cons_g(col, gw, xi, pss, dps):
                gidx = (col - O_GA) // 128
                b = gidx % 2
                op("act", lambda e: e.activation(out=qstage[b][:, xi * 512:(xi + 1) * 512], in_=pss[0], func=AF.Sigmoid),
                   reads=[dps[0]], writes=[dqs[b]])
                if xi == 1:
                    dma("sp", gT_d[gidx, :, :], qstage[b][:], dqs[b], reads=[dqs[b]], writes=[d_gT])
            lin_fm(wp, [W["w_in"]], D, O_GA, 8192, xch, pp, cons_g)

            def cons_iw(lo, n, t, ps, dp):
                op("act", lambda e: e.activation(out=wi_sb[:, t, :], in_=ps, func=AF.Copy, scale=float(4096 ** -0.5)),
                   reads=[dp], writes=[d_wi])
            lin_tm(wp, W["w_in"], D, O_IW, 32, hT_tile, lambda t: [dh[t]], 8, pp, cons_iw, CT=32)

            gq = fw.sb("gq", [128, 8], F32, st); dgq = Dep()
            dma("sp", gq[:], W["g_q_lat"][:, :], dgq, writes=[dgq])
            xc = fw.sb("xcq", [128, 8, 512], BF16, st); dxc = Dep(multi=True)
            sq = fw.sb("sqq", [128, 8, 512], BF16, st); dsq = Dep(multi=True)
            rs = fw.sb("rsq", [128, 512], F32, st); drs = Dep()
            for xi in range(2):
                def cons_cq(col, gw, xi_, pss, dps):
                    g = (col - O_CQ) // 128
                    op("act", lambda e: e.activation(out=xc[:, g, :], in_=pss[0], func=AF.Copy), reads=[dps[0]], writes=[dxc])
                    op("dve", lambda e: e.tensor_tensor(out=sq[:, g, :], in0=pss[0], in1=xc[:, g, :], op=ALU.mult),
                       reads=[dps[0], dxc], writes=[dsq])
                lin_fm(wp, [W["w_in"]], D, O_CQ, 1024, [xch[xi]], pp, cons_cq)
                ps, dp = pp.next()
                for g in range(8):
                    op("pe", lambda e: e.matmul(ps[:, :], lhsT=ones_bf[:], rhs=sq[:, g, :], start=(g == 0), stop=(g == 7)),
                       reads=[d_onb, dsq], writes=[dp], inc=(g == 7))
                op("act", lambda e: e.activation(out=rs[:], in_=ps[:, :], func=AF.Sqrt, scale=1.0 / 1024, bias=epsc[:, 0:1]),
                   reads=[dp, d_eps], writes=[drs])
                op("dve", lambda e: e.reciprocal(out=rs[:], in_=rs[:]), reads=[drs], writes=[drs])
                for g in range(8):
                    op("dve", lambda e: e.scalar_tensor_tensor(out=cqT[:, g, xi * 512:(xi + 1) * 512], in0=xc[:, g, :],
                                                               scalar=gq[:, g:g + 1], in1=rs[:], op0=ALU.mult, op1=ALU.mult),
                       reads=[dxc, dgq, drs], writes=[d_cqT, dxc, dsq])
            fw.barrier()

    def fox_cum():
        with ExitStack() as st:
            Wc = fw.sb("Wc", [16, 16, 128], F32, st); dWc = Dep()
            onesr = fw.sb("onesr", [16, 128], F32, st); dor = Dep()
            op("dve", lambda e: e.memset(onesr[:], 1.0), writes=[dor])
            for bl in range(16):
                op("dve", lambda e: e.tensor_tensor_scan(out=Wc[:, bl, :], data0=onesr[:], data1=Lsb[:, bl * 128:(bl + 1) * 128],
                                                         initial=0.0, op0=ALU.mult, op1=ALU.add),
                   reads=[dL, dor], writes=[dWc])
            prec_sb = fw.sb("prec_sb", [16, 16, 16], F32, st); dpr = Dep()
            dma("sp", prec_sb[:], C["c_prec"].rearrange("p (a b) -> p a b", a=16), dpr, writes=[dpr])
            tmpP = fw.sb("tmpP", [16, 16, 16], F32, st); dtp = Dep()
            Pfx = fw.sb("Pfx", [16, 16], F32, st); dpf = Dep()
            for b1 in range(16):
                op("dve", lambda e: e.tensor_tensor(out=tmpP[:, b1, :], in0=prec_sb[:, b1, :], in1=Wc[:, :, 127],
                                                    op=ALU.mult), reads=[dpr, dWc], writes=[dtp])
            op("dve", lambda e: e.tensor_reduce(out=Pfx[:], in_=tmpP[:], axis=AX.X, op=ALU.add), reads=[dtp], writes=[dpf])
            for bl in range(16):
                op("dve", lambda e: e.tensor_scalar(out=Wc[:, bl, :], in0=Wc[:, bl, :], scalar1=Pfx[:, bl:bl + 1], scalar2=None,
                                                    op0=ALU.add), reads=[dpf, dWc], writes=[dWc])
            pT = fw.ps("pTck", [128, 256], F32, st); dpT = Dep()
            for bl in range(16):
                op("pe", lambda e: e.transpose(pT[:, bl * 16:(bl + 1) * 16], Wc[:, bl, :], ident_f[0:16, 0:16]),
                   reads=[dWc, d_idf], writes=[dpT], inc=(bl == 15))
            evac(negck[:].rearrange("p a b -> p (a b)"), pT[:], [dpT], [d_negck], eng="dve")
            sel63, dsel = fw.sb("sel63", [128, 128], F32, st), Dep()
            dma("sp", sel63[:], C["c_sel63"][:, :], dsel, writes=[dsel])
            op("pe", lambda e: e.matmul(pT[:], lhsT=sel63[:], rhs=negck[:].rearrange("p a b -> p (a b)"), start=True, stop=True),
               reads=[dsel, d_negck], writes=[dpT])
            evac(Rref[:].rearrange("p a b -> p (a b)"), pT[:], [dpT], [d_Rref], eng="dve")
            fw.barrier()

    load_hT(0)
    kside(0)
    qside()
    load_hT(1)
    kside(1)
    hst.close()
    fox_cum()

    def dump_sb(name, t, shape, dt, dep):
        o = nc.dram_tensor("dbg_" + name, list(shape), dt, kind="ExternalOutput").ap()
        dd = Dep()
        dma("sp", o, t, dep, reads=[dep], writes=[dd])
        dbg_outs["dbg_" + name] = o

    if stop_after <= 1:
        dump_sb("negck", negck[:], [128, 16, 16], F32, d_negck)
        dump_sb("kvcT", kvcT[:], [128, 3, S], BF16, d_kvcT)
        dump_sb("kidxT", kidxT[:], [128, S], BF16, d_kidxT)
        dump_sb("cqT", cqT[:], [128, 8, NQ], BF16, d_cqT)
        fw.barrier()
        return nc, fw, dbg_outs, {}

    xchq = [(lambda c: cqT[:, c, 0:512], [d_cqT], 512), (lambda c: cqT[:, c, 512:1024], [d_cqT], 512)]
    with ExitStack() as st:
        pp = PsPool("pc", 6, st)
        wp = WPool("wc", st, 4, nelem=2048)
        wuk = fw.sb("wuk", [128, 16, 256], BF16, st); dwuk = Dep()
        dma("pool", wuk[:], W["w_uk"].rearrange("h n r -> n h r"), dwuk, writes=[dwuk])
        qn = [fw.sb("qn%d" % i, [128, NQ], BF16, st) for i in range(2)]; dqn = [Dep(), Dep()]
        qcs = [fw.sb("qcs%d" % i, [128, NQ], BF16, st) for i in range(2)]; dqcs = [Dep(), Dep()]
        cnt = [0]

        def cons_qn(col, gw, xi, pss, dps):
            h = col // 128
            b = h % 2
            evac(qn[b][:, xi * 512:(xi + 1) * 512], pss[0], [dps[0]], [dqn[b]])
            if xi == 1:
                for rc in range(2):
                    b2 = cnt[0] % 2
                    cnt[0] += 1
                    for tc in range(2):
                        ps, dp = pp.next()
                        op("pe", lambda e: e.matmul(ps[:, :], lhsT=wuk[:, h, rc * 128:(rc + 1) * 128],
                                                    rhs=qn[b][:, tc * 512:(tc + 1) * 512], start=True, stop=True),
                           reads=[dwuk, dqn[b]], writes=[dp])
                        evac(qcs[b2][:, tc * 512:(tc + 1) * 512], ps[:, :], [dp], [dqcs[b2]])
                    dma("sp", qcat_d[h, rc * 128:(rc + 1) * 128, :], qcs[b2][:], dqcs[b2], reads=[dqcs[b2]], writes=[d_qcat])
        lin_fm(wp, [W["w_uq_nope"]], 1024, 0, 2048, xchq, pp, cons_qn)

        tA = fw.sb("tAc", [128, 512], F32, st); dtA = Dep()
        tB = fw.sb("tBc", [128, 512], F32, st); dtB = Dep()
        cs = fw.sb("cs64x2", [128, NQ], F32, st); dcs = Dep()
        sn = fw.sb("sn64x2", [128, NQ], F32, st); dsn = Dep()
        for hh in range(2):
            dma("sp", cs[hh * 64:(hh + 1) * 64, :], C["c_cos64T"][:, 0:NQ], dcs, writes=[dcs])
            dma("sp", sn[hh * 64:(hh + 1) * 64, :], C["c_sin64T"][:, 0:NQ], dsn, writes=[dsn])

        def rope_cons(cosT, dcos, sinT, dsin, store):
            def cons(col, gw, xi, pss, dps):
                g = col // 128
                b = g % 2
                sl = slice(xi * 512, (xi + 1) * 512)
                op("dve", lambda e: e.tensor_tensor(out=tA[:, :], in0=pss[0], in1=cosT[:, sl], op=ALU.mult),
                   reads=[dps[0], dcos], writes=[dtA])
                op("dve", lambda e: e.tensor_tensor(out=tB[:, :], in0=pss[1], in1=sinT[:, sl], op=ALU.mult),
                   reads=[dps[1], dsin], writes=[dtB])
                op("pool", lambda e: e.tensor_tensor(out=qcs[b][:, sl], in0=tA[:, :], in1=tB[:, :], op=ALU.add),
                   reads=[dtA, dtB], writes=[dqcs[b]])
                if xi == 1:
                    store(g, qcs[b], dqcs[b])
            return cons

        def store_qr(g, t, d):
            for hh in range(2):
                dma("sp", qcat_d[2 * g + hh, 256:320, :], t[hh * 64:(hh + 1) * 64, :], d, reads=[d], writes=[d_qcat])
        lin_fm(wp, [W["w_uq_rope"], W["w_uq_rope_sw"]], 1024, 0, 1024, xchq, pp, rope_cons(cs, dcs, sn, dsn, store_qr))
        dma("sp", cs[:], C["c_cos128T"][:, :], dcs, writes=[dcs])
        dma("sp", sn[:], C["c_sin128T"][:, :], dsn, writes=[dsn])

        def store_qi(g, t, d):
            dma("sp", qidx_d[g, :, :], t[:], d, reads=[d], writes=[d_qidx])
        lin_fm(wp, [W["w_idx_q"], W["w_idx_q_sw"]], 1024, 0, 4096, xchq, pp, rope_cons(cs, dcs, sn, dsn, store_qi))
        fw.barrier()
    if stop_after <= 2:
        fw.barrier()
        return nc, fw, dbg_outs, {}

    def run_interleaved(gens):
        gens = list(gens)
        while gens:
            for g_ in list(gens):
                try:
                    next(g_)
                except StopIteration:
                    gens.remove(g_)

    SC_DSA = float(192 ** -0.5)
    with ExitStack() as st:
        pp = PsPool("pd", 4, st)
        acc = [fw.ps("accd%d" % i, [128, 512], F32, st) for i in range(3)]; dacc = [Dep() for _ in range(3)]
        ptm = fw.ps("ptm", [128, 1024], BF16, st); dptm = Dep()
        Ssc2 = [fw.sb("Ssc%d" % i, [128, S], F32, st) for i in range(2)]
        dS2 = [[Dep() for _ in range(4)] for _ in range(2)]
        wk = fw.sb("wk", [128, S], F32, st); dwk = Dep()
        qi2 = [fw.sb("qi%d" % i, [128, 32, 128], BF16, st) for i in range(2)]; dqi2 = [Dep(), Dep()]
        qc = fw.sb("qc", [128, 3, 16, 128], BF16, st); dqc = Dep()
        rl = [fw.sb("rl%d" % i, [128, 512], F32, st) for i in range(3)]; drl = [Dep(), Dep(), Dep()]
        m8 = fw.sb("m8", [128, 8], F32, st); dm8 = Dep()
        Mq = fw.sb("Mq", [128, S], BF16, st); dMq = Dep()
        MT = fw.sb("MT", [128, 16, 128], BF16, st); dMT = Dep()
        exs = [fw.sb("exs%d" % i, [128, 512], BF16, st) for i in range(3)]; dexs = [Dep(), Dep(), Dep()]
        PT = [fw.sb("PT%d" % i, [128, 512], BF16, st) for i in range(3)]; dPT = [Dep(), Dep(), Dep()]
        rden = fw.sb("rden", [128, 512], F32, st); drd = Dep()
        ol = fw.sb("ol", [128, 2, 512], BF16, st); dol = Dep()
        obst = fw.sb("obst", [128, 512], BF16, st); dob = Dep()
        wuv = fw.sb("wuv", [128, 16, 2, 128], BF16, st); dwuv = Dep()
        dma("pool", wuv[:], W["w_uv"].rearrange("h (rc r) v -> r h rc v", rc=2), dwuv, writes=[dwuv])
        causq = fw.sb("causq", [128, 128], F32, st); dcq = Dep()
        negm = fw.sb("negm", [128, 128], F32, st); dng = Dep()
        dma("sp", causq[:], C["c_causq"][:, :], dcq, writes=[dcq])
        op("dve", lambda e: e.tensor_scalar(out=negm[:], in0=causq[:], scalar1=-1.0, scalar2=1e30, op0=ALU.add, op1=ALU.mult),
           reads=[dcq], writes=[dng])
        cnts = {"rl": 0, "ex": 0}

        def chunks_of(i):
            nb_ = i + 1
            out_ = []
            for base, off in ((0, 0), (NQ, 128 * nb_)):
                for c0 in range(0, 128 * nb_, 512):
                    n = min(512, 128 * nb_ - c0)
                    out_.append((base + c0, n, off + c0))
            return out_

        def stage1(i):
            sb_ = i % 2
            Ssc, dSc, qi, dqi = Ssc2[sb_], dS2[sb_], qi2[sb_], dqi2[sb_]
            dma("sp", qi[:], qidx_d[:, :, i * 128:(i + 1) * 128].rearrange("h d q -> d h q"), dqi, reads=[d_qidx], writes=[dqi])
            chunks = chunks_of(i)
            for h in range(32):
                for ci, (t0, n, off) in enumerate(chunks):
                    ps, dp = pp.next()
                    op("pe", lambda e: e.matmul(ps[:, 0:n], lhsT=qi[:, h, :], rhs=kidxT[:, t0:t0 + n], start=True, stop=True),
                       reads=[dqi, d_kidxT], writes=[dp])
                    b = cnts["rl"] % 3
                    cnts["rl"] += 1
                    op("act", lambda e: e.activation(out=rl[b][:, 0:n], in_=ps[:, 0:n], func=AF.Relu), reads=[dp], writes=[drl[b]])
                    if h == 0:
                        op("dve", lambda e: e.tensor_scalar(out=Ssc[:, off:off + n], in0=rl[b][:, 0:n], scalar1=wi_sb[:, i, 0:1],
                                                            scalar2=None, op0=ALU.mult), reads=[drl[b], d_wi], writes=[dSc[ci]])
                    else:
                        op("dve", lambda e: e.scalar_tensor_tensor(out=Ssc[:, off:off + n], in0=rl[b][:, 0:n],
                                                                   scalar=wi_sb[:, i, h:h + 1], in1=Ssc[:, off:off + n],
                                                                   op0=ALU.mult, op1=ALU.add), reads=[drl[b], d_wi, dSc[ci]], writes=[dSc[ci]])
                    yield

        def stage23(i):
            sb_ = i % 2
            Ssc, dSc = Ssc2[sb_], dS2[sb_]
            nb_ = i + 1
            L = 256 * nb_
            chunks = chunks_of(i)
            dSall = dSc[0:len(chunks)]
            for ch in range(2):
                dma("sp", qc[:, ch, :, :], qcat_d[:, ch * 128:(ch + 1) * 128, i * 128:(i + 1) * 128].rearrange("h c q -> c h q"),
                    dqc, reads=[d_qcat], writes=[dqc])
            dma("sp", qc[0:64, 2, :, :], qcat_d[:, 256:320, i * 128:(i + 1) * 128].rearrange("h c q -> c h q"),
                dqc, reads=[d_qcat], writes=[dqc])
            dsl = slice(128 * i, 128 * (i + 1))
            op("dve", lambda e: e.tensor_tensor(out=Ssc[:, dsl], in0=Ssc[:, dsl], in1=causq[:], op=ALU.mult), reads=dSall + [dcq], writes=dSall)
            op("dve", lambda e: e.tensor_tensor(out=Ssc[:, dsl], in0=Ssc[:, dsl], in1=negm[:], op=ALU.add), reads=dSall + [dng], writes=dSall)
            osl = slice(128 * nb_ + 128 * i, 128 * nb_ + 128 * (i + 1))
            op("dve", lambda e: e.tensor_scalar(out=Ssc[:, osl], in0=Ssc[:, osl], scalar1=visnb[:, i, 0:1], scalar2=visnb[:, i, 2:3],
                                                op0=ALU.mult, op1=ALU.add), reads=dSall + [d_vis], writes=dSall)
            yield
            for r in range(32):
                src = Ssc if r == 0 else wk
                dsrc = dSall if r == 0 else [dwk]
                op("dve", lambda e: e.max(out=m8[:], in_=src[:, 0:L]), reads=dsrc, writes=[dm8])
                yield
                if r < 31:
                    op("dve", lambda e: e.match_replace(out=wk[:, 0:L], in_to_replace=m8[:], in_values=src[:, 0:L], imm_value=-3e38),
                       reads=dsrc + [dm8], writes=[dwk])
                    yield
            op("dve", lambda e: e.tensor_scalar(out=m8[:, 7:8], in0=m8[:, 7:8], scalar1=-1e29, scalar2=None, op0=ALU.max),
               reads=[dm8], writes=[dm8])
            op("dve", lambda e: e.tensor_scalar(out=Mq[:, 0:L], in0=Ssc[:, 0:L], scalar1=m8[:, 7:8], scalar2=None, op0=ALU.is_ge),
               reads=dSall + [dm8], writes=[dMq])
            yield
            nkb = 2 * nb_
            for k0 in range(0, nkb, 8):
                kn = min(8, nkb - k0)
                for j in range(kn):
                    op("pe", lambda e: e.transpose(ptm[:, j * 128:(j + 1) * 128], Mq[:, (k0 + j) * 128:(k0 + j + 1) * 128], ident_bf[:]),
                       reads=[dMq, d_idb], writes=[dptm], inc=(j == kn - 1))
                evac(MT[:, k0:k0 + kn, :], ptm[:, 0:kn * 128].rearrange("p (a b) -> p a b", a=kn), [dptm], [dMT])
                yield
            blks = list(range(nb_)) + [8 + m for m in range(nb_)]
            steps = [(hg, kb, bl) for hg in range(4) for kb, bl in enumerate(blks)]

            def front(stp):
                hg, kb, bl = stp
                t0 = bl * 128
                ps, dp = pp.next()
                for ch in range(3):
                    kp = 64 if ch == 2 else 128
                    op("pe", lambda e: e.matmul(ps[:, :], lhsT=kvcT[0:kp, ch, t0:t0 + 128],
                                                rhs=qc[0:kp, ch, hg * 4:(hg + 1) * 4, :].rearrange("p a b -> p (a b)"),
                                                start=(ch == 0), stop=(ch == 2)),
                       reads=[d_kvcT, dqc] if ch == 0 else (), writes=[dp], inc=(ch == 2))
                b = cnts["ex"] % 3
                cnts["ex"] += 1
                op("act", lambda e: e.activation(out=exs[b][:], in_=ps[:, :], func=AF.Exp, scale=SC_DSA), reads=[dp], writes=[dexs[b]])
                for hh in range(4):
                    eng = "dve" if hh % 2 == 0 else "pool"
                    op(eng, lambda e: e.tensor_tensor(out=PT[b][:, hh * 128:(hh + 1) * 128], in0=exs[b][:, hh * 128:(hh + 1) * 128],
                                                      in1=MT[:, kb, :], op=ALU.mult), reads=[dexs[b], dMT], writes=[dPT[b]])
                return b

            deferred = []

            def tail_pe(hg):
                ps, dp = pp.next()
                for hh in range(4):
                    h = hg * 4 + hh
                    for rc in range(2):
                        op("pe", lambda e: e.matmul(ps[:, hh * 128:(hh + 1) * 128], lhsT=wuv[:, h, rc, :], rhs=ol[:, rc, hh * 128:(hh + 1) * 128],
                                                    start=(rc == 0), stop=(rc == 1)),
                           reads=[dwuv, dol], writes=[dp], inc=(hh == 3 and rc == 1))
                evac(obst[:], ps[:, :], [dp], [dob])
                dma("sp", obT_d[hg * 4:(hg + 1) * 4, :, i * 128:(i + 1) * 128].rearrange("h v q -> v h q"),
                    obst[:].rearrange("p (a b) -> p a b", a=4), dob, reads=[dob], writes=[d_obT])

            def back(stp, b):
                hg, kb, bl = stp
                for a in range(3):
                    lh = ones_bf[:] if a == 2 else kvlat[:, bl, a * 128:(a + 1) * 128]
                    op("pe", lambda e: e.matmul(acc[a][:, :], lhsT=lh, rhs=PT[b][:], start=(kb == 0), stop=(kb == len(blks) - 1)),
                       reads=[dPT[b], d_kvlat, d_onb], writes=[dacc[a]], inc=(kb == len(blks) - 1))
                if kb == len(blks) - 1:
                    op("dve", lambda e: e.reciprocal(out=rden[:], in_=acc[2][:, :]), reads=[dacc[2]], writes=[drd])
                    for a in range(2):
                        op("dve", lambda e: e.tensor_tensor(out=ol[:, a, :], in0=acc[a][:, :], in1=rden[:], op=ALU.mult),
                           reads=[dacc[a], drd], writes=[dol])
                    deferred.append(hg)

            pend = None
            for stp in steps:
                b = front(stp)
                if pend is not None:
                    back(*pend)
                    if deferred and pend[0][0] != deferred[0]:
                        pass
                pend = (stp, b)
                while deferred and (deferred[0] != stp[0]):
                    tail_pe(deferred.pop(0))
                yield
            back(*pend)
            while deferred:
                tail_pe(deferred.pop(0))
            yield

        run_interleaved([stage1(0)])
        for i in range(8):
            gens = [stage23(i)]
            if i < 7:
                gens.append(stage1(i + 1))
            run_interleaved(gens)
        fw.barrier()
    if stop_after <= 3:
        fw.barrier()
        return nc, fw, dbg_outs, {}

    SC_FOX = float(128 ** -0.5)
    with ExitStack() as st:
        pp = PsPool("pe", 4, st, shape=(128, 128))
        ao = [fw.ps("ao%d" % i, [128, 128], F32, st) for i in range(2)]; dao = [Dep(), Dep()]
        ad = [fw.ps("ad%d" % i, [128, 128], F32, st) for i in range(2)]; dad = [Dep(), Dep()]
        Bias = fw.sb("Bias", [128, 8, 16, 16], F32, st); dB = Dep()
        for i in range(8):
            for bl in range(16):
                op("dve" if bl % 2 else "pool", lambda e: e.tensor_tensor(out=Bias[:, i, bl, :], in0=negck[:, bl, :], in1=Rref[:, i, :],
                                                                          op=ALU.subtract), reads=[d_negck, d_Rref], writes=[dB])
            op("dve", lambda e: e.tensor_scalar(out=Bias[:, i, 8 + i, :], in0=Bias[:, i, 8 + i, :], scalar1=visnb[:, i, 1:2], scalar2=None,
                                                op0=ALU.add), reads=[dB, d_vis], writes=[dB])
        kh = [fw.sb("kh%d" % i, [128, S], BF16, st) for i in range(2)]; dkh = [Dep(), Dep()]
        qh = [fw.sb("qh%d" % i, [128, NQ], BF16, st) for i in range(2)]; dqh = [Dep(), Dep()]
        vh = [fw.sb("vh%d" % i, [128, 16, 128], BF16, st) for i in range(2)]; dvh = [Dep(), Dep()]
        fost = [fw.sb("fost%d" % i, [128, NQ], BF16, st) for i in range(2)]; dfo = [Dep(), Dep()]
        ex = [[fw.sb("ex%d_%d" % (p_, i), [128, 128], BF16, st) for i in range(3)] for p_ in range(2)]
        dex = [[Dep(), Dep(), Dep()] for p_ in range(2)]
        rdn = [fw.sb("rdn%d" % i, [128, 128], F32, st) for i in range(2)]; drdn = [Dep(), Dep()]

        cnt_e = [0, 0]

        def fox_thread(hb):
            for h in range(hb, 16, 2):
                dma("sp", kh[hb][:], kT_d[h, :, :], dkh[hb], reads=[d_kT], writes=[dkh[hb]])
                dma("sp", qh[hb][:], qT_d[h, :, :], dqh[hb], reads=[d_qT], writes=[dqh[hb]])
                dma("sp", vh[hb][:], v_d[:, h * 128:(h + 1) * 128].rearrange("(b k) d -> k b d", k=128), dvh[hb], reads=[d_v], writes=[dvh[hb]])
                steps = [(i, kb, bl, len(range(i + 1)) * 2) for i in range(8) for kb, bl in enumerate(list(range(i + 1)) + [8 + m for m in range(i + 1)])]

                def front(stp):
                    i, kb, bl, nk = stp
                    ps, dp = pp.next()
                    op("pe", lambda e: e.matmul(ps[:, :], lhsT=kh[hb][:, bl * 128:(bl + 1) * 128], rhs=qh[hb][:, i * 128:(i + 1) * 128],
                                                start=True, stop=True), reads=[dkh[hb], dqh[hb]], writes=[dp])
                    b = cnt_e[hb] % 3
                    cnt_e[hb] += 1
                    op("act", lambda e: e.activation(out=ex[hb][b][:], in_=ps[:, :], func=AF.Exp, scale=SC_FOX, bias=Bias[:, i, bl, h:h + 1]),
                       reads=[dp, dB], writes=[dex[hb][b]])
                    if bl == i:
                        op("dve", lambda e: e.tensor_tensor(out=ex[hb][b][:], in0=ex[hb][b][:], in1=tri_bf[:], op=ALU.mult),
                           reads=[dex[hb][b], d_tri], writes=[dex[hb][b]])
                    return b

                def back(stp, b):
                    i, kb, bl, nk = stp
                    last = (kb == nk - 1)
                    op("pe", lambda e: e.matmul(ao[hb][:, :], lhsT=vh[hb][:, bl, :], rhs=ex[hb][b][:], start=(kb == 0), stop=last),
                       reads=[dvh[hb], dex[hb][b]], writes=[dao[hb]], inc=last)
                    op("pe", lambda e: e.matmul(ad[hb][:, :], lhsT=ones_bf[:], rhs=ex[hb][b][:], start=(kb == 0), stop=last),
                       reads=[d_onb, dex[hb][b]], writes=[dad[hb]], inc=last)
                    if last:
                        op("dve", lambda e: e.reciprocal(out=rdn[hb][:], in_=ad[hb][:, :]), reads=[dad[hb]], writes=[drdn[hb]])
                        op("dve", lambda e: e.tensor_tensor(out=fost[hb][:, i * 128:(i + 1) * 128], in0=ao[hb][:, :], in1=rdn[hb][:], op=ALU.mult),
                           reads=[dao[hb], drdn[hb]], writes=[dfo[hb]])

                pend = None
                for stp in steps:
                    b = front(stp)
                    if pend is not None:
                        back(*pend)
                    pend = (stp, b)
                    yield
                back(*pend)
                yield
                dma("sp", foT_d[h, :, :], fost[hb][:], dfo[hb], reads=[dfo[hb]], writes=[d_foT])

        run_interleaved([fox_thread(0), fox_thread(1)])
        fw.barrier()
    MS.close()
    if stop_after <= 4:
        fw.barrier()
        return nc, fw, dbg_outs, {}

    with ExitStack() as st:
        pp = PsPool("pf", 6, st)
        wp = WPool("wf", st, 3)
        foT = fw.sb("foT", [128, 16, NQ], BF16, st); dfoT = Dep()
        obT = fw.sb("obT", [128, 16, NQ], BF16, st); dobT = Dep()
        mg = fw.sb("mg", [128, 32, NQ], BF16, st); dmg = [Dep() for _ in range(32)]
        dma("sp", foT[:], foT_d.rearrange("h d q -> d h q"), dfoT, reads=[d_foT], writes=[dfoT])
        dma("sp", obT[:], obT_d.rearrange("h d q -> d h q"), dobT, reads=[d_obT], writes=[dobT])
        gt = [fw.sb("gt%d" % i, [128, NQ], BF16, st) for i in range(4)]; dgt = [Dep() for _ in range(4)]

        def gload(gi):
            dma("sp", gt[gi % 4][:], gT_d[gi, :, :], dgt[gi % 4], reads=[d_gT], writes=[dgt[gi % 4]])
        gload(0)
        gload(1)
        tmpb = fw.sb("tmpb", [128, 512], BF16, st); dtb = Dep()
        xfo = [(lambda c: foT[:, c, 0:512], [dfoT], 512), (lambda c: foT[:, c, 512:1024], [dfoT], 512)]
        xob = [(lambda c: obT[:, c, 0:512], [dobT], 512), (lambda c: obT[:, c, 512:1024], [dobT], 512)]

        def cons_ya(col, gw, xi, pss, dps):
            g = col // 128
            b = g % 4
            if xi == 0:
                gload(g + 2)
            op("dve", lambda e: e.tensor_tensor(out=mg[:, g, xi * 512:(xi + 1) * 512], in0=pss[0], in1=gt[b][:, xi * 512:(xi + 1) * 512],
                                                op=ALU.mult), reads=[dps[0], dgt[b]], writes=[dmg[g]])
        lin_fm(wp, [W["w_up_a"]], 2048, 0, D, xfo, pp, cons_ya)

        def cons_yb(col, gw, xi, pss, dps):
            g = col // 128
            b = (32 + g) % 4
            if xi == 0 and 32 + g + 2 < 64:
                gload(32 + g + 2)
            op("dve", lambda e: e.tensor_tensor(out=tmpb[:], in0=pss[0], in1=gt[b][:, xi * 512:(xi + 1) * 512], op=ALU.mult),
               reads=[dps[0], dgt[b]], writes=[dtb])
            op("pool", lambda e: e.tensor_tensor(out=mg[:, g, xi * 512:(xi + 1) * 512], in0=mg[:, g, xi * 512:(xi + 1) * 512], in1=tmpb[:],
                                                 op=ALU.add), reads=[dtb, dmg[g]], writes=[dmg[g]])
        lin_fm(wp, [W["w_up_b"]], 2048, 0, D, xob, pp, cons_yb)
        xt = [fw.sb("xt%d" % i, [128, 256], F32, st) for i in range(4)]; dxt = [Dep() for _ in range(4)]
        cn = [0]

        def cons_o(lo, n, t, ps, dp):
            b = cn[0] % 4
            cn[0] += 1
            dma("sp", xt[b][:, 0:n], x_seq[t * 128:(t + 1) * 128, lo:lo + n], dxt[b], writes=[dxt[b]])
            op("dve", lambda e: e.tensor_tensor(out=xt[b][:, 0:n], in0=ps, in1=xt[b][:, 0:n], op=ALU.add), reads=[dp, dxt[b]], writes=[dxt[b]])
            dma("act", x1_d[t * 128:(t + 1) * 128, lo:lo + n], xt[b][:, 0:n], dxt[b], reads=[dxt[b]], writes=[d_x1])
        lin_tm(wp, W["w_out"], D, 0, D, lambda c, t: mg[:, c, t * 128:(t + 1) * 128], lambda t: dmg, 8, pp, cons_o)
        fw.barrier()
    if stop_after <= 5:
        fw.barrier()
        return nc, fw, dbg_outs, {}

    SC_MEM = float(128 ** -0.5)
    with ExitStack() as st:
        memT = fw.sb("memT", [128, 32, 256], BF16, st); dmemT = [Dep(), Dep()]
        hxT = fw.sb("hxT", [128, 32, NQ], BF16, st); dhx = [Dep() for _ in range(8)]
        with ExitStack() as st2:
            norm_T("nM", st2, mem_in, 256, W["g_mem"], lambda t: (memT[:, :, t * 128:(t + 1) * 128], dmemT[t]))
            fw.barrier()
        with ExitStack() as st2:
            norm_T("nX", st2, x1_d, NQ, W["g_norm_mem_x"], lambda t: (hxT[:, :, t * 128:(t + 1) * 128], dhx[t]), src_deps=[d_x1])
            fw.barrier()
        pp = PsPool("pg", 4, st)
        wp = WPool("wg", st, 3)
        kmT = fw.sb("kmT", [128, 4, 256], BF16, st); dkm = Dep(multi=True)
        vm = fw.sb("vm", [128, 2, 512], BF16, st); dvm = Dep(multi=True)
        qmT = fw.sb("qmT", [128, 4, NQ], BF16, st); dqm = Dep(multi=True)
        omT = fw.sb("omT", [128, 4, NQ], BF16, st); dom = Dep(multi=True)
        lin_fm(wp, [W["w_km"]], D, 0, 512, [(lambda c: memT[:, c, :], dmemT, 256)], pp,
               lambda col, gw, xi, pss, dps: evac(kmT[:, col // 128, :], pss[0], [dps[0]], [dkm]))
        lin_tm(wp, W["w_vm"], D, 0, 512, lambda c, t: memT[:, c, t * 128:(t + 1) * 128], lambda t: [dmemT[t]], 2, pp,
               lambda lo, n, t, ps, dp: evac(vm[:, t, lo:lo + n], ps, [dp], [dvm]))
        xhx = [(lambda c: hxT[:, c, 0:512], dhx[0:4], 512), (lambda c: hxT[:, c, 512:1024], dhx[4:8], 512)]
        lin_fm(wp, [W["w_qm"]], D, 0, 512, xhx, pp,
               lambda col, gw, xi, pss, dps: evac(qmT[:, col // 128, xi * 512:(xi + 1) * 512], pss[0], [dps[0]], [dqm]))
        accm = [fw.ps("accm%d" % i, [128, 512], F32, st) for i in range(2)]; daccm = [Dep(), Dep()]
        exm = [fw.sb("exm%d" % i, [128, 512], BF16, st) for i in range(2)]; dexm = [Dep(), Dep()]
        rdm = fw.sb("rdm", [128, 512], F32, st); drdm = Dep()
        nex = 0
        for h in range(4):
            for tc in range(2):
                for mb in range(2):
                    ps, dp = pp.next()
                    op("pe", lambda e: e.matmul(ps[:, :], lhsT=kmT[:, h, mb * 128:(mb + 1) * 128], rhs=qmT[:, h, tc * 512:(tc + 1) * 512],
                                                start=True, stop=True), reads=[dkm, dqm], writes=[dp])
                    b = nex % 2
                    nex += 1
                    op("act", lambda e: e.activation(out=exm[b][:], in_=ps[:, :], func=AF.Exp, scale=SC_MEM), reads=[dp], writes=[dexm[b]])
                    op("pe", lambda e: e.matmul(accm[0][:, :], lhsT=vm[:, mb, h * 128:(h + 1) * 128], rhs=exm[b][:], start=(mb == 0), stop=(mb == 1)),
                       reads=[dvm, dexm[b]], writes=[daccm[0]], inc=(mb == 1))
                    op("pe", lambda e: e.matmul(accm[1][:, :], lhsT=ones_bf[:], rhs=exm[b][:], start=(mb == 0), stop=(mb == 1)),
                       reads=[d_onb, dexm[b]], writes=[daccm[1]], inc=(mb == 1))
                op("dve", lambda e: e.reciprocal(out=rdm[:], in_=accm[1][:, :]), reads=[daccm[1]], writes=[drdm])
                op("dve", lambda e: e.tensor_tensor(out=omT[:, h, tc * 512:(tc + 1) * 512], in0=accm[0][:, :], in1=rdm[:], op=ALU.mult),
                   reads=[daccm[0], drdm], writes=[dom])
        xt = [fw.sb("xtg%d" % i, [128, 256], F32, st) for i in range(4)]; dxt = [Dep() for _ in range(4)]
        cn = [0]

        def cons_om(lo, n, t, ps, dp):
            b = cn[0] % 4
            cn[0] += 1
            dma("sp", xt[b][:, 0:n], x1_d[t * 128:(t + 1) * 128, lo:lo + n], dxt[b], reads=[d_x1], writes=[dxt[b]])
            op("dve", lambda e: e.tensor_tensor(out=xt[b][:, 0:n], in0=ps, in1=xt[b][:, 0:n], op=ALU.add), reads=[dp, dxt[b]], writes=[dxt[b]])
            dma("act", x2_d[t * 128:(t + 1) * 128, lo:lo + n], xt[b][:, 0:n], dxt[b], reads=[dxt[b]], writes=[d_x2])
        lin_tm(wp, W["w_om"], 512, 0, D, lambda c, t: omT[:, c, t * 128:(t + 1) * 128], lambda t: [dom], 8, pp, cons_om)
        fw.barrier()
    if stop_after <= 6:
        fw.barrier()
        return nc, fw, dbg_outs, {}

    with ExitStack() as st:
        hf = fw.sb("hf", [128, 8, D], BF16, st); dhf = [Dep() for _ in range(8)]
        Sel = fw.sb("Sel", [128, 8, 8, CAP], BF16, st); dSel = Dep(multi=True)
        RWg = fw.sb("RWg", [128, 8, 2, 8], F32, st); dRWg = Dep(multi=True)
        oh = fw.sb("oh", [128, 8, 8], F32, st); doh = Dep(multi=True)
        RW = fw.sb("RW", [128, 8, 64], F32, st); dRW = Dep(multi=True)
        pp = PsPool("ph", 4, st)
        ptb = fw.ps("ptb", [128, 1024], BF16, st); dptb = Dep()
        with ExitStack() as s2:
            gbc = fw.sb("hgbc", [128, D], F32, s2); dg = Dep()
            dma("sp", gbc[:], W["g_norm_ffn"].partition_broadcast(128), dg, writes=[dg])
            xin = fw.sb("hxin", [128, D], F32, s2); dx = Dep()
            hT32 = fw.sb("hT32", [128, 32, 128], F32, s2); dh32 = Dep()
            wr = fw.sb("wr", [128, 32, 72], F32, s2); dwr = Dep()
            dma("sp", wr[:], W["w_r"].rearrange("(c p) n -> p c n", p=128), dwr, writes=[dwr])
            brb = fw.sb("brb", [128, 72], F32, s2); dbr = Dep()
            dma("sp", brb[:], W["b_r"].partition_broadcast(128), dbr, writes=[dbr])
            lg = fw.sb("lg", [128, 72], F32, s2); dlg = Dep()
            sm = fw.sb("sm", [128, 16], F32, s2); dsm = Dep()
            e8 = fw.sb("e8", [128, 8], F32, s2); de8 = Dep()
            les = fw.sb("les", [128, 8], F32, s2); dles = Dep()
            m8 = fw.sb("hm8", [128, 8], F32, s2); dm8 = Dep()
            o12 = fw.sb("o12", [128, 2, 8], F32, s2); do12 = Dep()
            rwl = fw.sb("rwl", [128, 8], F32, s2); drwl = Dep()
            plg = fw.ps("plg", [128, 72], F32, s2); dplg = Dep()
            ptr = [fw.ps("ptr%d" % i, [128, 512], F32, s2) for i in range(2)]; dptr = [Dep(), Dep()]
            ntr = 0
            for m in range(8):
                dma("sp", xin[:], x2_d[m * 128:(m + 1) * 128, :], dx, reads=[d_x2], writes=[dx])
                op("act", lambda e: e.activation(out=hf[:, m, :], in_=xin[:], func=AF.Square, accum_out=sm[:, 0:1]),
                   reads=[dx], writes=[dhf[m], dsm])
                op("dve", lambda e: e.tensor_scalar(out=sm[:, 1:2], in0=sm[:, 0:1], scalar1=1.0 / D, scalar2=EPS, op0=ALU.mult, op1=ALU.add),
                   reads=[dsm], writes=[dsm])
                op("act", lambda e: e.activation(out=sm[:, 1:2], in_=sm[:, 1:2], func=AF.Sqrt), reads=[dsm], writes=[dsm])
                op("dve", lambda e: e.reciprocal(out=sm[:, 1:2], in_=sm[:, 1:2]), reads=[dsm], writes=[dsm])
                op("dve", lambda e: e.scalar_tensor_tensor(out=xin[:], in0=xin[:], scalar=sm[:, 1:2], in1=gbc[:], op0=ALU.mult, op1=ALU.mult),
                   reads=[dx, dsm, dg], writes=[dx])
                op("act", lambda e: e.activation(out=hf[:, m, :], in_=xin[:], func=AF.Copy), reads=[dx], writes=[dhf[m]])
                for q in range(8):
                    b = ntr % 2
                    ntr += 1
                    for j in range(4):
                        c = q * 4 + j
                        op("pe", lambda e: e.transpose(ptr[b][:, j * 128:(j + 1) * 128], xin[:, c * 128:(c + 1) * 128], ident_f[:]),
                           reads=[dx, d_idf], writes=[dptr[b]], inc=(j == 3))
                    evac(hT32[:, q * 4:(q + 1) * 4, :], ptr[b][:, :].rearrange("p (a b) -> p a b", a=4), [dptr[b]], [dh32])
                for c in range(32):
                    op("pe", lambda e: e.matmul(plg[:, :], lhsT=hT32[:, c, :], rhs=wr[:, c, :], start=(c == 0), stop=(c == 31)),
                       reads=[dh32, dwr], writes=[dplg], inc=(c == 31))
                op("dve", lambda e: e.tensor_tensor(out=lg[:], in0=plg[:, :], in1=brb[:], op=ALU.add), reads=[dplg, dbr], writes=[dlg])
                op("dve", lambda e: e.tensor_reduce(out=sm[:, 2:3], in_=lg[:, 0:8], axis=AX.X, op=ALU.max), reads=[dlg], writes=[dsm])
                op("dve", lambda e: e.tensor_scalar(out=sm[:, 3:4], in0=sm[:, 2:3], scalar1=-1.0, scalar2=None, op0=ALU.mult), reads=[dsm], writes=[dsm])
                op("act", lambda e: e.activation(out=e8[:], in_=lg[:, 0:8], func=AF.Exp, bias=sm[:, 3:4], scale=1.0, accum_out=sm[:, 4:5]),
                   reads=[dlg, dsm], writes=[de8, dsm])
                op("dve", lambda e: e.reciprocal(out=sm[:, 5:6], in_=sm[:, 4:5]), reads=[dsm], writes=[dsm])
                op("dve", lambda e: e.tensor_scalar(out=oh[:, m, :], in0=lg[:, 0:8], scalar1=sm[:, 2:3], scalar2=None, op0=ALU.is_equal),
                   reads=[dlg, dsm], writes=[doh])
                op("dve", lambda e: e.tensor_scalar(out=les[:], in0=lg[:, 8:16], scalar1=oh[:, m, 0:1], scalar2=None, op0=ALU.mult),
                   reads=[dlg, doh], writes=[dles])
                for g in range(1, 8):
                    op("dve", lambda e: e.scalar_tensor_tensor(out=les[:], in0=lg[:, 8 + g * 8:16 + g * 8], scalar=oh[:, m, g:g + 1], in1=les[:],
                                                               op0=ALU.mult, op1=ALU.add), reads=[dlg, doh, dles], writes=[dles])
                op("dve", lambda e: e.max(out=m8[:], in_=les[:]), reads=[dles], writes=[dm8])
                op("dve", lambda e: e.tensor_tensor(out=sm[:, 6:7], in0=m8[:, 1:2], in1=m8[:, 0:1], op=ALU.subtract), reads=[dm8], writes=[dsm])
                op("act", lambda e: e.activation(out=sm[:, 7:8], in_=sm[:, 6:7], func=AF.Exp), reads=[dsm], writes=[dsm])
                op("dve", lambda e: e.tensor_scalar(out=sm[:, 7:8], in0=sm[:, 7:8], scalar1=1.0, scalar2=None, op0=ALU.add), reads=[dsm], writes=[dsm])
                op("dve", lambda e: e.reciprocal(out=sm[:, 8:9], in_=sm[:, 7:8]), reads=[dsm], writes=[dsm])
                op("dve", lambda e: e.tensor_tensor(out=sm[:, 9:10], in0=sm[:, 8:9], in1=sm[:, 5:6], op=ALU.mult), reads=[dsm], writes=[dsm])
                op("dve", lambda e: e.tensor_tensor(out=sm[:, 10:11], in0=sm[:, 5:6], in1=sm[:, 9:10], op=ALU.subtract), reads=[dsm], writes=[dsm])
                op("dve", lambda e: e.tensor_scalar(out=o12[:, 0, :], in0=les[:], scalar1=m8[:, 0:1], scalar2=sm[:, 9:10], op0=ALU.is_equal, op1=ALU.mult),
                   reads=[dles, dm8, dsm], writes=[do12])
                op("dve", lambda e: e.tensor_scalar(out=o12[:, 1, :], in0=les[:], scalar1=m8[:, 1:2], scalar2=sm[:, 10:11], op0=ALU.is_equal, op1=ALU.mult),
                   reads=[dles, dm8, dsm], writes=[do12])
                op("dve", lambda e: e.tensor_tensor(out=rwl[:], in0=o12[:, 0, :], in1=o12[:, 1, :], op=ALU.add), reads=[do12], writes=[drwl])
                for g in range(8):
                    op("dve", lambda e: e.tensor_scalar(out=RW[:, m, g * 8:(g + 1) * 8], in0=rwl[:], scalar1=oh[:, m, g:g + 1], scalar2=None, op0=ALU.mult),
                       reads=[drwl, doh], writes=[dRW])
            tril, dtril = fw.sb("tril", [128, 128], BF16, s2), Dep()
            dma("sp", tril[:], C["c_tril_bf"][:, :], dtril, writes=[dtril])
            iota, diota = fw.sb("iota", [128, CAP], F32, s2), Dep()
            dma("sp", iota[:], C["c_iota"][:, :], diota, writes=[diota])
            ohb = fw.sb("ohb", [128, 8, 8], BF16, s2); dohb = Dep()
            op("dve", lambda e: e.tensor_copy(out=ohb[:], in_=oh[:]), reads=[doh], writes=[dohb])
            RWh = fw.sb("RWh", [128, 8, 64], BF16, s2); dRWh = Dep()
            RWl = fw.sb("RWl", [128, 8, 64], BF16, s2); dRWl = Dep()
            op("dve", lambda e: e.tensor_copy(out=RWh[:], in_=RW[:]), reads=[dRW], writes=[dRWh])
            op("dve", lambda e: e.tensor_tensor(out=RWl[:], in0=RW[:], in1=RWh[:], op=ALU.subtract), reads=[dRW, dRWh], writes=[dRWl])
            t8 = fw.sb("t8", [128, 8], F32, s2); dt8 = Dep()
            posv = fw.sb("posv", [128, 1], F32, s2); dpos = Dep()
            pcn = plg; dpcn = dplg
            for m in range(8):
                for m2 in range(m + 1):
                    op("pe", lambda e: e.matmul(pcn[:, 0:8], lhsT=(ones_bf[:] if m2 < m else tril[:]), rhs=ohb[:, m2, :], start=(m2 == 0), stop=(m2 == m)),
                       reads=[d_onb, dtril, dohb], writes=[dpcn], inc=(m2 == m))
                op("dve", lambda e: e.tensor_tensor(out=t8[:], in0=pcn[:, 0:8], in1=oh[:, m, :], op=ALU.mult), reads=[dpcn, doh], writes=[dt8])
                op("dve", lambda e: e.tensor_reduce(out=posv[:], in_=t8[:], axis=AX.X, op=ALU.add), reads=[dt8], writes=[dpos])
                for g in range(8):
                    op("dve" if g % 2 else "pool", lambda e: e.tensor_scalar(out=Sel[:, m, g, :], in0=iota[:], scalar1=posv[:, 0:1], scalar2=oh[:, m, g:g + 1],
                                                                             op0=ALU.is_equal, op1=ALU.mult), reads=[diota, dpos, doh], writes=[dSel])
            for g in range(8):
                for sc in range(2):
                    ps, dp = pp.next()
                    for m in range(8):
                        for hl, (Rt, dRt) in enumerate(((RWh, dRWh), (RWl, dRWl))):
                            op("pe", lambda e: e.matmul(ps[:, 0:8], lhsT=Sel[:, m, g, sc * 128:(sc + 1) * 128], rhs=Rt[:, m, g * 8:(g + 1) * 8],
                                                        start=(m == 0 and hl == 0), stop=(m == 7 and hl == 1)),
                               reads=[dSel, dRt], writes=[dp], inc=(m == 7 and hl == 1))
                    evac(RWg[:, g, sc, :], ps[:, 0:8], [dp], [dRWg], eng="dve")
            fw.barrier()
        with ExitStack() as s2:
            wp = WPool("wh", s2, 4)
            xgT = fw.sb("xgT", [128, 32, CAP], BF16, s2); dxg = Dep()
            actT = fw.sb("actT", [128, 8, 4, CAP], BF16, s2); daT = Dep(multi=True)
            sg = fw.sb("sg", [128, 256], F32, s2); dsg = Dep()
            asb = [fw.sb("asb%d" % i, [128, 512], BF16, s2) for i in range(2)]; dasb = [Dep(), Dep()]
            ygst = [fw.sb("ygst%d" % i, [128, 256], F32, s2) for i in range(2)]; dyg = [Dep(), Dep()]
            ny = 0
            for g in range(8):
                for c in range(32):
                    ps, dp = pp.next()
                    for m in range(8):
                        op("pe", lambda e: e.matmul(ps[:, 0:CAP], lhsT=hf[:, m, c * 128:(c + 1) * 128], rhs=Sel[:, m, g, :], start=(m == 0), stop=(m == 7)),
                           reads=[dhf[m], dSel] if c == 0 else (), writes=[dp], inc=(m == 7))
                    evac(xgT[:, c, :], ps[:, 0:CAP], [dp], [dxg])
                for e8i in range(8):
                    ex_ = g * 8 + e8i
                    for fh in range(2):
                        wg, dwg = wp.load(W["w_gate"][ex_], D, fh * 256, 256, 256)
                        wu, dwu = wp.load(W["w_up"][ex_], D, fh * 256, 256, 256)
                        for s_ in range(2):
                            psg, dpg = pp.next()
                            for c in range(32):
                                op("pe", lambda e: e.matmul(psg[:, 0:256], lhsT=xgT[:, c, s_ * 128:(s_ + 1) * 128], rhs=wg[:, c, :], start=(c == 0), stop=(c == 31)),
                                   reads=[dxg, dwg] if c == 0 else (), writes=[dpg], inc=(c == 31))
                            psu, dpu = pp.next()
                            for c in range(32):
                                op("pe", lambda e: e.matmul(psu[:, 0:256], lhsT=xgT[:, c, s_ * 128:(s_ + 1) * 128], rhs=wu[:, c, :], start=(c == 0), stop=(c == 31)),
                                   reads=[dxg, dwu] if c == 0 else (), writes=[dpu], inc=(c == 31))
                            op("act", lambda e: e.activation(out=sg[:], in_=psg[:, 0:256], func=AF.Silu), reads=[dpg], writes=[dsg])
                            op("dve", lambda e: e.scalar_tensor_tensor(out=asb[s_][:, fh * 256:(fh + 1) * 256], in0=psu[:, 0:256],
                                                                       scalar=RWg[:, g, s_, e8i:e8i + 1], in1=sg[:], op0=ALU.mult, op1=ALU.mult),
                               reads=[dpu, dRWg, dsg], writes=[dasb[s_]])
                    for s_ in range(2):
                        for fc in range(4):
                            op("pe", lambda e: e.transpose(ptb[:, fc * 128:(fc + 1) * 128], asb[s_][:, fc * 128:(fc + 1) * 128], ident_bf[:]),
                               reads=[dasb[s_], d_idb], writes=[dptb], inc=(fc == 3))
                        evac(actT[:, e8i, :, s_ * 128:(s_ + 1) * 128], ptb[:, 0:512].rearrange("p (a b) -> p a b", a=4), [dptb], [daT])
                wdsrc = W["w_down"][g * 8:(g + 1) * 8].rearrange("e f n -> (e f) n")
                for ct in range(16):
                    wd, dwd = wp.load(wdsrc, D, ct * 256, 256, 256)
                    for s_ in range(2):
                        ps, dp = pp.next()
                        for kc in range(32):
                            op("pe", lambda e: e.matmul(ps[:, 0:256], lhsT=actT[:, kc // 4, kc % 4, s_ * 128:(s_ + 1) * 128], rhs=wd[:, kc, :],
                                                        start=(kc == 0), stop=(kc == 31)),
                               reads=[daT, dwd] if kc == 0 else (), writes=[dp], inc=(kc == 31))
                        b = ny % 2
                        ny += 1
                        evac(ygst[b][:], ps[:, 0:256], [dp], [dyg[b]])
                        dma("sp", yg_d[g, s_ * 128:(s_ + 1) * 128, ct * 256:(ct + 1) * 256], ygst[b][:], dyg[b], reads=[dyg[b]], writes=[d_yg])
            fw.barrier()
        with ExitStack() as s2:
            SelT = fw.sb("SelT", [128, 8, 2, NQ], BF16, s2); dST = Dep(multi=True)
            for g in range(8):
                for sc in range(2):
                    for m in range(8):
                        op("pe", lambda e: e.transpose(ptb[:, m * 128:(m + 1) * 128], Sel[:, m, g, sc * 128:(sc + 1) * 128], ident_bf[:]),
                           reads=[dSel, d_idb], writes=[dptb], inc=(m == 7))
                    evac(SelT[:, g, sc, :], ptb[:, :], [dptb], [dST])
            ygf = fw.sb("ygf", [128, 16, 512], F32, s2); dygf = Dep()
            yh = fw.sb("yh", [128, 16, 512], BF16, s2); dyh = Dep()
            yl = fw.sb("yl", [128, 16, 512], BF16, s2); dyl = Dep()
            xz = [fw.sb("xz%d" % i, [128, 512], F32, s2) for i in range(2)]; dxz = [Dep(), Dep()]
            nz = 0
            for ct in range(8):
                dma("sp", ygf[:], yg_d[:, :, ct * 512:(ct + 1) * 512].rearrange("g (sc s) n -> s (g sc) n", sc=2), dygf, reads=[d_yg], writes=[dygf])
                op("act", lambda e: e.activation(out=yh[:], in_=ygf[:], func=AF.Copy), reads=[dygf], writes=[dyh])
                op("dve", lambda e: e.tensor_tensor(out=yl[:], in0=ygf[:], in1=yh[:], op=ALU.subtract), reads=[dygf, dyh], writes=[dyl])
                for m in range(8):
                    ps, dp = pp.next()
                    k = 0
                    for gs in range(16):
                        for (Yt, dY) in ((yh, dyh), (yl, dyl)):
                            op("pe", lambda e: e.matmul(ps[:, :], lhsT=SelT[:, gs // 2, gs % 2, m * 128:(m + 1) * 128], rhs=Yt[:, gs, :],
                                                        start=(k == 0), stop=(k == 31)), reads=[dST, dY], writes=[dp], inc=(k == 31))
                            k += 1
                    b = nz % 2
                    nz += 1
                    dma("sp", xz[b][:], x2_d[m * 128:(m + 1) * 128, ct * 512:(ct + 1) * 512], dxz[b], reads=[d_x2], writes=[dxz[b]])
                    op("dve", lambda e: e.tensor_tensor(out=xz[b][:], in0=ps[:, :], in1=xz[b][:], op=ALU.add), reads=[dp, dxz[b]], writes=[dxz[b]])
                    dma("act", x1_d[m * 128:(m + 1) * 128, ct * 512:(ct + 1) * 512], xz[b][:], dxz[b], reads=[dxz[b]], writes=[d_x1])
            fw.barrier()
    with ExitStack() as st:
        gbc = fw.sb("fgbc", [128, D], F32, st); dg = Dep()
        dma("sp", gbc[:], W["g_final"].partition_broadcast(128), dg, writes=[dg])
        zin = [fw.sb("zin%d" % i, [128, D], F32, st) for i in range(2)]; dz = [Dep(), Dep()]
        zo = [fw.sb("zo%d" % i, [128, D], F32, st) for i in range(2)]; dzo = [Dep(), Dep()]
        sm = [fw.sb("fsm%d" % i, [128, 2], F32, st) for i in range(2)]; dsm = [Dep(), Dep()]
        for m in range(8):
            b = m % 2
            dma("sp", zin[b][:], x1_d[m * 128:(m + 1) * 128, :], dz[b], reads=[d_x1], writes=[dz[b]])
            op("act", lambda e: e.activation(out=zo[b][:], in_=zin[b][:], func=AF.Square, accum_out=sm[b][:, 0:1]), reads=[dz[b]], writes=[dzo[b], dsm[b]])
            op("dve", lambda e: e.tensor_scalar(out=sm[b][:, 1:2], in0=sm[b][:, 0:1], scalar1=1.0 / D, scalar2=EPS, op0=ALU.mult, op1=ALU.add),
               reads=[dsm[b]], writes=[dsm[b]])
            op("act", lambda e: e.activation(out=sm[b][:, 1:2], in_=sm[b][:, 1:2], func=AF.Sqrt), reads=[dsm[b]], writes=[dsm[b]])
            op("dve", lambda e: e.reciprocal(out=sm[b][:, 1:2], in_=sm[b][:, 1:2]), reads=[dsm[b]], writes=[dsm[b]])
            op("dve", lambda e: e.scalar_tensor_tensor(out=zo[b][:], in0=zin[b][:], scalar=sm[b][:, 1:2], in1=gbc[:], op0=ALU.mult, op1=ALU.mult),
               reads=[dz[b], dsm[b], dg], writes=[dzo[b]])
            dma("act", out_d[m * 128:(m + 1) * 128, :], zo[b][:], dzo[b], reads=[dzo[b]], writes=[d_out])
        fw.barrier()
    return nc, fw, dbg_outs, {}


_PROG = {}


def kernel(**inputs):
    hw = host_weights(inputs)
    x = np.asarray(inputs["x"], np.float32)
    mem = np.asarray(inputs["mem"], np.float32)
    if "nc" not in _PROG:
        _PROG["nc"] = build_program()[0]
    nc = _PROG["nc"]
    csts = [host_consts(0), host_consts(1)]
    in_maps = []
    for core in range(8):
        b, par = core // 2, core % 2
        perm = OWN[par] + OWN[1 - par]
        xb = np.ascontiguousarray(x[b].reshape(16, 128, D)[perm].reshape(S, D))
        m = {"x_seq": xb, "mem_b": np.ascontiguousarray(mem[b])}
        m.update(hw)
        m.update(csts[par])
        in_maps.append(m)
    res = run_bass_kernel_spmd(nc, in_maps, core_ids=list(range(8)))
    out = np.empty((4, S, D), np.float32)
    for core in range(8):
        b, par = core // 2, core % 2
        o = np.asarray(res.results[core]["out"], np.float32).reshape(8, 128, D)
        for i, blk in enumerate(OWN[par]):
            out[b, blk * 128:(blk + 1) * 128, :] = o[i]
    return out
```
